# Optimizing a Trainium2 kernel written in Bass

```python
import math
import jax, jax.numpy as jnp
from jax import lax
import numpy as np

D_MODEL = 4096
BATCH = 8
SEQ = 2048
DEPTH = 4

BRANCH_W = 1024
N_BRANCH = 3
A_HEADS = 8
A_DK = 128
A_DV = 128
A_WIDTH = A_HEADS * A_DK
A_CHUNK = 64
B_HEADS = 8
B_DH = 128
B_KV_RANK = 512
IDX_HEADS = 16
IDX_DH = 64
TOPK_MAX = 256
B_QBLOCK = 128
C_HEADS = 16
C_KV_HEADS = 2
C_DH = 64
WINDOW = 128
REL_BUCKETS = 32
REL_MAX_DIST = 128
D_FF = 4096
FFN_RES = 0.5
COND_RANK = 256
N_MOD = 9
RMS_EPS = 1e-6
NEG_INF = -1e30

IN_SPLITS = (
    A_WIDTH, A_WIDTH, A_HEADS * A_DV, A_HEADS * A_DV,
    B_HEADS * B_DH, B_KV_RANK, IDX_HEADS * IDX_DH, IDX_DH, IDX_HEADS,
    C_HEADS * C_DH, C_KV_HEADS * C_DH, C_KV_HEADS * C_DH,
    N_BRANCH * D_MODEL,
)
D_IN = sum(IN_SPLITS)

kernel_name = "hybrid_hgrn2_dsa_swa_macaron_block"


def rms_norm(x, g):
    x32 = x.astype(jnp.float32)
    y = x32 * lax.rsqrt(jnp.mean(x32 * x32, axis=-1, keepdims=True) + RMS_EPS)
    return (y * g.astype(jnp.float32)).astype(x.dtype)


def t5_bucket(dist):
    max_exact = REL_BUCKETS // 2
    d = jnp.maximum(dist, 0)
    df = jnp.maximum(d, 1).astype(jnp.float32)
    large = max_exact + (jnp.log(df / max_exact) / math.log(REL_MAX_DIST / max_exact)
                         * (REL_BUCKETS - max_exact)).astype(jnp.int32)
    large = jnp.minimum(large, REL_BUCKETS - 1)
    return jnp.where(d < max_exact, d, large)


def swiglu(h, w_in, w_out):
    u, v = jnp.split(h @ w_in, 2, axis=-1)
    return (jax.nn.silu(u) * v) @ w_out


def hgrn2_mixer(q, f_raw, i_raw, g, lb, norm_g):
    bsz, s, _ = q.shape
    nc = s // A_CHUNK
    f = lb + (1.0 - lb) * jax.nn.sigmoid(f_raw.astype(jnp.float32))
    log_f = jnp.log(jnp.maximum(f, 1e-20))
    k = 1.0 - f
    v = jax.nn.silu(i_raw.astype(jnp.float32))

    def to_chunks(t, d):
        return t.astype(jnp.float32).reshape(bsz, nc, A_CHUNK, A_HEADS, d).transpose(1, 0, 3, 2, 4)

    qc, kc, lfc = to_chunks(q, A_DK), to_chunks(k, A_DK), to_chunks(log_f, A_DK)
    vc = to_chunks(v, A_DV)
    causal = jnp.tril(jnp.ones((A_CHUNK, A_CHUNK), dtype=bool))

    def step(state, inp):
        qj, kj, vj, lfj = inp
        b = jnp.cumsum(lfj, axis=2)
        diff = b[:, :, :, None, :] - b[:, :, None, :, :]
        decay = jnp.where(causal[:, :, None], jnp.exp(jnp.minimum(diff, 0.0)), 0.0)
        attn = jnp.einsum('bhtd,bhsd,bhtsd->bhts', qj, kj, decay)
        o = jnp.einsum('bhts,bhsv->bhtv', attn, vj)
        o = o + jnp.einsum('bhtd,bhdv->bhtv', qj * jnp.exp(b), state)
        b_end = b[:, :, -1:, :]
        new_state = jnp.exp(b_end[:, :, 0, :])[..., None] * state + \
            jnp.einsum('bhsd,bhsv->bhdv', kj * jnp.exp(b_end - b), vj)
        return new_state, o

    s0 = jnp.zeros((bsz, A_HEADS, A_DK, A_DV), jnp.float32)
    _, o = lax.scan(step, s0, (qc, kc, vc, lfc))
    o = o.transpose(1, 0, 3, 2, 4).reshape(bsz, s, A_HEADS, A_DV)
    o = rms_norm(o, norm_g).reshape(bsz, s, A_HEADS * A_DV).astype(g.dtype)
    return o * jax.nn.silu(g)


def dsa_mixer(q, latent, iq, ik, iw, kv_norm_g, w_kv_up, rel_table_b):
    bsz, s, _ = q.shape
    kv = rms_norm(latent, kv_norm_g) @ w_kv_up
    k, v = jnp.split(kv, 2, axis=-1)
    nblk = s // B_QBLOCK
    topk = min(TOPK_MAX, s // 4)
    qb = q.reshape(bsz, nblk, B_QBLOCK, B_HEADS, B_DH).transpose(1, 0, 2, 3, 4)
    iqb = iq.reshape(bsz, nblk, B_QBLOCK, IDX_HEADS, IDX_DH).transpose(1, 0, 2, 3, 4)
    iwb = (iw * (IDX_HEADS ** -0.5 * IDX_DH ** -0.5)).reshape(
        bsz, nblk, B_QBLOCK, IDX_HEADS).transpose(1, 0, 2, 3)
    s_pos = jnp.arange(s)
    gather = jax.vmap(lambda table, idx: table[idx])

    def block(args):
        qj, iqj, iwj, j = args
        t_pos = j * B_QBLOCK + jnp.arange(B_QBLOCK)
        score = jax.nn.relu(jnp.einsum('bthd,bsd->bths', iqj, ik))
        score = jnp.einsum('bths,bth->bts', score, iwj).astype(jnp.float32)
        visible = s_pos[None, :] <= t_pos[:, None]
        score = jnp.where(visible[None], score, NEG_INF)
        _, idx = lax.top_k(score, topk)
        k_sel = gather(k, idx)
        v_sel = gather(v, idx)
        dist = t_pos[None, :, None] - idx
        bias = rel_table_b[t5_bucket(dist)].astype(jnp.float32)
        logits = jnp.einsum('bthd,btkd->bthk', qj, k_sel).astype(jnp.float32) * (B_DH ** -0.5) \
            + bias.transpose(0, 1, 3, 2)
        logits = jnp.where((dist >= 0)[:, :, None, :], logits, NEG_INF)
        p = jax.nn.softmax(logits, axis=-1).astype(v.dtype)
        return jnp.einsum('bthk,btkd->bthd', p, v_sel)

    o = lax.map(block, (qb, iqb, iwb, jnp.arange(nblk)))
    return o.transpose(1, 0, 2, 3, 4).reshape(bsz, s, B_HEADS * B_DH)


def swa_mixer(q, k, v, sinks, rel_table_c):
    bsz, s, _ = q.shape
    nb = s // WINDOW
    grp = C_HEADS // C_KV_HEADS
    qb = q.reshape(bsz, nb, WINDOW, C_KV_HEADS, grp, C_DH)

    def band(t):
        t = t.reshape(bsz, nb, WINDOW, C_KV_HEADS, C_DH)
        prev = jnp.pad(t, ((0, 0), (1, 0), (0, 0), (0, 0), (0, 0)))[:, :-1]
        return jnp.concatenate([prev, t], axis=2)

    kb, vb = band(k), band(v)
    r = jnp.arange(WINDOW)
    u = jnp.arange(2 * WINDOW)
    dist = r[:, None] + WINDOW - u[None, :]
    blk = jnp.arange(nb)
    key_ok = (blk[:, None] * WINDOW - WINDOW + u[None, :]) >= 0
    valid = ((dist >= 0) & (dist < WINDOW))[None] & key_ok[:, None, :]
    bias = rel_table_c[t5_bucket(dist)].astype(jnp.float32)
    bias = bias.reshape(WINDOW, 2 * WINDOW, C_KV_HEADS, grp).transpose(2, 3, 0, 1)
    logits = jnp.einsum('bnqhgd,bnkhd->bnhgqk', qb, kb).astype(jnp.float32) * (C_DH ** -0.5) + bias
    logits = jnp.where(valid[None, :, None, None], logits, NEG_INF)
    sink = jnp.broadcast_to(
        sinks.astype(jnp.float32).reshape(C_KV_HEADS, grp)[None, None, :, :, None, None],
        logits.shape[:-1] + (1,))
    p = jax.nn.softmax(jnp.concatenate([logits, sink], axis=-1), axis=-1)[..., :-1]
    o = jnp.einsum('bnhgqk,bnkhd->bnqhgd', p.astype(v.dtype), vb)
    return o.reshape(bsz, s, C_HEADS * C_DH)


def setup_inputs(seed: int = 0) -> dict:
    key = jax.random.key(seed)
    ks = jax.random.split(key, 18)

    def nrm(k, shape, scale):
        return jax.random.normal(k, shape, jnp.float32) * scale

    return {
        "x": nrm(ks[0], (BATCH, SEQ, D_MODEL), 1.0),
        "c": nrm(ks[1], (BATCH, D_MODEL), 1.0),
        "w_c_down": nrm(ks[2], (D_MODEL, COND_RANK), D_MODEL ** -0.5),
        "w_c_up": nrm(ks[3], (DEPTH, COND_RANK, N_MOD * D_MODEL), 0.5 * COND_RANK ** -0.5),
        "norm_gains": 1.0 + nrm(ks[4], (DEPTH, 6, D_MODEL), 0.1),
        "w_in": nrm(ks[5], (DEPTH, D_MODEL, D_IN), D_MODEL ** -0.5),
        "lb_logits": nrm(ks[6], (DEPTH, A_WIDTH), 1.0),
        "hgrn_norm": 1.0 + nrm(ks[7], (DEPTH, A_DV), 0.1),
        "kv_norm": 1.0 + nrm(ks[8], (DEPTH, B_KV_RANK), 0.1),
        "w_kv_up": nrm(ks[9], (DEPTH, B_KV_RANK, 2 * B_DH), B_KV_RANK ** -0.5),
        "rel_table": nrm(ks[10], (REL_BUCKETS, B_HEADS + C_HEADS), 0.5),
        "sinks": nrm(ks[11], (DEPTH, C_HEADS), 1.0),
        "w_branch": nrm(ks[12], (DEPTH, N_BRANCH, BRANCH_W, D_MODEL), BRANCH_W ** -0.5),
        "w_out": nrm(ks[13], (DEPTH, D_MODEL, D_MODEL), D_MODEL ** -0.5),
        "ffn1_in": nrm(ks[14], (DEPTH, D_MODEL, 2 * D_FF), D_MODEL ** -0.5),
        "ffn1_out": nrm(ks[15], (DEPTH, D_FF, D_MODEL), D_FF ** -0.5),
        "ffn2_in": nrm(ks[16], (DEPTH, D_MODEL, 2 * D_FF), D_MODEL ** -0.5),
        "ffn2_out": nrm(ks[17], (DEPTH, D_FF, D_MODEL), D_FF ** -0.5),
    }


def reference(x, c, w_c_down, w_c_up, norm_gains, w_in, lb_logits, hgrn_norm, kv_norm,
              w_kv_up, rel_table, sinks, w_branch, w_out, ffn1_in, ffn1_out, ffn2_in, ffn2_out):
    bsz, s, d = x.shape
    cond = jax.nn.silu(c @ w_c_down)
    lb_p = jax.nn.softmax(lb_logits.astype(jnp.float32), axis=0)
    lb_cs = jnp.cumsum(lb_p, axis=0)
    lower_bounds = lb_cs - lb_cs[0:1]
    split_at = np.cumsum(IN_SPLITS)[:-1].tolist()

    for l in range(DEPTH):
        mod = (cond @ w_c_up[l]).reshape(bsz, N_MOD, d)[:, :, None, :]
        g = norm_gains[l]

        h = rms_norm(x, g[0]) * (1.0 + mod[:, 1]) + mod[:, 0]
        y = swiglu(h, ffn1_in[l], ffn1_out[l])
        x = x + FFN_RES * mod[:, 2] * rms_norm(y, g[1])

        h = rms_norm(x, g[2]) * (1.0 + mod[:, 4]) + mod[:, 3]
        (aq, af, ai, ag, bq, blat, biq, bik, biw, cq, ck, cv, gates) = jnp.split(
            h @ w_in[l], split_at, axis=-1)
        ya = hgrn2_mixer(aq, af, ai, ag, lower_bounds[l], hgrn_norm[l])
        yb = dsa_mixer(bq, blat, biq, bik, biw, kv_norm[l], w_kv_up[l], rel_table[:, :B_HEADS])
        yc = swa_mixer(cq, ck, cv, sinks[l], rel_table[:, B_HEADS:])
        ga, gb, gc = jnp.split(jax.nn.sigmoid(gates), N_BRANCH, axis=-1)
        m = ga * (ya @ w_branch[l, 0]) + gb * (yb @ w_branch[l, 1]) + gc * (yc @ w_branch[l, 2])
        y = m @ w_out[l]
        x = x + mod[:, 5] * rms_norm(y, g[3])

        h = rms_norm(x, g[4]) * (1.0 + mod[:, 7]) + mod[:, 6]
        y = swiglu(h, ffn2_in[l], ffn2_out[l])
        x = x + FFN_RES * mod[:, 8] * rms_norm(y, g[5])
    return x
```

```python
import math
import contextlib
import numpy as np
import concourse.bass as bass
import concourse.mybir as mybir
from concourse.bass_utils import run_bass_kernel_spmd

DT = mybir.dt
F32, BF16 = DT.float32, DT.bfloat16
ALU = mybir.AluOpType
AF = mybir.ActivationFunctionType
AX = mybir.AxisListType

FULL = dict(D=4096, B=8, S=2048, DEPTH=4, DFF=4096, TOPK=256, NCORES=8)


class DSem:
    def __init__(self, sem):
        self.sem = sem
        self.count = 0


class Buf:
    def __init__(self, name):
        self.name = name
        self.writes = {}
        self.reads = {}


ENGS = ("pe", "act", "dve", "pool", "sp")


class Sched:
    SERIAL = False

    def __init__(self, nc, stack):
        self.nc = nc
        self.stack = stack
        self.h = dict(pe=nc.tensor, act=nc.scalar, dve=nc.vector, pool=nc.gpsimd, sp=nc.sync)
        self.q = {e: [] for e in ENGS}
        self.sem = {e: stack.enter_context(nc.semaphore("c_" + e)) for e in ENGS if e != "sp"}
        self.cnt = {e: 0 for e in ENGS}
        self.waited = {e: {} for e in ENGS}
        self.dsems = []
        self.dnext = 0
        self.ninst = 0

    def get_dsem(self):
        if self.dnext == len(self.dsems):
            self.dsems.append(DSem(self.stack.enter_context(self.nc.semaphore("d%d" % self.dnext))))
        self.dnext += 1
        return self.dsems[self.dnext - 1]

    def _wait(self, eng, key, val):
        if isinstance(key, DSem):
            val = key.count
            sem = key.sem
        else:
            if key == eng and eng in ("pe", "sp"):
                return
            sem = self.sem[key]
        w = self.waited[eng]
        if w.get(key, 0) >= val:
            return
        w[key] = val
        h = self.h[eng]
        self.q[eng].append(lambda e, sem=sem, val=val: e.wait_ge(sem, val))

    def op(self, eng, fn, reads=(), writes=(), accs=(), dsem=None):
        deps = []
        for b in reads:
            deps.extend(b.writes.items())
        for b in writes:
            deps.extend(b.writes.items())
            deps.extend(b.reads.items())
        for b in accs:
            deps.extend(b.reads.items())
        is_dma = dsem is not None
        if Sched.SERIAL is True or (Sched.SERIAL and eng in Sched.SERIAL):
            for k2 in ENGS:
                if k2 != "sp" and self.cnt[k2] > 0:
                    self._wait(eng, k2, self.cnt[k2])
            for d in self.dsems:
                if d.count:
                    self._wait(eng, d, d.count)
        for key, val in deps:
            if not is_dma and key == eng:
                if not any(key in b.writes for b in reads):
                    continue
            self._wait(eng, key, val)
        h = self.h[eng]
        if is_dma:
            dsem.count += 16
            tok = (dsem, dsem.count)
            sem, inc = dsem.sem, 16
        else:
            self.cnt[eng] += 1
            tok = (eng, self.cnt[eng])
            sem, inc = self.sem[eng], 1
        self.q[eng].append(lambda e, fn=fn, sem=sem, inc=inc: fn(e).then_inc(sem, inc))
        self.ninst += 1
        for b in reads:
            b.reads[tok[0]] = tok[1]
        for b in writes:
            b.writes = {tok[0]: tok[1]}
            b.reads = {}
        for b in accs:
            b.writes[tok[0]] = tok[1]
            b.reads = {}

    def dma(self, out, in_, reads=(), writes=(), accs=(), dsem=None, eng="sp", **kw):
        self.op(eng, lambda h: h.dma_start(out=out, in_=in_, **kw), reads=reads, writes=writes,
                accs=accs, dsem=dsem)

    def barrier(self):
        for e in ENGS:
            for k in ENGS:
                if k != "sp" and k != e and self.cnt[k] > 0:
                    self._wait(e, k, self.cnt[k])
            for d in self.dsems:
                if d.count:
                    self._wait(e, d, d.count)

    def flush(self):
        self.barrier()
        with self.nc.Block() as block:
            self._flush(block)
        self.q = {e: [] for e in ENGS}
        self.dnext = 0

    def _flush(self, block):
        q = self.q

        @block.tensor
        def _(e):
            for f in q["pe"]:
                f(e)

        @block.scalar
        def _(e):
            for f in q["act"]:
                f(e)

        @block.vector
        def _(e):
            for f in q["dve"]:
                f(e)

        @block.gpsimd
        def _(e):
            for f in q["pool"]:
                f(e)

        @block.sync
        def _(e):
            for f in q["sp"]:
                f(e)


class T:
    uid = 0

    def __init__(self, sched, stack, name, shape, dtype, psum=False, dma=False):
        nc = sched.nc
        alloc = nc.psum_tensor if psum else nc.sbuf_tensor
        T.uid += 1
        name = "%s_%d" % (name, T.uid)
        self.t = stack.enter_context(alloc(name, shape, dtype))
        self.b = Buf(name)
        self.d = sched.get_dsem() if dma else None

    def __getitem__(self, idx):
        return self.t[idx]


class K:
    def __init__(self, cfg):
        self.cfg = cfg
        self.nc = bass.Bass("TRN2", target_bir_lowering=False)
        self.stack = contextlib.ExitStack()
        self.s = Sched(self.nc, self.stack)
        self.dram = {}

    def ext(self, name, shape, kind="ExternalInput"):
        self.dram[name] = self.nc.dram_tensor(name, list(shape), F32, kind=kind).ap()
        return self.dram[name]

    def scratch(self, name, shape, dtype=F32):
        if name in self.cfg.get("dbg", ()):
            ap = self.nc.dram_tensor(name, list(shape), dtype, kind="ExternalOutput").ap()
        elif name in self.cfg.get("ext_in", ()):
            ap = self.nc.dram_tensor(name, list(shape), dtype, kind="ExternalInput").ap()
        else:
            ap = self.nc.dram_tensor(name, list(shape), dtype).ap()
        self.dram[name] = ap
        return ap

    def consts(self):
        s, st = self.s, self.stack
        self.identb = T(s, st, "identb", [128, 128], BF16)
        self.identf = T(s, st, "identf", [128, 128], F32)
        self.ones_row = T(s, st, "ones_row", [1, 128], F32)
        s.op("pool", lambda e: e.memset(self.identf[:], 0.0), writes=[self.identf.b])
        s.op("pool", lambda e: e.affine_select(out=self.identf[:], in_=self.identf[:], pattern=[[-1, 128]],
                                               compare_op=ALU.not_equal, fill=1.0, base=0, channel_multiplier=1),
             reads=[self.identf.b], accs=[self.identf.b])
        s.op("dve", lambda e: e.tensor_copy(self.identb[:], self.identf[:]), reads=[self.identf.b], writes=[self.identb.b])
        s.op("pool", lambda e: e.memset(self.ones_row[:], 1.0), writes=[self.ones_row.b])
        self.eps = T(s, st, "eps", [128, 1], F32)
        s.op("pool", lambda e: e.memset(self.eps[:], 1e-6), writes=[self.eps.b])
        s.flush()

    def scramble(self):
        s = self.s
        with contextlib.ExitStack() as ph:
            big = T(s, ph, "scr", [128, 40000], F32)
            ps = [T(s, ph, "scrp%d" % i, [128, 512], F32, psum=True) for i in range(8)]
            s.op("pool", lambda e: e.memset(big[:, 0:20000], 12345.0), writes=[big.b])
            s.op("dve", lambda e: e.memset(big[:, 20000:40000], -54321.0), accs=[big.b])
            for p in ps:
                s.op("dve", lambda e, p=p: e.memset(p[:], 777.0), writes=[p.b])
            s.flush()

    class WStream:
        def __init__(self, k, ph, nbuf=3, kc=4, n=512):
            self.k = k
            s = k.s
            self.raw = [T(s, ph, "wraw%d" % i, [128, kc, n], F32, dma=True) for i in range(nbuf)]
            self.bf = [T(s, ph, "wbf%d" % i, [128, kc, n], BF16) for i in range(nbuf)]
            self.i = 0
            self.pending = []
            self.loaded = 0
            self.nbuf = nbuf

        def plan(self, aps):
            self.pending = list(aps)
            self.issued = 0
            self.taken = 0
            for _ in range(min(self.nbuf - 1, len(self.pending))):
                self._issue()

        def _issue(self):
            ap = self.pending[self.issued]
            j = (self.i + self.issued) % self.nbuf
            raw = self.raw[j]
            kk, nn = ap.shape
            kc = kk // 128
            self.k.s.dma(raw[:, 0:kc, 0:nn], ap.rearrange("(c p) n -> p c n", p=128), writes=[raw.b], dsem=raw.d)
            self.issued += 1

        def next(self):
            s = self.k.s
            if self.issued < len(self.pending):
                self._issue()
            ap = self.pending[self.taken]
            j = (self.i + self.taken) % self.nbuf
            raw, bf = self.raw[j], self.bf[j]
            kk, nn = ap.shape
            kc = kk // 128
            s.op("pool", lambda e: e.tensor_copy(bf[:, 0:kc, 0:nn], raw[:, 0:kc, 0:nn]), reads=[raw.b], writes=[bf.b])
            self.taken += 1
            if self.taken == len(self.pending):
                self.i = (self.i + self.taken) % self.nbuf
            return bf, kc, nn


    def norm_block(self, xin, tok0, NT, BIG, aT, hT, xn, ss, rstd, ps, A, B, pctr):
        s, D = self.s, self.cfg["D"]
        KC = D // 128
        if True:
            if True:
                for t in range(NT):
                    s.dma(BIG[:, t, :], xin[tok0 + t * 128: tok0 + (t + 1) * 128, :], accs=[BIG.b] if t else (),
                          writes=() if t else [BIG.b], dsem=BIG.d)
                for t in range(NT):
                    s.op("dve", lambda e, t=t: e.tensor_tensor(out=xn, in0=BIG[:, t, :], in1=BIG[:, t, :], op=ALU.mult),
                         reads=[BIG.b], writes=[aT.b])
                    s.op("dve", lambda e, t=t: e.reduce_sum(out=ss[:, t:t + 1], in_=xn, axis=AX.X),
                         reads=[aT.b], writes=[ss.b] if t == 0 else (), accs=[ss.b] if t else ())
                    self.rsqrt_mean(rstd[:, t:t + 1], ss[:, t:t + 1], rstd.b, ss.b, D)
                    s.op("dve", lambda e, t=t: e.tensor_scalar(out=xn, in0=BIG[:, t, :], scalar1=rstd[:, t:t + 1], scalar2=None,
                                                          op0=ALU.mult), reads=[BIG.b, rstd.b], writes=[aT.b])
                    for g in range(0, KC, 8):
                        p = ps[pctr % 8]
                        pctr += 1
                        pb = p.t[:].bitcast(BF16)
                        nch = min(8, KC - g)
                        for c in range(nch):
                            s.op("pe", lambda e, c=c, g=g, pb=pb: e.transpose(pb[:, c * 128:(c + 1) * 128], xn[:, (g + c) * 128:(g + c + 1) * 128], self.identb[:]),
                                 reads=[aT.b, self.identb.b], writes=[p.b] if c == 0 else (), accs=[p.b] if c else ())
                        for c in range(nch):
                            s.op("act", lambda e, c=c, g=g, pb=pb, t=t: e.activation(out=hT[:, g + c, t * 128:(t + 1) * 128], in_=pb[:, c * 128:(c + 1) * 128],
                                                                                 func=AF.Identity, scale=A[:, g + c:g + c + 1], bias=B[:, g + c:g + c + 1]),
                                 reads=[p.b, A.b, B.b], writes=[hT.b] if (t == 0 and g == 0 and c == 0) else (),
                                 accs=() if (t == 0 and g == 0 and c == 0) else [hT.b])
        return pctr

    def ffn_phase(self, l, which, xin, xout, A, B, Gsrc):
        cfg, s = self.cfg, self.s
        D, DFF, S = cfg["D"], cfg["DFF"], cfg["S"]
        KC, FC = D // 128, DFF // 128
        TB = min(512, S)
        NT = TB // 128
        w_in = self.dram["ffn%d_in" % which][l]
        w_out = self.dram["ffn%d_out" % which][l]
        with contextlib.ExitStack() as ph:
            hT = T(s, ph, "hT", [128, KC, TB], BF16, dma=True)
            aT = T(s, ph, "aT", [128, FC, TB], BF16)
            BIG = T(s, ph, "BIG", [128, NT, D], F32, dma=True)
            G = T(s, ph, "G", [128, D], F32)
            grow = T(s, ph, "grow", [1, D], F32, dma=True)
            ss = T(s, ph, "ss", [128, NT], F32)
            rstd = T(s, ph, "rstd", [128, NT], F32)
            ssy = T(s, ph, "ssy", [128, NT, D // 512], F32)
            junk = T(s, ph, "junk", [128, 512], F32)
            su = [T(s, ph, "su%d" % i, [128, TB], F32) for i in range(2)]
            ps = [T(s, ph, "ps%d" % i, [128, 512], F32, psum=True) for i in range(8)]
            ws = K.WStream(self, ph)
            xn_v = aT.t[:, 0:KC // NT if False else FC, :]
            xn = aT.t[:].rearrange("p c t -> p (c t)")[:, 0:D]
            xh = hT.t[:].rearrange("p c t -> p (c t)").bitcast(F32)[:, 0:D]
            s.dma(grow[:], Gsrc, writes=[grow.b], dsem=grow.d)
            for n in range(D // 512):
                p = ps[n % 8]
                s.op("pe", lambda e, p=p, n=n: e.matmul(p[:], self.ones_row[:], grow[:, n * 512:(n + 1) * 512], start=True, stop=True),
                     reads=[grow.b, self.ones_row.b], writes=[p.b])
                s.op("act", lambda e, p=p, n=n: e.copy(G[:, n * 512:(n + 1) * 512], p[:]), reads=[p.b], accs=[G.b])
            if "dbg_G" in cfg.get("dbg", ()):
                dd = self.scratch("dbg_G", (128, D))
                s.dma(dd, G[:], reads=[G.b], dsem=BIG.d)
            pctr = 0
            for tb in range(S // TB):
                tok0 = tb * TB
                pctr = self.norm_block(xin, tok0, NT, BIG, aT, hT, xn, ss, rstd, ps, A, B, pctr)
                for fg in range(DFF // 512):
                    aps = []
                    for half in range(2):
                        for ks in range(D // 512):
                            aps.append(w_in[ks * 512:(ks + 1) * 512, half * DFF + fg * 512: half * DFF + (fg + 1) * 512])
                    ws.plan(aps)
                    for half in range(2):
                        for ks in range(D // 512):
                            bf, kc, nn = ws.next()
                            for k4 in range(kc):
                                kk = ks * 4 + k4
                                for c in range(4):
                                    p = ps[half * 4 + c]
                                    first = kk == 0
                                    s.op("pe", lambda e, p=p, bf=bf, k4=k4, c=c, kk=kk: e.matmul(p[:, 0:TB], bf[:, k4, c * 128:(c + 1) * 128], hT[:, kk, :],
                                                                                              start=(kk == 0), stop=(kk == KC - 1)),
                                         reads=[bf.b, hT.b], writes=[p.b] if first else (), accs=() if first else [p.b])
                    for c in range(4):
                        sb = su[c % 2]
                        s.op("act", lambda e, c=c, sb=sb: e.activation(out=sb[:], in_=ps[c][:, 0:TB], func=AF.Silu), reads=[ps[c].b], writes=[sb.b])
                        first = (fg == 0 and c == 0)
                        s.op("dve", lambda e, c=c, sb=sb, fg=fg: e.tensor_tensor(out=aT[:, fg * 4 + c, :], in0=sb[:], in1=ps[4 + c][:, 0:TB], op=ALU.mult),
                             reads=[sb.b, ps[4 + c].b], writes=[aT.b] if first else (), accs=() if first else [aT.b])
                for ng in range(D // 512):
                    ws.plan([w_out[ks * 512:(ks + 1) * 512, ng * 512:(ng + 1) * 512] for ks in range(DFF // 512)])
                    pb0 = (ng % 2) * 4
                    for ks in range(DFF // 512):
                        bf, kc, nn = ws.next()
                        for k4 in range(kc):
                            kk = ks * 4 + k4
                            for t in range(NT):
                                p = ps[pb0 + t]
                                first = kk == 0
                                s.op("pe", lambda e, p=p, bf=bf, k4=k4, t=t, kk=kk: e.matmul(p[:], aT[:, kk, t * 128:(t + 1) * 128], bf[:, k4, :],
                                                                                          start=(kk == 0), stop=(kk == FC - 1)),
                                     reads=[bf.b, aT.b], writes=[p.b] if first else (), accs=() if first else [p.b])
                    for t in range(NT):
                        p = ps[pb0 + t]
                        first = (ng == 0 and t == 0)
                        s.op("act", lambda e, p=p, t=t, ng=ng: e.copy(BIG[:, t, ng * 512:(ng + 1) * 512], p[:]), reads=[p.b],
                             writes=[BIG.b] if first else (), accs=() if first else [BIG.b])
                        s.op("dve", lambda e, t=t, ng=ng: e.tensor_tensor(out=junk[:], in0=BIG[:, t, ng * 512:(ng + 1) * 512],
                                                                       in1=BIG[:, t, ng * 512:(ng + 1) * 512], op=ALU.mult),
                             reads=[BIG.b], writes=[junk.b])
                        s.op("dve", lambda e, t=t, ng=ng: e.reduce_sum(out=ssy[:, t, ng:ng + 1], in_=junk[:], axis=AX.X),
                             reads=[junk.b], accs=[ssy.b])
                if "dbg_y" in cfg.get("dbg", ()) and tb == 0:
                    dd = self.scratch("dbg_y", (128, NT * D))
                    s.dma(dd, BIG[:].rearrange("p t d -> p (t d)"), reads=[BIG.b], dsem=BIG.d)
                    dd2 = self.scratch("dbg_a", (128, FC * TB), BF16)
                    s.dma(dd2, aT[:].rearrange("p c t -> p (c t)"), reads=[aT.b], dsem=BIG.d)
                for t in range(NT):
                    s.op("dve", lambda e, t=t: e.reduce_sum(out=ss[:, t:t + 1], in_=ssy[:, t, :], axis=AX.X), reads=[ssy.b], accs=[ss.b])
                    self.rsqrt_mean(rstd[:, t:t + 1], ss[:, t:t + 1], rstd.b, ss.b, D)
                    s.dma(xh, xin[tok0 + t * 128: tok0 + (t + 1) * 128, :], writes=[hT.b], dsem=hT.d)
                    s.op("dve", lambda e, t=t: e.scalar_tensor_tensor(out=BIG[:, t, :], in0=BIG[:, t, :], scalar=rstd[:, t:t + 1], in1=G[:],
                                                                 op0=ALU.mult, op1=ALU.mult), reads=[BIG.b, rstd.b, G.b], accs=[BIG.b])
                    s.op("dve", lambda e, t=t: e.tensor_tensor(out=BIG[:, t, :], in0=BIG[:, t, :], in1=xh, op=ALU.add),
                         reads=[BIG.b, hT.b], accs=[BIG.b])
                    s.dma(xout[tok0 + t * 128: tok0 + (t + 1) * 128, :], BIG[:, t, :], reads=[BIG.b], dsem=BIG.d)
            s.flush()


    def merge_phase(self, l, xin, xout, Gsrc):
        cfg, s = self.cfg, self.s
        D, S = cfg["D"], cfg["S"]
        KC = D // 128
        TB = min(512, S)
        NT = TB // 128
        w_out = self.dram["w_out"][l]
        w_br = self.dram["w_branch"][l]
        gT = self.mx["gT"]
        yT = self.mx["yT"]
        with contextlib.ExitStack() as ph:
            mT = T(s, ph, "mT", [128, KC, TB], BF16)
            BIG = T(s, ph, "BIG", [128, NT, D], F32, dma=True)
            G = T(s, ph, "G", [128, D], F32)
            grow = T(s, ph, "grow", [1, D], F32, dma=True)
            ss = T(s, ph, "ss", [128, NT], F32)
            rstd = T(s, ph, "rstd", [128, NT], F32)
            ssy = T(s, ph, "ssy", [128, NT, D // 512], F32)
            junk = T(s, ph, "junk", [128, 512], F32)
            ybuf = T(s, ph, "ybuf", [128, 3, 8, TB], BF16, dma=True)
            gts = [T(s, ph, "gts%d" % i, [128, TB], F32, dma=True) for i in range(4)]
            macc = T(s, ph, "macc", [128, 4, TB], F32)
            tmp = T(s, ph, "tmp", [128, TB], F32)
            ps = [T(s, ph, "ps%d" % i, [128, 512], F32, psum=True) for i in range(8)]
            ws = K.WStream(self, ph, nbuf=2)
            xh = ybuf.t[:].rearrange("p a c t -> p (a c t)").bitcast(F32)[:, 0:D]
            s.dma(grow[:], Gsrc, writes=[grow.b], dsem=grow.d)
            for n in range(D // 512):
                p = ps[n % 8]
                s.op("pe", lambda e, p=p, n=n: e.matmul(p[:], self.ones_row[:], grow[:, n * 512:(n + 1) * 512], start=True, stop=True),
                     reads=[grow.b, self.ones_row.b], writes=[p.b])
                s.op("act", lambda e, p=p, n=n: e.copy(G[:, n * 512:(n + 1) * 512], p[:]), reads=[p.b], accs=[G.b])
            gi = 0
            grp = 0
            for tb in range(S // TB):
                tok0 = tb * TB
                for br in range(3):
                    s.dma(ybuf[:, br, :, :], yT[br].rearrange("(c p) s -> p c s", p=128)[:, :, tok0:tok0 + TB],
                          writes=[ybuf.b] if br == 0 else (), accs=[ybuf.b] if br else (), dsem=ybuf.d)
                for dg in range(D // 512):
                    for br in range(3):
                        ws.plan([w_br[br, ks * 512:(ks + 1) * 512, dg * 512:(dg + 1) * 512] for ks in range(2)])
                        pb0 = (grp % 2) * 4
                        grp += 1
                        for ks in range(2):
                            bf, kc, nn = ws.next()
                            for k4 in range(kc):
                                kk = ks * 4 + k4
                                for c in range(4):
                                    p = ps[pb0 + c]
                                    first = kk == 0
                                    s.op("pe", lambda e, p=p, bf=bf, k4=k4, c=c, kk=kk, br=br: e.matmul(
                                        p[:, 0:TB], bf[:, k4, c * 128:(c + 1) * 128], ybuf[:, br, kk, :], start=(kk == 0), stop=(kk == 7)),
                                        reads=[bf.b, ybuf.b], writes=[p.b] if first else (), accs=() if first else [p.b])
                        for c in range(4):
                            p = ps[pb0 + c]
                            dch = dg * 4 + c
                            gt = gts[gi % 4]
                            gi += 1
                            s.dma(gt[:], gT[br * D + dch * 128: br * D + (dch + 1) * 128, tok0:tok0 + TB], writes=[gt.b], dsem=gt.d)
                            if br == 0:
                                s.op("dve", lambda e, p=p, c=c, gt=gt: e.tensor_tensor(out=macc[:, c, :], in0=p[:, 0:TB], in1=gt[:], op=ALU.mult),
                                     reads=[p.b, gt.b], writes=[macc.b] if c == 0 else (), accs=[macc.b] if c else ())
                            else:
                                s.op("dve", lambda e, p=p, gt=gt: e.tensor_tensor(out=tmp[:], in0=p[:, 0:TB], in1=gt[:], op=ALU.mult),
                                     reads=[p.b, gt.b], writes=[tmp.b])
                                s.op("dve", lambda e, c=c: e.tensor_tensor(out=macc[:, c, :], in0=macc[:, c, :], in1=tmp[:], op=ALU.add),
                                     reads=[macc.b, tmp.b], accs=[macc.b])
                            if br == 2:
                                first = (dg == 0 and c == 0)
                                s.op("act", lambda e, c=c, dch=dch: e.copy(mT[:, dch, :], macc[:, c, :]), reads=[macc.b],
                                     writes=[mT.b] if first else (), accs=() if first else [mT.b])
                for ng in range(D // 512):
                    ws.plan([w_out[ks * 512:(ks + 1) * 512, ng * 512:(ng + 1) * 512] for ks in range(D // 512)])
                    pb0 = (ng % 2) * 4
                    for ks in range(D // 512):
                        bf, kc, nn = ws.next()
                        for k4 in range(kc):
                            kk = ks * 4 + k4
                            for t in range(NT):
                                p = ps[pb0 + t]
                                first = kk == 0
                                s.op("pe", lambda e, p=p, bf=bf, k4=k4, t=t, kk=kk: e.matmul(p[:], mT[:, kk, t * 128:(t + 1) * 128], bf[:, k4, :],
                                                                                          start=(kk == 0), stop=(kk == KC - 1)),
                                     reads=[bf.b, mT.b], writes=[p.b] if first else (), accs=() if first else [p.b])
                    for t in range(NT):
                        p = ps[pb0 + t]
                        first = (ng == 0 and t == 0)
                        s.op("act", lambda e, p=p, t=t, ng=ng: e.copy(BIG[:, t, ng * 512:(ng + 1) * 512], p[:]), reads=[p.b],
                             writes=[BIG.b] if first else (), accs=() if first else [BIG.b])
                        s.op("dve", lambda e, t=t, ng=ng: e.tensor_tensor(out=junk[:], in0=BIG[:, t, ng * 512:(ng + 1) * 512],
                                                                       in1=BIG[:, t, ng * 512:(ng + 1) * 512], op=ALU.mult),
                             reads=[BIG.b], writes=[junk.b])
                        s.op("dve", lambda e, t=t, ng=ng: e.reduce_sum(out=ssy[:, t, ng:ng + 1], in_=junk[:], axis=AX.X),
                             reads=[junk.b], accs=[ssy.b])
                for t in range(NT):
                    s.op("dve", lambda e, t=t: e.reduce_sum(out=ss[:, t:t + 1], in_=ssy[:, t, :], axis=AX.X), reads=[ssy.b], accs=[ss.b])
                    self.rsqrt_mean(rstd[:, t:t + 1], ss[:, t:t + 1], rstd.b, ss.b, D)
                    s.dma(xh, xin[tok0 + t * 128: tok0 + (t + 1) * 128, :], writes=[ybuf.b], dsem=ybuf.d)
                    s.op("dve", lambda e, t=t: e.scalar_tensor_tensor(out=BIG[:, t, :], in0=BIG[:, t, :], scalar=rstd[:, t:t + 1], in1=G[:],
                                                                 op0=ALU.mult, op1=ALU.mult), reads=[BIG.b, rstd.b, G.b], accs=[BIG.b])
                    s.op("dve", lambda e, t=t: e.tensor_tensor(out=BIG[:, t, :], in0=BIG[:, t, :], in1=xh, op=ALU.add),
                         reads=[BIG.b, ybuf.b], accs=[BIG.b])
                    s.dma(xout[tok0 + t * 128: tok0 + (t + 1) * 128, :], BIG[:, t, :], reads=[BIG.b], dsem=BIG.d)
            s.flush()

    def rsqrt_mean(self, out, ssq, outb, ssb, n, npart=128):
        s = self.s
        s.op("act", lambda e: e.activation(out=out, in_=ssq, func=AF.Sqrt, scale=1.0 / n, bias=self.eps[0:npart, 0:1]), reads=[ssb, self.eps.b], accs=[outb])
        s.op("dve", lambda e: e.reciprocal(out, out), reads=[outb], accs=[outb])

    def colload(self, ph, dst_ap, src_vec, C, ps, name):
        s = self.s
        tmp = T(s, ph, "cl_" + name, [C, 128], F32, dma=True)
        s.dma(tmp[:], src_vec.rearrange("(c p) -> c p", p=128), writes=[tmp.b], dsem=tmp.d)
        s.op("pe", lambda e: e.transpose(ps[:, 0:C], tmp[:], self.identf[0:C, 0:C]), reads=[tmp.b, self.identf.b], writes=[ps.b])
        return ps

    def mod_setup(self):
        s, st, cfg = self.s, self.stack, self.cfg
        KC = cfg["D"] // 128
        self.AB = [T(s, st, "AB%d" % i, [128, KC], F32) for i in range(6)]
        self.condT = T(s, st, "condT", [128, 2], F32)

    def cond_phase(self, cvec):
        s, cfg = self.s, self.cfg
        D = cfg["D"]
        KC = D // 128
        wcd = self.dram["w_c_down"]
        with contextlib.ExitStack() as ph:
            ps = [T(s, ph, "ps%d" % i, [128, 512], F32, psum=True) for i in range(2)]
            cT = T(s, ph, "cT", [128, KC], F32)
            w = T(s, ph, "wcd", [128, KC, 256], F32, dma=True)
            self.colload(ph, None, cvec, KC, ps[0], "c")
            s.op("dve", lambda e: e.tensor_copy(cT[:], ps[0][:, 0:KC]), reads=[ps[0].b], writes=[cT.b])
            s.dma(w[:], wcd.rearrange("(c p) r -> p c r", p=128), writes=[w.b], dsem=w.d)
            for rc in range(2):
                for c in range(KC):
                    s.op("pe", lambda e, rc=rc, c=c: e.matmul(ps[1][:, rc:rc + 1], w[:, c, rc * 128:(rc + 1) * 128], cT[:, c:c + 1],
                                                         start=(c == 0), stop=(c == KC - 1)),
                         reads=[w.b, cT.b], writes=[ps[1].b] if (c == 0 and rc == 0) else (), accs=() if (c == 0 and rc == 0) else [ps[1].b])
            s.op("act", lambda e: e.activation(out=self.condT[:], in_=ps[1][:, 0:2], func=AF.Silu), reads=[ps[1].b], writes=[self.condT.b])
            s.flush()

    def mod_phase(self, l, grow_dram):
        s, cfg = self.s, self.cfg
        D = cfg["D"]
        KC = D // 128
        wcu = self.dram["w_c_up"][l]
        gains = self.dram["norm_gains"][l]
        PW = min(1024, D)
        with contextlib.ExitStack() as ph:
            ps = [T(s, ph, "ps%d" % i, [128, 512], F32, psum=True) for i in range(4)]
            piece = [T(s, ph, "wcu%d" % i, [128, 2, PW], F32, dma=True) for i in range(2)]
            gcol = T(s, ph, "gcol", [128, KC], F32)
            grow = T(s, ph, "grow", [1, D], F32, dma=True)
            gain_row = T(s, ph, "gain_row", [1, D], F32, dma=True)
            pi = 0
            for sub in range(3):
                self.colload(ph, None, gains[2 * sub], KC, ps[0], "g%d" % sub)
                s.op("dve", lambda e: e.tensor_copy(gcol[:], ps[0][:, 0:KC]), reads=[ps[0].b], writes=[gcol.b])
                for which in range(3):
                    m = 3 * sub + which
                    if which == 2:
                        s.dma(gain_row[:], gains[2 * sub + 1].rearrange("(o d) -> o d", o=1), writes=[gain_row.b], dsem=gain_row.d)
                    for pc in range(D // PW):
                        pt = piece[pi % 2]
                        pi += 1
                        s.dma(pt[:], wcu[:, m * D + pc * PW: m * D + (pc + 1) * PW].rearrange("(c p) n -> p c n", p=128),
                              writes=[pt.b], dsem=pt.d)
                        if which < 2:
                            pcol = ps[1 + which]
                            for cc in range(PW // 128):
                                c = pc * (PW // 128) + cc
                                for rc in range(2):
                                    first = (c == 0 and rc == 0)
                                    s.op("pe", lambda e, pt=pt, rc=rc, cc=cc, c=c, pcol=pcol: e.matmul(
                                        pcol[:, c:c + 1], pt[:, rc, cc * 128:(cc + 1) * 128], self.condT[:, rc:rc + 1], start=(rc == 0), stop=(rc == 1)),
                                        reads=[pt.b, self.condT.b], writes=[pcol.b] if first else (), accs=() if first else [pcol.b])
                        else:
                            for n in range(PW // 512):
                                for rc in range(2):
                                    s.op("pe", lambda e, pt=pt, rc=rc, n=n: e.matmul(ps[3][0:1, :], self.condT[:, rc:rc + 1], pt[:, rc, n * 512:(n + 1) * 512],
                                                                                 start=(rc == 0), stop=(rc == 1)),
                                         reads=[pt.b, self.condT.b], writes=[ps[3].b] if rc == 0 else (), accs=[ps[3].b] if rc else ())
                                col0 = pc * PW + n * 512
                                res = 0.5 if sub != 1 else 1.0
                                s.op("dve", lambda e, col0=col0, res=res: e.scalar_tensor_tensor(
                                    out=grow[:, col0:col0 + 512], in0=ps[3][0:1, :], scalar=res, in1=gain_row[:, col0:col0 + 512],
                                    op0=ALU.mult, op1=ALU.mult), reads=[ps[3].b, gain_row.b], accs=[grow.b])
                    if which == 0:
                        s.op("dve", lambda e, sub=sub: e.tensor_copy(self.AB[2 * sub + 1][:], ps[1][:, 0:KC]), reads=[ps[1].b], writes=[self.AB[2 * sub + 1].b])
                    elif which == 1:
                        s.op("dve", lambda e, sub=sub: e.scalar_tensor_tensor(out=self.AB[2 * sub][:], in0=ps[2][:, 0:KC], scalar=1.0, in1=gcol[:],
                                                                         op0=ALU.add, op1=ALU.mult),
                             reads=[ps[2].b, gcol.b], writes=[self.AB[2 * sub].b])
                    else:
                        s.dma(grow_dram[sub], grow[:], reads=[grow.b], dsem=grow.d)
            s.flush()

    def mixer_scratch(self):
        cfg = self.cfg
        S, D = cfg["S"], cfg["D"]
        sc = self.scratch
        self.mx = dict(
            aqT=sc("aqT", (1024, S)), afT=sc("afT", (1024, S)), af=sc("af", (S, 1024)), ai=sc("ai", (S, 1024)),
            ag=sc("ag", (S, 1024)), bqT=sc("bqT", (1024, S)), blat=sc("blat", (S, 512)), biqT=sc("biqT", (1024, S)),
            bikT=sc("bikT", (64, S)), biw=sc("biw", (S, 16)), cqT=sc("cqT", (1024, S)), ckT=sc("ckT", (128, S)),
            cv=sc("cv", (S, 128)), gT=sc("gT", (3 * D, S)),
            yT=sc("yT", (3, 1024, S), BF16),
        )

    def proj_phase(self, l, xin, A, B):
        cfg, s = self.cfg, self.s
        D, S = cfg["D"], cfg["S"]
        KC = D // 128
        TB = min(512, S)
        NT = TB // 128
        w_in = self.dram["w_in"][l]
        mx = self.mx
        jobs = []
        col = 0
        for name, ncols, modes in (("aq", 1024, ("fm",)), ("af", 1024, ("fm", "tm")), ("ai", 1024, ("tm",)), ("ag", 1024, ("tm",)),
                                   ("bq", 1024, ("fm",)), ("blat", 512, ("tm",)), ("biq", 1024, ("fm",)), ("bik", 64, ("fm",)),
                                   ("biw", 16, ("tm",)), ("cq", 1024, ("fm",)), ("ck", 128, ("fm",)), ("cv", 128, ("tm",)),
                                   ("g", 3 * D, ("fm",))):
            for m in modes:
                dst = mx[name + "T"] if m == "fm" else mx[name]
                jobs.append((col, ncols, m, dst, AF.Sigmoid if name == "g" else AF.Identity))
            col += ncols
        with contextlib.ExitStack() as ph:
            hT = T(s, ph, "hT", [128, KC, TB], BF16)
            xnT = T(s, ph, "xnT", [128, D], BF16)
            BIG = T(s, ph, "BIG", [128, NT, D], F32, dma=True)
            ss = T(s, ph, "ss", [128, NT], F32)
            rstd = T(s, ph, "rstd", [128, NT], F32)
            stage = [T(s, ph, "stg%d" % i, [128, 512], F32, dma=True) for i in range(4)]
            ps = [T(s, ph, "ps%d" % i, [128, 512], F32, psum=True) for i in range(8)]
            ws = K.WStream(self, ph)
            pctr = 0
            sctr = 0
            gctr = 0
            for tb in range(S // TB):
                tok0 = tb * TB
                pctr = self.norm_block(xin, tok0, NT, BIG, xnT, hT, xnT[:], ss, rstd, ps, A, B, pctr)
                if "dbg_AB" in cfg.get("dbg", ()) and tb == 0:
                    dd = self.scratch("dbg_AB", (128, 2 * KC + NT))
                    s.dma(dd[:, 0:KC], A[:], reads=[A.b], dsem=BIG.d)
                    s.dma(dd[:, KC:2 * KC], B[:], reads=[B.b], dsem=BIG.d)
                    s.dma(dd[:, 2 * KC:], rstd[:], reads=[rstd.b], dsem=BIG.d)
                if "dbg_hT" in cfg.get("dbg", ()) and tb == 0:
                    dd = self.scratch("dbg_hT", (128, KC * TB), BF16)
                    s.dma(dd, hT[:].rearrange("p c t -> p (c t)"), reads=[hT.b], dsem=BIG.d)
                for (col0, ncols, mode, dst, func) in jobs:
                    for c0 in range(0, ncols, 512):
                        w = min(512, ncols - c0)
                        ws.plan([w_in[ks * 512:(ks + 1) * 512, col0 + c0: col0 + c0 + w] for ks in range(D // 512)])
                        pb0 = (gctr % 2) * 4
                        gctr += 1
                        nchunk = (w + 127) // 128
                        for ks in range(D // 512):
                            bf, kc, nn = ws.next()
                            if "dbg_w" in cfg.get("dbg", ()) and gctr == 1 and tb == 0:
                                dd = self.scratch("dbg_w", (128, 4 * 512), BF16)
                                s.dma(dd, bf[:].rearrange("p c t -> p (c t)"), reads=[bf.b], dsem=BIG.d)
                            for k4 in range(kc):
                                kk = ks * 4 + k4
                                first = kk == 0
                                if mode == "fm":
                                    for c in range(nchunk):
                                        mc = min(128, w - c * 128)
                                        p = ps[pb0 + c]
                                        s.op("pe", lambda e, p=p, bf=bf, k4=k4, c=c, kk=kk, mc=mc: e.matmul(
                                            p[0:mc, 0:TB], bf[:, k4, c * 128:c * 128 + mc], hT[:, kk, :], start=(kk == 0), stop=(kk == KC - 1)),
                                            reads=[bf.b, hT.b], writes=[p.b] if first else (), accs=() if first else [p.b])
                                else:
                                    for t in range(NT):
                                        p = ps[pb0 + t]
                                        s.op("pe", lambda e, p=p, bf=bf, k4=k4, t=t, kk=kk, w=w: e.matmul(
                                            p[:, 0:w], hT[:, kk, t * 128:(t + 1) * 128], bf[:, k4, 0:w], start=(kk == 0), stop=(kk == KC - 1)),
                                            reads=[bf.b, hT.b], writes=[p.b] if first else (), accs=() if first else [p.b])
                        if mode == "fm":
                            for c in range(nchunk):
                                mc = min(128, w - c * 128)
                                p = ps[pb0 + c]
                                sg = stage[sctr % 4]
                                sctr += 1
                                s.op("act", lambda e, p=p, sg=sg, mc=mc, func=func: e.activation(out=sg[0:mc, 0:TB], in_=p[0:mc, 0:TB], func=func),
                                     reads=[p.b], writes=[sg.b])
                                s.dma(dst[c0 + c * 128: c0 + c * 128 + mc, tok0:tok0 + TB], sg[0:mc, 0:TB], reads=[sg.b], dsem=sg.d)
                        else:
                            for t in range(NT):
                                p = ps[pb0 + t]
                                sg = stage[sctr % 4]
                                sctr += 1
                                s.op("act", lambda e, p=p, sg=sg, w=w: e.copy(sg[:, 0:w], p[:, 0:w]), reads=[p.b], writes=[sg.b])
                                s.dma(dst[tok0 + t * 128: tok0 + (t + 1) * 128, c0:c0 + w], sg[:, 0:w], reads=[sg.b], dsem=sg.d)
            s.flush()

    PITCH = 512
    YSZ = 128 * 512 + 512

    def bcast_rows(self, dst_ap, row_ap, n, ps, npart, reads, dstb, first=True):
        s = self.s
        for c0 in range(0, n, 512):
            w = min(512, n - c0)
            s.op("pe", lambda e, c0=c0, w=w: e.matmul(ps[0:npart, 0:w], self.ones_row[0:1, 0:npart], row_ap[0:1, c0:c0 + w], start=True, stop=True),
                 reads=list(reads) + [self.ones_row.b], writes=[ps.b])
            s.op("act", lambda e, c0=c0, w=w: e.copy(dst_ap[0:npart, c0:c0 + w], ps[0:npart, 0:w]), reads=[ps.b],
                 writes=[dstb] if (first and c0 == 0) else (), accs=() if (first and c0 == 0) else [dstb])

    def setup_phase(self):
        s, st, cfg = self.s, self.stack, self.cfg
        DEPTH = cfg["DEPTH"]
        self.U = T(s, st, "U", [64, 64], F32)
        self.Mgt = T(s, st, "Mgt", [64, 64], F32)
        s.op("pool", lambda e: e.memset(self.U[:], 1.0), writes=[self.U.b])
        s.op("pool", lambda e: e.affine_select(out=self.U[:], in_=self.U[:], pattern=[[1, 64]], compare_op=ALU.is_ge, fill=0.0,
                                               base=0, channel_multiplier=-1), reads=[self.U.b], accs=[self.U.b])
        s.op("pool", lambda e: e.memset(self.Mgt[:], 1.0), writes=[self.Mgt.b])
        s.op("pool", lambda e: e.affine_select(out=self.Mgt[:], in_=self.Mgt[:], pattern=[[-1, 64]], compare_op=ALU.is_gt, fill=0.0,
                                               base=0, channel_multiplier=1), reads=[self.Mgt.b], accs=[self.Mgt.b])
        self.cmask = T(s, st, "cmask", [128, 128], F32)
        s.op("pool", lambda e: e.memset(self.cmask[:], 0.0), writes=[self.cmask.b])
        s.op("pool", lambda e: e.affine_select(out=self.cmask[:], in_=self.cmask[:], pattern=[[-1, 128]], compare_op=ALU.is_ge, fill=-1e30,
                                               base=0, channel_multiplier=1), reads=[self.cmask.b], accs=[self.cmask.b])
        lb_d = self.scratch("lb_d", (DEPTH, 1024))
        Z_d = self.scratch("Z_d", (2, 24, 384))
        self.Yc_d = self.nc.dram_tensor("Yc_d", [16 * K.YSZ], F32)
        self.Yb_d = self.nc.dram_tensor("Yb_d", [8 * K.YSZ], F32)
        with contextlib.ExitStack() as ph:
            lg = T(s, ph, "lg", [1, DEPTH, 1024], F32, dma=True)
            ex = T(s, ph, "ex", [1, DEPTH, 1024], F32)
            lbrow = T(s, ph, "lbrow", [1, DEPTH, 1024], F32, dma=True)
            mx = T(s, ph, "mx", [1, 1024], F32)
            sm = T(s, ph, "sm", [1, 1024], F32)
            cum = T(s, ph, "cum", [1, 1024], F32)
            s.dma(lg[:], self.dram["lb_logits"].rearrange("(o l) d -> o l d", o=1), writes=[lg.b], dsem=lg.d)
            s.op("dve", lambda e: e.tensor_copy(mx[:], lg[:, 0, :]), reads=[lg.b], writes=[mx.b])
            for l in range(1, DEPTH):
                s.op("dve", lambda e, l=l: e.tensor_tensor(out=mx[:], in0=mx[:], in1=lg[:, l, :], op=ALU.max), reads=[mx.b, lg.b], accs=[mx.b])
            for l in range(DEPTH):
                s.op("dve", lambda e, l=l: e.tensor_tensor(out=ex[:, l, :], in0=lg[:, l, :], in1=mx[:], op=ALU.subtract), reads=[lg.b, mx.b],
                     writes=[ex.b] if l == 0 else (), accs=[ex.b] if l else ())
            s.op("act", lambda e: e.activation(out=ex[:], in_=ex[:], func=AF.Exp), reads=[ex.b], accs=[ex.b])
            s.op("dve", lambda e: e.tensor_copy(sm[:], ex[:, 0, :]), reads=[ex.b], writes=[sm.b])
            for l in range(1, DEPTH):
                s.op("dve", lambda e, l=l: e.tensor_tensor(out=sm[:], in0=sm[:], in1=ex[:, l, :], op=ALU.add), reads=[sm.b, ex.b], accs=[sm.b])
            s.op("dve", lambda e: e.reciprocal(sm[:], sm[:]), reads=[sm.b], accs=[sm.b])
            s.op("dve", lambda e: e.memset(lbrow[:, 0, :], 0.0), writes=[lbrow.b])
            s.op("dve", lambda e: e.memset(cum[:], 0.0), writes=[cum.b])
            for l in range(1, DEPTH):
                s.op("dve", lambda e, l=l: e.tensor_tensor(out=ex[:, l, :], in0=ex[:, l, :], in1=sm[:], op=ALU.mult), reads=[ex.b, sm.b], accs=[ex.b])
                s.op("dve", lambda e, l=l: e.tensor_tensor(out=cum[:], in0=cum[:], in1=ex[:, l, :], op=ALU.add), reads=[cum.b, ex.b], accs=[cum.b])
                s.op("dve", lambda e, l=l: e.tensor_copy(lbrow[:, l, :], cum[:]), reads=[cum.b], accs=[lbrow.b])
            s.dma(lb_d.rearrange("(o l) d -> o l d", o=1), lbrow[:], reads=[lbrow.b], dsem=lbrow.d)
            relT = T(s, ph, "relT", [33, 24], F32, dma=True)
            oh = T(s, ph, "oh", [33, 2 * 383], F32, dma=True)
            Zsb = T(s, ph, "Zsb", [24, 2, 384], F32, dma=True)
            psz = [T(s, ph, "psz%d" % i, [128, 512], F32, psum=True) for i in range(2)]
            s.op("pool", lambda e: e.memset(relT[:], -1e30), writes=[relT.b])
            s.dma(relT[0:32, :], self.dram["rel_table"], reads=[relT.b], accs=[relT.b], dsem=relT.d)
            s.dma(oh[:], self.dram["t5_onehot"], writes=[oh.b], dsem=oh.d)
            s.op("pool", lambda e: e.memset(Zsb[:], 0.0), writes=[Zsb.b])
            for j in range(2):
                s.op("pe", lambda e, j=j: e.matmul(psz[j][0:24, 0:383], relT[:], oh[:, j * 383:(j + 1) * 383], start=True, stop=True),
                     reads=[relT.b, oh.b], writes=[psz[j].b])
                s.op("act", lambda e, j=j: e.copy(Zsb[:, j, 0:383], psz[j][0:24, 0:383]), reads=[psz[j].b], accs=[Zsb.b])
            s.dma(Z_d.rearrange("j h i -> h j i"), Zsb[:], reads=[Zsb.b], dsem=Zsb.d)
            s.flush()
        with contextlib.ExitStack() as ph:
            dummy = T(s, ph, "dummy", [1, 8], F32, dma=True)
            for j, (Y, nh, h0) in enumerate(((self.Yc_d, 16, 8), (self.Yb_d, 8, 0))):
                for h in range(nh):
                    src = bass.AP(tensor=Z_d.tensor, offset=(j * 24 + h0 + h) * 384, ap=[[0, 128], [1, 383]])
                    dst = bass.AP(tensor=Y, offset=h * K.YSZ, ap=[[K.PITCH + 1, 128], [1, 383]])
                    s.dma(dst, src, accs=[dummy.b], dsem=dummy.d)
            s.flush()

    def bias_view(self, Y, h):
        return bass.AP(tensor=Y, offset=h * K.YSZ + 127, ap=[[K.PITCH, 128], [1, 256]])

    def swa_phase(self, l):
        cfg, s = self.cfg, self.s
        S = cfg["S"]
        NB = S // 128
        mx_ = self.mx
        with contextlib.ExitStack() as ph:
            ps = [T(s, ph, "ps%d" % i, [128, 512], F32, psum=True) for i in range(8)]
            stg = T(s, ph, "stg", [128, S], F32, dma=True)
            qb = T(s, ph, "qb", [128, S], BF16)
            kdup = [T(s, ph, "kdup%d" % g, [128, S], BF16) for g in range(2)]
            vb = T(s, ph, "vb", [128, NB, 128], BF16)
            vst = T(s, ph, "vst", [128, NB, 128], F32, dma=True)
            biasC = T(s, ph, "biasC", [128, 16, 256], F32, dma=True)
            sinkB = T(s, ph, "sinkB", [128, 16], F32)
            srow = T(s, ph, "srow", [1, 16], F32, dma=True)
            lg = T(s, ph, "lg", [128, 256], F32)
            pe_ = T(s, ph, "pexp", [128, 256], F32)
            pn = T(s, ph, "pn", [128, 256], BF16)
            pT = T(s, ph, "pT", [128, 2, 128], BF16)
            sm = T(s, ph, "sm", [128, 4], F32)
            yc = T(s, ph, "yc", [128, NB, 1024], BF16)
            ycT = T(s, ph, "ycT", [128, 8, S], BF16, dma=True)
            for h in range(16):
                s.dma(biasC[:, h, :], self.bias_view(self.Yc_d, h), writes=[biasC.b] if h == 0 else (), accs=[biasC.b] if h else (), dsem=biasC.d)
            s.dma(srow[:], self.dram["sinks"][l].rearrange("(o h) -> o h", o=1), writes=[srow.b], dsem=srow.d)
            self.bcast_rows(sinkB, srow, 16, ps[0], 128, [srow.b], sinkB.b)
            for g in range(2):
                s.dma(stg[0:64, :], mx_["ckT"][g * 64:(g + 1) * 64, :], writes=[stg.b], dsem=stg.d)
                s.dma(stg[64:128, :], mx_["ckT"][g * 64:(g + 1) * 64, :], accs=[stg.b], dsem=stg.d)
                s.op("dve", lambda e, g=g: e.tensor_copy(kdup[g][:], stg[:]), reads=[stg.b], writes=[kdup[g].b])
            s.dma(vst[:], mx_["cv"].rearrange("(n s) d -> s n d", s=128), writes=[vst.b], dsem=vst.d)
            s.op("dve", lambda e: e.tensor_copy(vb[:], vst[:]), reads=[vst.b], writes=[vb.b])
            pc = 0
            for j in range(8):
                g = j // 4
                s.dma(stg[:], mx_["cqT"][j * 128:(j + 1) * 128, :], writes=[stg.b], dsem=stg.d)
                s.op("dve", lambda e: e.tensor_copy(qb[:], stg[:]), reads=[stg.b], writes=[qb.b])
                for hh in range(2):
                    h = 2 * j + hh
                    hb = hh * 64
                    for nb in range(NB):
                        k0 = max(nb - 1, 0) * 128
                        kw = 256 if nb > 0 else 128
                        b0 = 0 if nb > 0 else 128
                        pL = ps[pc % 8]; pc += 1
                        s.op("pe", lambda e, pL=pL, hb=hb, nb=nb, k0=k0, kw=kw, g=g: e.matmul(
                            pL[:, 0:kw], qb[hb:hb + 64, nb * 128:(nb + 1) * 128], kdup[g][hb:hb + 64, k0:k0 + kw], start=True, stop=True),
                            reads=[qb.b, kdup[g].b], writes=[pL.b])
                        s.op("dve", lambda e, pL=pL, kw=kw, b0=b0, h=h: e.scalar_tensor_tensor(
                            out=lg[:, 0:kw], in0=pL[:, 0:kw], scalar=0.125, in1=biasC[:, h, b0:b0 + kw], op0=ALU.mult, op1=ALU.add),
                            reads=[pL.b, biasC.b], writes=[lg.b])
                        s.op("dve", lambda e, kw=kw: e.reduce_max(out=sm[:, 0:1], in_=lg[:, 0:kw], axis=AX.X), reads=[lg.b], writes=[sm.b])
                        s.op("dve", lambda e, h=h: e.tensor_tensor(out=sm[:, 0:1], in0=sm[:, 0:1], in1=sinkB[:, h:h + 1], op=ALU.max),
                             reads=[sm.b, sinkB.b], accs=[sm.b])
                        s.op("dve", lambda e: e.tensor_scalar(out=sm[:, 1:2], in0=sm[:, 0:1], scalar1=-1.0, scalar2=None, op0=ALU.mult),
                             reads=[sm.b], accs=[sm.b])
                        s.op("act", lambda e, kw=kw: e.activation(out=pe_[:, 0:kw], in_=lg[:, 0:kw], func=AF.Exp, bias=sm[:, 1:2]),
                             reads=[lg.b, sm.b], writes=[pe_.b])
                        s.op("act", lambda e, h=h: e.activation(out=sm[:, 2:3], in_=sinkB[:, h:h + 1], func=AF.Exp, bias=sm[:, 1:2]),
                             reads=[sinkB.b, sm.b], accs=[sm.b])
                        s.op("dve", lambda e, kw=kw: e.reduce_sum(out=sm[:, 3:4], in_=pe_[:, 0:kw], axis=AX.X), reads=[pe_.b], accs=[sm.b])
                        s.op("dve", lambda e: e.tensor_tensor(out=sm[:, 3:4], in0=sm[:, 3:4], in1=sm[:, 2:3], op=ALU.add), reads=[sm.b], accs=[sm.b])
                        s.op("dve", lambda e: e.reciprocal(sm[:, 3:4], sm[:, 3:4]), reads=[sm.b], accs=[sm.b])
                        s.op("dve", lambda e, kw=kw: e.tensor_scalar(out=pn[:, 0:kw], in0=pe_[:, 0:kw], scalar1=sm[:, 3:4], scalar2=None, op0=ALU.mult),
                             reads=[pe_.b, sm.b], writes=[pn.b])
                        nkb = kw // 128
                        pt = ps[pc % 8]; pc += 1
                        ptb = pt.t[:].bitcast(BF16)
                        for kb in range(nkb):
                            s.op("pe", lambda e, kb=kb, ptb=ptb: e.transpose(ptb[:, kb * 128:(kb + 1) * 128], pn[:, kb * 128:(kb + 1) * 128], self.identb[:]),
                                 reads=[pn.b, self.identb.b], writes=[pt.b] if kb == 0 else (), accs=[pt.b] if kb else ())
                        s.op("act", lambda e, ptb=ptb, kw=kw: e.copy(pT[:].rearrange("p a b -> p (a b)")[:, 0:kw], ptb[:, 0:kw]), reads=[pt.b], writes=[pT.b])
                        pO = ps[pc % 8]; pc += 1
                        for kb in range(nkb):
                            blk = k0 // 128 + kb
                            s.op("pe", lambda e, kb=kb, blk=blk, pO=pO, g=g, nkb=nkb: e.matmul(
                                pO[:, 0:64], pT[:, kb, :], vb[:, blk, g * 64:(g + 1) * 64], start=(kb == 0), stop=(kb == nkb - 1)),
                                reads=[pT.b, vb.b], writes=[pO.b] if kb == 0 else (), accs=[pO.b] if kb else ())
                        first = (j == 0 and hh == 0 and nb == 0)
                        s.op("act", lambda e, pO=pO, nb=nb, h=h: e.copy(yc[:, nb, h * 64:(h + 1) * 64], pO[:, 0:64]), reads=[pO.b],
                             writes=[yc.b] if first else (), accs=() if first else [yc.b])
            for nb in range(NB):
                pt = ps[pc % 8]; pc += 1
                ptb = pt.t[:].bitcast(BF16)
                for c in range(8):
                    s.op("pe", lambda e, c=c, nb=nb, ptb=ptb: e.transpose(ptb[:, c * 128:(c + 1) * 128], yc[:, nb, c * 128:(c + 1) * 128], self.identb[:]),
                         reads=[yc.b, self.identb.b], writes=[pt.b] if c == 0 else (), accs=[pt.b] if c else ())
                s.op("act", lambda e, nb=nb, ptb=ptb: e.copy(ycT[:, :, nb * 128:(nb + 1) * 128], ptb.rearrange("p (c t) -> p c t", c=8)), reads=[pt.b],
                     writes=[ycT.b] if nb == 0 else (), accs=[ycT.b] if nb else ())
            s.dma(self.mx["yT"][2].rearrange("(c p) s -> p c s", p=128), ycT[:], reads=[ycT.b], dsem=ycT.d)
            s.flush()

    def dsa_phase(self, l):
        cfg, s = self.cfg, self.s
        S, TOPK = cfg["S"], cfg["TOPK"]
        NB = S // 128
        mx_ = self.mx
        SCALE = 128 ** -0.5
        with contextlib.ExitStack() as ph:
            ps = [T(s, ph, "ps%d" % i, [128, 512], F32, psum=True) for i in range(8)]
            stg = T(s, ph, "stg", [128, S], F32, dma=True)
            lat = T(s, ph, "lat", [128, 512], F32, dma=True)
            latn = T(s, ph, "latn", [128, 512], BF16)
            latT = T(s, ph, "latT", [128, 4, S], BF16)
            sq = T(s, ph, "sq", [128, 512], F32)
            sm = T(s, ph, "sm", [128, 8], F32)
            kvg = T(s, ph, "kvg", [128, 512], F32)
            grow_ = T(s, ph, "kvgrow", [1, 512], F32, dma=True)
            wkv = T(s, ph, "wkv", [128, 4, 256], F32, dma=True)
            wkvb = T(s, ph, "wkvb", [128, 4, 256], BF16)
            kT = T(s, ph, "kT", [128, S], BF16)
            vb = T(s, ph, "vb", [128, NB, 128], BF16)
            s.dma(grow_[:], self.dram["kv_norm"][l].rearrange("(o d) -> o d", o=1), writes=[grow_.b], dsem=grow_.d)
            self.bcast_rows(kvg, grow_, 512, ps[0], 128, [grow_.b], kvg.b)
            s.dma(wkv[:], self.dram["w_kv_up"][l].rearrange("(c p) n -> p c n", p=128), writes=[wkv.b], dsem=wkv.d)
            s.op("dve", lambda e: e.tensor_copy(wkvb[:], wkv[:]), reads=[wkv.b], writes=[wkvb.b])
            pc = 0
            for nb in range(NB):
                s.dma(lat[:], mx_["blat"][nb * 128:(nb + 1) * 128, :], writes=[lat.b], dsem=lat.d)
                s.op("dve", lambda e, nb=nb: e.tensor_tensor(out=sq[:], in0=lat[:], in1=lat[:], op=ALU.mult), reads=[lat.b], writes=[sq.b])
                s.op("dve", lambda e: e.reduce_sum(out=sm[:, 0:1], in_=sq[:], axis=AX.X), reads=[sq.b], writes=[sm.b])
                self.rsqrt_mean(sm[:, 1:2], sm[:, 0:1], sm.b, sm.b, 512)
                s.op("dve", lambda e, nb=nb: e.scalar_tensor_tensor(out=latn[:], in0=lat[:], scalar=sm[:, 1:2], in1=kvg[:], op0=ALU.mult, op1=ALU.mult),
                     reads=[lat.b, sm.b, kvg.b], writes=[latn.b])
                pt = ps[pc % 8]; pc += 1
                ptb = pt.t[:].bitcast(BF16)
                for c in range(4):
                    s.op("pe", lambda e, c=c, ptb=ptb: e.transpose(ptb[:, c * 128:(c + 1) * 128], latn[:, c * 128:(c + 1) * 128], self.identb[:]),
                         reads=[latn.b, self.identb.b], writes=[pt.b] if c == 0 else (), accs=[pt.b] if c else ())
                s.op("act", lambda e, nb=nb, ptb=ptb: e.copy(latT[:, :, nb * 128:(nb + 1) * 128], ptb[:, 0:512].rearrange("p (c t) -> p c t", c=4)),
                     reads=[pt.b], writes=[latT.b] if nb == 0 else (), accs=[latT.b] if nb else ())
            for t0 in range(0, S, 512):
                p = ps[pc % 8]; pc += 1
                for c in range(4):
                    s.op("pe", lambda e, c=c, p=p, t0=t0: e.matmul(p[:], wkvb[:, c, 0:128], latT[:, c, t0:t0 + 512], start=(c == 0), stop=(c == 3)),
                         reads=[wkvb.b, latT.b], writes=[p.b] if c == 0 else (), accs=[p.b] if c else ())
                s.op("act", lambda e, p=p, t0=t0: e.copy(kT[:, t0:t0 + 512], p[:]), reads=[p.b], writes=[kT.b] if t0 == 0 else (), accs=[kT.b] if t0 else ())
            for nb in range(NB):
                p = ps[pc % 8]; pc += 1
                for c in range(4):
                    s.op("pe", lambda e, c=c, p=p, nb=nb: e.matmul(p[:, 0:128], latT[:, c, nb * 128:(nb + 1) * 128], wkvb[:, c, 128:256], start=(c == 0), stop=(c == 3)),
                         reads=[wkvb.b, latT.b], writes=[p.b] if c == 0 else (), accs=[p.b] if c else ())
                s.op("act", lambda e, p=p, nb=nb: e.copy(vb[:, nb, :], p[:, 0:128]), reads=[p.b], writes=[vb.b] if nb == 0 else (), accs=[vb.b] if nb else ())
            ikd = T(s, ph, "ikd", [128, S], BF16)
            iqb = T(s, ph, "iqb", [128, 8, S], BF16)
            qb = T(s, ph, "qb", [128, 8, S], BF16)
            iw = T(s, ph, "iw", [128, NB, 16], F32, dma=True)
            biasB = T(s, ph, "biasB", [128, 8, 256], F32, dma=True)
            cB = T(s, ph, "cB", [128, 24], F32)
            crow = T(s, ph, "crow", [1, 24], F32, dma=True)
            s.dma(stg[0:64, :], mx_["bikT"], writes=[stg.b], dsem=stg.d)
            s.dma(stg[64:128, :], mx_["bikT"], accs=[stg.b], dsem=stg.d)
            s.op("dve", lambda e: e.tensor_copy(ikd[:], stg[:]), reads=[stg.b], writes=[ikd.b])
            for c in range(8):
                s.dma(stg[:], mx_["biqT"][c * 128:(c + 1) * 128, :], writes=[stg.b], dsem=stg.d)
                s.op("dve", lambda e, c=c: e.tensor_copy(iqb[:, c, :], stg[:]), reads=[stg.b], writes=[iqb.b] if c == 0 else (), accs=[iqb.b] if c else ())
            for c in range(8):
                s.dma(stg[:], mx_["bqT"][c * 128:(c + 1) * 128, :], writes=[stg.b], dsem=stg.d)
                s.op("dve", lambda e, c=c: e.tensor_copy(qb[:, c, :], stg[:]), reads=[stg.b], writes=[qb.b] if c == 0 else (), accs=[qb.b] if c else ())
            s.dma(iw[:], mx_["biw"].rearrange("(n t) h -> t n h", t=128), writes=[iw.b], dsem=iw.d)
            for h in range(8):
                s.dma(biasB[:, h, :], self.bias_view(self.Yb_d, h), writes=[biasB.b] if h == 0 else (), accs=[biasB.b] if h else (), dsem=biasB.d)
            s.dma(crow[:], self.dram["rel_table"][31:32, :], writes=[crow.b], dsem=crow.d)
            self.bcast_rows(cB, crow, 24, ps[0], 128, [crow.b], cB.b)
            acc = T(s, ph, "acc", [128, S], F32)
            work = T(s, ph, "work", [128, S], F32)
            rl = T(s, ph, "rl", [128, S], F32)
            mb = work
            m8 = T(s, ph, "m8", [128, 8], F32)
            lg = T(s, ph, "lg", [128, S], F32)
            pn = T(s, ph, "pn", [128, S], BF16)
            pT = T(s, ph, "pT", [128, NB, 128], BF16)
            ybT = T(s, ph, "ybT", [128, 8, S], BF16, dma=True)
            for qt in range(NB):
                kw = (qt + 1) * 128
                q0 = qt * 128
                use_topk = kw > TOPK
                if use_topk:
                    for ih in range(16):
                        c, hb = ih // 2, (ih % 2) * 64
                        pb = (ih % 2) * 4
                        for n0 in range(0, kw, 512):
                            w = min(512, kw - n0)
                            p = ps[pb + n0 // 512]
                            s.op("pe", lambda e, p=p, c=c, hb=hb, n0=n0, w=w, q0=q0: e.matmul(
                                p[:, 0:w], iqb[hb:hb + 64, c, q0:q0 + 128], ikd[hb:hb + 64, n0:n0 + w], start=True, stop=True),
                                reads=[iqb.b, ikd.b], writes=[p.b])
                            s.op("act", lambda e, p=p, n0=n0, w=w: e.activation(out=rl[:, n0:n0 + w], in_=p[:, 0:w], func=AF.Relu),
                                 reads=[p.b], writes=[rl.b] if n0 == 0 else (), accs=[rl.b] if n0 else ())
                        if ih == 0:
                            s.op("dve", lambda e, kw=kw, qt=qt: e.tensor_scalar(out=acc[:, 0:kw], in0=rl[:, 0:kw], scalar1=iw[:, qt, 0:1], scalar2=None, op0=ALU.mult),
                                 reads=[rl.b, iw.b], writes=[acc.b])
                        else:
                            s.op("dve", lambda e, kw=kw, qt=qt, ih=ih: e.scalar_tensor_tensor(
                                out=acc[:, 0:kw], in0=rl[:, 0:kw], scalar=iw[:, qt, ih:ih + 1], in1=acc[:, 0:kw], op0=ALU.mult, op1=ALU.add),
                                reads=[rl.b, iw.b, acc.b], accs=[acc.b])
                    s.op("dve", lambda e, q0=q0: e.tensor_tensor(out=acc[:, q0:q0 + 128], in0=acc[:, q0:q0 + 128], in1=self.cmask[:], op=ALU.add),
                         reads=[acc.b, self.cmask.b], accs=[acc.b])
                    s.op("dve", lambda e, kw=kw: e.tensor_copy(work[:, 0:kw], acc[:, 0:kw]), reads=[acc.b], writes=[work.b])
                    for r in range(TOPK // 8):
                        s.op("dve", lambda e, kw=kw: e.max(out=m8[:], in_=work[:, 0:kw]), reads=[work.b], writes=[m8.b])
                        if r < TOPK // 8 - 1:
                            s.op("dve", lambda e, kw=kw: e.match_replace(out=work[:, 0:kw], in_to_replace=m8[:], in_values=work[:, 0:kw], imm_value=-3e38),
                                 reads=[work.b, m8.b], accs=[work.b])
                    s.op("dve", lambda e, kw=kw: e.tensor_scalar(out=mb[:, 0:kw], in0=acc[:, 0:kw], scalar1=m8[:, 7:8], scalar2=-1e30,
                                                            op0=ALU.is_lt, op1=ALU.mult), reads=[acc.b, m8.b], writes=[mb.b])
                for h in range(8):
                    pb = (h % 2) * 4
                    for n0 in range(0, kw, 512):
                        w = min(512, kw - n0)
                        p = ps[pb + n0 // 512]
                        s.op("pe", lambda e, p=p, h=h, n0=n0, w=w, q0=q0: e.matmul(p[:, 0:w], qb[:, h, q0:q0 + 128], kT[:, n0:n0 + w], start=True, stop=True),
                             reads=[qb.b, kT.b], writes=[p.b])
                    w0 = max(kw - 256, 0)
                    first = True
                    for n0 in range(0, w0, 512):
                        w = min(512, w0 - n0)
                        p = ps[pb + n0 // 512]
                        s.op("dve", lambda e, p=p, n0=n0, w=w, h=h: e.tensor_scalar(out=lg[:, n0:n0 + w], in0=p[:, 0:w], scalar1=SCALE, scalar2=cB[:, h:h + 1],
                                                                               op0=ALU.mult, op1=ALU.add), reads=[p.b, cB.b],
                             writes=[lg.b] if first else (), accs=() if first else [lg.b])
                        first = False
                    ww = kw - w0
                    b0 = 256 - ww
                    for n0 in range(w0, kw, 128):
                        p = ps[pb + n0 // 512]
                        o = n0 % 512
                        bo = b0 + (n0 - w0)
                        s.op("dve", lambda e, p=p, n0=n0, o=o, bo=bo, h=h: e.scalar_tensor_tensor(
                            out=lg[:, n0:n0 + 128], in0=p[:, o:o + 128], scalar=SCALE, in1=biasB[:, h, bo:bo + 128], op0=ALU.mult, op1=ALU.add),
                            reads=[p.b, biasB.b], writes=[lg.b] if first else (), accs=() if first else [lg.b])
                        first = False
                    if use_topk:
                        s.op("dve", lambda e, kw=kw: e.tensor_tensor(out=lg[:, 0:kw], in0=lg[:, 0:kw], in1=mb[:, 0:kw], op=ALU.add), reads=[lg.b, mb.b], accs=[lg.b])
                    s.op("dve", lambda e, kw=kw: e.reduce_max(out=sm[:, 2:3], in_=lg[:, 0:kw], axis=AX.X), reads=[lg.b], writes=[sm.b])
                    s.op("dve", lambda e: e.tensor_scalar(out=sm[:, 3:4], in0=sm[:, 2:3], scalar1=-1.0, scalar2=None, op0=ALU.mult), reads=[sm.b], accs=[sm.b])
                    s.op("act", lambda e, kw=kw: e.activation(out=lg[:, 0:kw], in_=lg[:, 0:kw], func=AF.Exp, bias=sm[:, 3:4]), reads=[lg.b, sm.b], accs=[lg.b])
                    s.op("dve", lambda e, kw=kw: e.reduce_sum(out=sm[:, 4:5], in_=lg[:, 0:kw], axis=AX.X), reads=[lg.b], accs=[sm.b])
                    s.op("dve", lambda e: e.reciprocal(sm[:, 4:5], sm[:, 4:5]), reads=[sm.b], accs=[sm.b])
                    s.op("dve", lambda e, kw=kw: e.tensor_scalar(out=pn[:, 0:kw], in0=lg[:, 0:kw], scalar1=sm[:, 4:5], scalar2=None, op0=ALU.mult),
                         reads=[lg.b, sm.b], writes=[pn.b])
                    nkb = kw // 128
                    for k8 in range(0, nkb, 8):
                        pt = ps[pc % 8]; pc += 1
                        ptb = pt.t[:].bitcast(BF16)
                        nn = min(8, nkb - k8)
                        for kb in range(nn):
                            s.op("pe", lambda e, kb=kb, k8=k8, ptb=ptb: e.transpose(ptb[:, kb * 128:(kb + 1) * 128], pn[:, (k8 + kb) * 128:(k8 + kb + 1) * 128], self.identb[:]),
                                 reads=[pn.b, self.identb.b], writes=[pt.b] if kb == 0 else (), accs=[pt.b] if kb else ())
                        s.op("act", lambda e, ptb=ptb, k8=k8, nn=nn: e.copy(pT[:, k8:k8 + nn, :], ptb[:, 0:nn * 128].rearrange("p (a b) -> p a b", b=128)),
                             reads=[pt.b], writes=[pT.b] if k8 == 0 else (), accs=[pT.b] if k8 else ())
                    pO = ps[pc % 8]; pc += 1
                    for kb in range(nkb):
                        s.op("pe", lambda e, kb=kb, pO=pO, nkb=nkb: e.matmul(pO[:, 0:128], vb[:, kb, :], pT[:, kb, :], start=(kb == 0), stop=(kb == nkb - 1)),
                             reads=[pT.b, vb.b], writes=[pO.b] if kb == 0 else (), accs=[pO.b] if kb else ())
                    first = (qt == 0 and h == 0)
                    s.op("act", lambda e, pO=pO, h=h, q0=q0: e.copy(ybT[:, h, q0:q0 + 128], pO[:, 0:128]), reads=[pO.b],
                         writes=[ybT.b] if first else (), accs=() if first else [ybT.b])
            s.dma(self.mx["yT"][1].rearrange("(c p) s -> p c s", p=128), ybT[:], reads=[ybT.b], dsem=ybT.d)
            s.flush()

    def hgrn_phase(self, l):
        cfg, s = self.cfg, self.s
        S = cfg["S"]
        CS = 32
        SEG = cfg.get("HSEG", min(512, S))
        NCHT = S // CS
        NCH = SEG // CS
        mx_ = self.mx
        lb_d = self.dram["lb_d"]
        with contextlib.ExitStack() as ph:
            ps = [T(s, ph, "ps%d" % i, [128, 512], F32, psum=True) for i in range(8)]
            lbc = T(s, ph, "lbc", [128, 8], F32)
            omc = T(s, ph, "omc", [128, 8], F32)
            lbB = T(s, ph, "lbB", [CS, 1024], F32)
            omB = T(s, ph, "omB", [CS, 1024], F32)
            hgB = T(s, ph, "hgB", [CS, 128], F32)
            row = T(s, ph, "row", [1, 1024], F32, dma=True)
            row2 = T(s, ph, "row2", [1, 128], F32, dma=True)
            self.colload(ph, None, lb_d[l], 8, ps[0], "lb")
            s.op("dve", lambda e: e.tensor_copy(lbc[:], ps[0][:, 0:8]), reads=[ps[0].b], writes=[lbc.b])
            s.op("dve", lambda e: e.tensor_scalar(out=omc[:], in0=lbc[:], scalar1=-1.0, scalar2=1.0, op0=ALU.mult, op1=ALU.add), reads=[lbc.b], writes=[omc.b])
            s.dma(row[:], lb_d[l:l + 1, :], writes=[row.b], dsem=row.d)
            self.bcast_rows(lbB, row, 1024, ps[1], CS, [row.b], lbB.b)
            s.op("dve", lambda e: e.tensor_scalar(out=omB[:], in0=lbB[:], scalar1=-1.0, scalar2=1.0, op0=ALU.mult, op1=ALU.add), reads=[lbB.b], writes=[omB.b])
            s.dma(row2[:], self.dram["hgrn_norm"][l].rearrange("(o d) -> o d", o=1), writes=[row2.b], dsem=row2.d)
            self.bcast_rows(hgB, row2, 128, ps[1], CS, [row2.b], hgB.b)
            qT = T(s, ph, "qT", [128, NCH, CS], F32, dma=True)
            kTf = T(s, ph, "kTf", [128, NCH, CS], F32, dma=True)
            bT = T(s, ph, "bT", [128, NCH, CS], F32)
            d1 = T(s, ph, "d1", [128, NCH, CS], F32)
            bmid = T(s, ph, "bmid", [128, NCH], F32)
            bend = T(s, ph, "bend", [128, NCH], F32)
            qtil = T(s, ph, "qtil", [128, NCH, CS], BF16)
            ktil = T(s, ph, "ktil", [128, NCH, CS], BF16)
            qb = T(s, ph, "qbb", [128, NCH, CS], BF16)
            f_tm = T(s, ph, "f_tm", [CS, NCH, 128], F32, dma=True)
            lf_tm = T(s, ph, "lf_tm", [CS, NCH, 128], F32)
            k_tm = T(s, ph, "k_tm", [CS, NCH, 128], F32)
            i_tm = T(s, ph, "i_tm", [CS, NCH, 128], F32, dma=True)
            g_tm = T(s, ph, "g_tm", [CS, NCH, 128], F32, dma=True)
            kend = T(s, ph, "kend", [CS, NCH, 128], BF16)
            vb = T(s, ph, "vb", [CS, NCH, 128], BF16)
            o_sb = T(s, ph, "o_sb", [CS, NCH, 128], F32)
            ssq = T(s, ph, "ssq", [CS, NCH], F32)
            att = T(s, ph, "att", [CS, CS], BF16)
            state = T(s, ph, "state", [128, 128], F32)
            stb = T(s, ph, "stb", [128, 128], BF16)
            yaT = T(s, ph, "yaT", [128, SEG], BF16, dma=True)
            pc = 2
            for h, sg in [(h_, g_) for h_ in range(8) for g_ in range(S // SEG)]:
                hs = slice(h * 128, (h + 1) * 128)
                ts = slice(sg * SEG, (sg + 1) * SEG)
                cs_ = slice(sg * NCH, (sg + 1) * NCH)
                bc = lambda t, hs=hs: t[:, hs].unsqueeze(1).to_broadcast([CS, NCH, 128])
                s.dma(qT[:].rearrange("p n t -> p (n t)"), mx_["aqT"][hs, ts], writes=[qT.b], dsem=qT.d)
                s.dma(kTf[:].rearrange("p n t -> p (n t)"), mx_["afT"][hs, ts], writes=[kTf.b], dsem=kTf.d)
                s.dma(f_tm[:], mx_["af"].rearrange("(n t) d -> t n d", t=CS)[:, cs_, hs], writes=[f_tm.b], dsem=f_tm.d)
                s.dma(i_tm[:], mx_["ai"].rearrange("(n t) d -> t n d", t=CS)[:, cs_, hs], writes=[i_tm.b], dsem=i_tm.d)
                s.dma(g_tm[:], mx_["ag"].rearrange("(n t) d -> t n d", t=CS)[:, cs_, hs], writes=[g_tm.b], dsem=g_tm.d)
                s.op("act", lambda e: e.activation(out=kTf[:], in_=kTf[:], func=AF.Sigmoid, scale=-1.0), reads=[kTf.b], accs=[kTf.b])
                s.op("dve", lambda e, h=h: e.tensor_scalar(out=kTf[:], in0=kTf[:], scalar1=omc[:, h:h + 1], scalar2=None, op0=ALU.mult), reads=[kTf.b, omc.b], accs=[kTf.b])
                s.op("act", lambda e: e.activation(out=k_tm[:], in_=f_tm[:], func=AF.Sigmoid, scale=-1.0), reads=[f_tm.b], writes=[k_tm.b])
                s.op("dve", lambda e, bc=bc: e.tensor_tensor(out=k_tm[:], in0=k_tm[:], in1=bc(omB), op=ALU.mult), reads=[k_tm.b, omB.b], accs=[k_tm.b])
                s.op("act", lambda e: e.activation(out=lf_tm[:], in_=f_tm[:], func=AF.Sigmoid), reads=[f_tm.b], writes=[lf_tm.b])
                s.op("dve", lambda e, bc=bc: e.tensor_tensor(out=lf_tm[:], in0=lf_tm[:], in1=bc(omB), op=ALU.mult), reads=[lf_tm.b, omB.b], accs=[lf_tm.b])
                s.op("dve", lambda e, bc=bc: e.tensor_tensor(out=lf_tm[:], in0=lf_tm[:], in1=bc(lbB), op=ALU.add), reads=[lf_tm.b, lbB.b], accs=[lf_tm.b])
                s.op("dve", lambda e: e.tensor_scalar(out=lf_tm[:], in0=lf_tm[:], scalar1=1e-20, scalar2=None, op0=ALU.max), reads=[lf_tm.b], accs=[lf_tm.b])
                s.op("act", lambda e: e.activation(out=lf_tm[:], in_=lf_tm[:], func=AF.Ln), reads=[lf_tm.b], accs=[lf_tm.b])
                s.op("act", lambda e: e.activation(out=vb[:], in_=i_tm[:], func=AF.Silu), reads=[i_tm.b], writes=[vb.b])
                s.op("act", lambda e: e.activation(out=g_tm[:], in_=g_tm[:], func=AF.Silu), reads=[g_tm.b], accs=[g_tm.b])
                NPB = 512 // CS
                for n8 in range(0, NCH, NPB):
                    p = ps[pc % 8]; pc += 1
                    for n in range(n8, min(n8 + NPB, NCH)):
                        s.op("pe", lambda e, p=p, n=n, n8=n8: e.matmul(p[:, (n - n8) * CS:(n - n8 + 1) * CS], lf_tm[:, n, :], self.U[0:CS, 0:CS], start=True, stop=True),
                             reads=[lf_tm.b, self.U.b], writes=[p.b] if n == n8 else (), accs=[p.b] if n != n8 else ())
                    nn = min(NPB, NCH - n8)
                    s.op("act", lambda e, p=p, n8=n8, nn=nn: e.copy(bT[:, n8:n8 + nn, :], p[:, 0:nn * CS].rearrange("p (a b) -> p a b", b=CS)),
                         reads=[p.b], writes=[bT.b] if n8 == 0 else (), accs=[bT.b] if n8 else ())
                for n4 in range(0, NCH, 4):
                    p = ps[pc % 8]; pc += 1
                    for n in range(n4, min(n4 + 4, NCH)):
                        s.op("pe", lambda e, p=p, n=n, n4=n4: e.matmul(p[0:CS, (n - n4) * 128:(n - n4 + 1) * 128], self.Mgt[0:CS, 0:CS], lf_tm[:, n, :], start=True, stop=True),
                             reads=[lf_tm.b, self.Mgt.b], writes=[p.b] if n == n4 else (), accs=[p.b] if n != n4 else ())
                    nn = min(4, NCH - n4)
                    s.op("act", lambda e, p=p, n4=n4, nn=nn: e.activation(out=o_sb[:, n4:n4 + nn, :], in_=p[0:CS, 0:nn * 128].rearrange("p (a b) -> p a b", b=128), func=AF.Exp),
                         reads=[p.b], writes=[o_sb.b] if n4 == 0 else (), accs=[o_sb.b] if n4 else ())
                s.op("dve", lambda e: e.tensor_tensor(out=kend[:], in0=k_tm[:], in1=o_sb[:], op=ALU.mult), reads=[k_tm.b, o_sb.b], writes=[kend.b])
                s.op("dve", lambda e: e.tensor_copy(bmid[:], bT[:, :, CS // 2 - 1]), reads=[bT.b], writes=[bmid.b])
                s.op("dve", lambda e: e.tensor_tensor(out=d1[:], in0=bT[:], in1=bmid[:].unsqueeze(2).to_broadcast([128, NCH, CS]), op=ALU.subtract),
                     reads=[bT.b, bmid.b], writes=[d1.b])
                s.op("act", lambda e: e.activation(out=bT[:], in_=bT[:], func=AF.Exp), reads=[bT.b], accs=[bT.b])
                s.op("dve", lambda e: e.tensor_copy(bend[:], bT[:, :, CS - 1]), reads=[bT.b], writes=[bend.b])
                s.op("dve", lambda e: e.tensor_tensor(out=qb[:], in0=qT[:], in1=bT[:], op=ALU.mult), reads=[qT.b, bT.b], writes=[qb.b])
                s.op("act", lambda e: e.activation(out=bT[:], in_=d1[:], func=AF.Exp), reads=[d1.b], writes=[bT.b])
                s.op("dve", lambda e: e.tensor_tensor(out=qtil[:], in0=qT[:], in1=bT[:], op=ALU.mult), reads=[qT.b, bT.b], writes=[qtil.b])
                s.op("act", lambda e: e.activation(out=bT[:], in_=d1[:], func=AF.Exp, scale=-1.0), reads=[d1.b], writes=[bT.b])
                s.op("dve", lambda e: e.tensor_tensor(out=ktil[:], in0=kTf[:], in1=bT[:], op=ALU.mult), reads=[kTf.b, bT.b], writes=[ktil.b])
                for n in range(NCH):
                    gn = sg * NCH + n
                    pA = ps[pc % 8]; pc += 1
                    s.op("pe", lambda e, pA=pA, n=n: e.matmul(pA[0:CS, 0:CS], ktil[:, n, :], qtil[:, n, :], start=True, stop=True),
                         reads=[ktil.b, qtil.b], writes=[pA.b])
                    s.op("dve", lambda e, pA=pA: e.tensor_tensor(out=att[:], in0=pA[0:CS, 0:CS], in1=self.U[0:CS, 0:CS], op=ALU.mult), reads=[pA.b, self.U.b], writes=[att.b])
                    pO = ps[pc % 8]; pc += 1
                    s.op("pe", lambda e, pO=pO, n=n, gn=gn: e.matmul(pO[0:CS, 0:128], att[:], vb[:, n, :], start=True, stop=(gn == 0)),
                         reads=[att.b, vb.b], writes=[pO.b])
                    if gn > 0:
                        s.op("pe", lambda e, pO=pO, n=n: e.matmul(pO[0:CS, 0:128], qb[:, n, :], stb[:], start=False, stop=True),
                             reads=[qb.b, stb.b], accs=[pO.b])
                    s.op("act", lambda e, pO=pO, n=n: e.copy(o_sb[:, n, :], pO[0:CS, 0:128]), reads=[pO.b], writes=[o_sb.b] if n == 0 else (), accs=[o_sb.b] if n else ())
                    if gn < NCHT - 1:
                        pS = ps[pc % 8]; pc += 1
                        s.op("pe", lambda e, pS=pS, n=n: e.matmul(pS[:, 0:128], kend[:, n, :], vb[:, n, :], start=True, stop=True),
                             reads=[kend.b, vb.b], writes=[pS.b])
                        if gn == 0:
                            s.op("dve", lambda e, pS=pS: e.tensor_copy(state[:], pS[:, 0:128]), reads=[pS.b], writes=[state.b])
                        else:
                            s.op("dve", lambda e, pS=pS, n=n: e.scalar_tensor_tensor(out=state[:], in0=state[:], scalar=bend[:, n:n + 1], in1=pS[:, 0:128],
                                                                                 op0=ALU.mult, op1=ALU.add), reads=[state.b, bend.b, pS.b], accs=[state.b])
                        s.op("act", lambda e: e.copy(stb[:], state[:]), reads=[state.b], writes=[stb.b])
                s.op("dve", lambda e: e.tensor_tensor(out=lf_tm[:], in0=o_sb[:], in1=o_sb[:], op=ALU.mult), reads=[o_sb.b], writes=[lf_tm.b])
                s.op("dve", lambda e: e.tensor_reduce(out=ssq[:], in_=lf_tm[:], axis=AX.X, op=ALU.add), reads=[lf_tm.b], writes=[ssq.b])
                self.rsqrt_mean(ssq[:], ssq[:], ssq.b, ssq.b, 128, npart=CS)
                s.op("dve", lambda e: e.tensor_tensor(out=o_sb[:], in0=o_sb[:], in1=ssq[:].unsqueeze(2).to_broadcast([CS, NCH, 128]), op=ALU.mult),
                     reads=[o_sb.b, ssq.b], accs=[o_sb.b])
                s.op("dve", lambda e: e.tensor_tensor(out=o_sb[:], in0=o_sb[:], in1=hgB[:].unsqueeze(1).to_broadcast([CS, NCH, 128]), op=ALU.mult),
                     reads=[o_sb.b, hgB.b], accs=[o_sb.b])
                s.op("dve", lambda e: e.tensor_tensor(out=o_sb[:], in0=o_sb[:], in1=g_tm[:], op=ALU.mult), reads=[o_sb.b, g_tm.b], accs=[o_sb.b])
                for n8 in range(0, NCH, NPB):
                    p = ps[pc % 8]; pc += 1
                    nn = min(NPB, NCH - n8)
                    for n in range(n8, n8 + nn):
                        s.op("pe", lambda e, p=p, n=n, n8=n8: e.transpose(p[:, (n - n8) * CS:(n - n8 + 1) * CS], o_sb[:, n, :], self.identf[0:CS, 0:CS]),
                             reads=[o_sb.b, self.identf.b], writes=[p.b] if n == n8 else (), accs=[p.b] if n != n8 else ())
                    s.op("act", lambda e, p=p, n8=n8, nn=nn: e.copy(yaT[:, n8 * CS:(n8 + nn) * CS], p[:, 0:nn * CS]), reads=[p.b],
                         writes=[yaT.b] if n8 == 0 else (), accs=[yaT.b] if n8 else ())
                s.dma(self.mx["yT"][0][hs, ts], yaT[:], reads=[yaT.b], dsem=yaT.d)
            s.flush()


def build_program(cfg):
    k = K(cfg)
    D, S, DEPTH, DFF, NBE = cfg["D"], cfg["S"], cfg["DEPTH"], cfg["DFF"], cfg["NBE"]
    DIN = 8016 + 3 * D
    for name, shape in (("x", (NBE, S, D)), ("c", (NBE, D)), ("w_c_down", (D, 256)), ("w_c_up", (DEPTH, 256, 9 * D)),
                        ("norm_gains", (DEPTH, 6, D)), ("w_in", (DEPTH, D, DIN)), ("lb_logits", (DEPTH, 1024)),
                        ("hgrn_norm", (DEPTH, 128)), ("kv_norm", (DEPTH, 512)), ("w_kv_up", (DEPTH, 512, 256)),
                        ("rel_table", (32, 24)), ("sinks", (DEPTH, 16)), ("w_branch", (DEPTH, 3, 1024, D)),
                        ("w_out", (DEPTH, D, D)), ("ffn1_in", (DEPTH, D, 2 * DFF)), ("ffn1_out", (DEPTH, DFF, D)),
                        ("ffn2_in", (DEPTH, D, 2 * DFF)), ("ffn2_out", (DEPTH, DFF, D)), ("t5_onehot", (33, 766))):
        k.ext(name, shape)
    out = k.ext("out", (NBE, S, D), kind="ExternalOutput")
    xres = k.scratch("xres", (S, D))
    growd = k.scratch("grow_d", (3, 1, D))
    k.mixer_scratch()
    k.consts()
    k.mod_setup()
    k.setup_phase()
    for b in range(NBE):
        k.cond_phase(k.dram["c"][b])
        for l in range(DEPTH):
            k.mod_phase(l, growd)
            k.ffn_phase(l, 1, k.dram["x"][b] if l == 0 else xres, xres, k.AB[0], k.AB[1], growd[0])
            k.proj_phase(l, xres, k.AB[2], k.AB[3])
            k.hgrn_phase(l)
            k.dsa_phase(l)
            k.swa_phase(l)
            k.merge_phase(l, xres, xres, growd[1])
            k.ffn_phase(l, 2, xres, out[b] if l == DEPTH - 1 else xres, k.AB[4], k.AB[5], growd[2])
    return k


def t5_bucket_np(d):
    d = np.maximum(d, 0)
    df = np.maximum(d, 1).astype(np.float32)
    large = 16 + (np.log(df / np.float32(16)) / np.float32(math.log(128 / 16)) * np.float32(16)).astype(np.int32)
    large = np.minimum(large, 31)
    return np.where(d < 16, d, large)
def t5_onehot():
    oh = np.zeros((33, 2, 383), np.float32)
    for i in range(383):
        dist = 255 - i
        b = int(t5_bucket_np(np.array([dist]))[0])
        if 0 <= dist < 128:
            oh[b, 0, i] = 1
        else:
            oh[32, 0, i] = 1
        if dist >= 0:
            oh[b, 1, i] = 1
        else:
            oh[32, 1, i] = 1
    return oh.reshape(33, 766)


N_CORES = 4


def run_cores(inputs, n_cores, batch_ids):
    cfg = dict(FULL)
    cfg["NBE"] = len(batch_ids[0])
    Sched.SERIAL = True
    prog = build_program(cfg)
    oh = t5_onehot()
    shared = {kk: np.ascontiguousarray(v, dtype=np.float32) for kk, v in inputs.items() if kk not in ("x", "c")}
    in_maps = []
    for ids in batch_ids:
        m = dict(shared)
        m["x"] = np.ascontiguousarray(inputs["x"][ids], dtype=np.float32)
        m["c"] = np.ascontiguousarray(inputs["c"][ids], dtype=np.float32)
        m["t5_onehot"] = oh
        in_maps.append(m)
    res = run_bass_kernel_spmd(prog.nc, in_maps, core_ids=list(range(n_cores)))
    return [np.asarray(r["out"]) for r in res.results]


def kernel(**inputs):
    B = inputs["x"].shape[0]
    per = B // N_CORES
    batch_ids = [list(range(c * per, (c + 1) * per)) for c in range(N_CORES)]
    outs = run_cores(inputs, N_CORES, batch_ids)
    return np.concatenate(outs, axis=0).astype(np.float32)
```

```python
import math
import contextlib
import numpy as np
import concourse.bass as bass
import concourse.mybir as mybir
from concourse.bass_utils import run_bass_kernel_spmd

DT = mybir.dt
F32, BF16 = DT.float32, DT.bfloat16
ALU = mybir.AluOpType
AF = mybir.ActivationFunctionType
AX = mybir.AxisListType

FULL = dict(D=4096, B=8, S=2048, DEPTH=4, DFF=4096, TOPK=256, NCORES=8)


class DSem:
    def __init__(self, sem):
        self.sem = sem
        self.count = 0


class Buf:
    def __init__(self, name):
        self.name = name
        self.writes = {}
        self.reads = {}


ENGS = ("pe", "act", "dve", "pool", "sp")


class Sched:
    SERIAL = False

    def __init__(self, nc, stack):
        self.nc = nc
        self.stack = stack
        self.h = dict(pe=nc.tensor, act=nc.scalar, dve=nc.vector, pool=nc.gpsimd, sp=nc.sync)
        self.q = {e: [] for e in ENGS}
        self.sem = {e: stack.enter_context(nc.semaphore("c_" + e)) for e in ENGS if e != "sp"}
        self.cnt = {e: 0 for e in ENGS}
        self.waited = {e: {} for e in ENGS}
        self.dsems = []
        self.dnext = 0
        self.ninst = 0

    def get_dsem(self):
        if self.dnext == len(self.dsems):
            self.dsems.append(DSem(self.stack.enter_context(self.nc.semaphore("d%d" % self.dnext))))
        self.dnext += 1
        return self.dsems[self.dnext - 1]

    def _wait(self, eng, key, val):
        if isinstance(key, DSem):
            val = key.count
            sem = key.sem
        else:
            if key == eng and eng in ("pe", "sp"):
                return
            sem = self.sem[key]
        w = self.waited[eng]
        if w.get(key, 0) >= val:
            return
        w[key] = val
        h = self.h[eng]
        self.q[eng].append(lambda e, sem=sem, val=val: e.wait_ge(sem, val))

    def op(self, eng, fn, reads=(), writes=(), accs=(), dsem=None):
        deps = []
        for b in reads:
            deps.extend(b.writes.items())
        for b in writes:
            deps.extend(b.writes.items())
            deps.extend(b.reads.items())
        for b in accs:
            deps.extend(b.reads.items())
        is_dma = dsem is not None
        if Sched.SERIAL is True or (Sched.SERIAL and eng in Sched.SERIAL):
            for k2 in ENGS:
                if k2 != "sp" and self.cnt[k2] > 0:
                    self._wait(eng, k2, self.cnt[k2])
            for d in self.dsems:
                if d.count:
                    self._wait(eng, d, d.count)
        for key, val in deps:
            if not is_dma and key == eng:
                if not any(key in b.writes for b in reads):
                    continue
            self._wait(eng, key, val)
        h = self.h[eng]
        if is_dma:
            dsem.count += 16
            tok = (dsem, dsem.count)
            sem, inc = dsem.sem, 16
        else:
            self.cnt[eng] += 1
            tok = (eng, self.cnt[eng])
            sem, inc = self.sem[eng], 1
        self.q[eng].append(lambda e, fn=fn, sem=sem, inc=inc: fn(e).then_inc(sem, inc))
        self.ninst += 1
        for b in reads:
            b.reads[tok[0]] = tok[1]
        for b in writes:
            b.writes = {tok[0]: tok[1]}
            b.reads = {}
        for b in accs:
            b.writes[tok[0]] = tok[1]
            b.reads = {}

    def dma(self, out, in_, reads=(), writes=(), accs=(), dsem=None, eng="sp", **kw):
        self.op(eng, lambda h: h.dma_start(out=out, in_=in_, **kw), reads=reads, writes=writes,
                accs=accs, dsem=dsem)

    def barrier(self):
        for e in ENGS:
            for k in ENGS:
                if k != "sp" and k != e and self.cnt[k] > 0:
                    self._wait(e, k, self.cnt[k])
            for d in self.dsems:
                if d.count:
                    self._wait(e, d, d.count)

    def flush(self):
        self.barrier()
        with self.nc.Block() as block:
            self._flush(block)
        self.q = {e: [] for e in ENGS}
        self.dnext = 0

    def _flush(self, block):
        q = self.q

        @block.tensor
        def _(e):
            for f in q["pe"]:
                f(e)

        @block.scalar
        def _(e):
            for f in q["act"]:
                f(e)

        @block.vector
        def _(e):
            for f in q["dve"]:
                f(e)

        @block.gpsimd
        def _(e):
            for f in q["pool"]:
                f(e)

        @block.sync
        def _(e):
            for f in q["sp"]:
                f(e)


class T:
    uid = 0

    def __init__(self, sched, stack, name, shape, dtype, psum=False, dma=False):
        nc = sched.nc
        alloc = nc.psum_tensor if psum else nc.sbuf_tensor
        T.uid += 1
        name = "%s_%d" % (name, T.uid)
        self.t = stack.enter_context(alloc(name, shape, dtype))
        self.b = Buf(name)
        self.d = sched.get_dsem() if dma else None

    def __getitem__(self, idx):
        return self.t[idx]


class K:
    def __init__(self, cfg):
        self.cfg = cfg
        self.nc = bass.Bass("TRN2", target_bir_lowering=False)
        self.stack = contextlib.ExitStack()
        self.s = Sched(self.nc, self.stack)
        self.dram = {}

    def ext(self, name, shape, kind="ExternalInput"):
        self.dram[name] = self.nc.dram_tensor(name, list(shape), F32, kind=kind).ap()
        return self.dram[name]

    def scratch(self, name, shape, dtype=F32):
        if name in self.cfg.get("dbg", ()):
            ap = self.nc.dram_tensor(name, list(shape), dtype, kind="ExternalOutput").ap()
        elif name in self.cfg.get("ext_in", ()):
            ap = self.nc.dram_tensor(name, list(shape), dtype, kind="ExternalInput").ap()
        else:
            ap = self.nc.dram_tensor(name, list(shape), dtype).ap()
        self.dram[name] = ap
        return ap

    def consts(self):
        s, st = self.s, self.stack
        self.identb = T(s, st, "identb", [128, 128], BF16)
        self.identf = T(s, st, "identf", [128, 128], F32)
        self.ones_row = T(s, st, "ones_row", [1, 128], F32)
        s.op("pool", lambda e: e.memset(self.identf[:], 0.0), writes=[self.identf.b])
        s.op("pool", lambda e: e.affine_select(out=self.identf[:], in_=self.identf[:], pattern=[[-1, 128]],
                                               compare_op=ALU.not_equal, fill=1.0, base=0, channel_multiplier=1),
             reads=[self.identf.b], accs=[self.identf.b])
        s.op("dve", lambda e: e.tensor_copy(self.identb[:], self.identf[:]), reads=[self.identf.b], writes=[self.identb.b])
        s.op("pool", lambda e: e.memset(self.ones_row[:], 1.0), writes=[self.ones_row.b])
        self.eps = T(s, st, "eps", [128, 1], F32)
        s.op("pool", lambda e: e.memset(self.eps[:], 1e-6), writes=[self.eps.b])
        s.flush()

    def scramble(self):
        s = self.s
        with contextlib.ExitStack() as ph:
            big = T(s, ph, "scr", [128, 40000], F32)
            ps = [T(s, ph, "scrp%d" % i, [128, 512], F32, psum=True) for i in range(8)]
            s.op("pool", lambda e: e.memset(big[:, 0:20000], 12345.0), writes=[big.b])
            s.op("dve", lambda e: e.memset(big[:, 20000:40000], -54321.0), accs=[big.b])
            for p in ps:
                s.op("dve", lambda e, p=p: e.memset(p[:], 777.0), writes=[p.b])
            s.flush()

    class WStream:
        def __init__(self, k, ph, nbuf=3, kc=4, n=512):
            self.k = k
            s = k.s
            self.raw = [T(s, ph, "wraw%d" % i, [128, kc, n], F32, dma=True) for i in range(nbuf)]
            self.bf = [T(s, ph, "wbf%d" % i, [128, kc, n], BF16) for i in range(nbuf)]
            self.i = 0
            self.pending = []
            self.loaded = 0
            self.nbuf = nbuf

        def plan(self, aps):
            self.pending = list(aps)
            self.issued = 0
            self.taken = 0
            for _ in range(min(self.nbuf - 1, len(self.pending))):
                self._issue()

        def _issue(self):
            ap = self.pending[self.issued]
            j = (self.i + self.issued) % self.nbuf
            raw = self.raw[j]
            kk, nn = ap.shape
            kc = kk // 128
            self.k.s.dma(raw[:, 0:kc, 0:nn], ap.rearrange("(c p) n -> p c n", p=128), writes=[raw.b], dsem=raw.d)
            self.issued += 1

        def next(self):
            s = self.k.s
            if self.issued < len(self.pending):
                self._issue()
            ap = self.pending[self.taken]
            j = (self.i + self.taken) % self.nbuf
            raw, bf = self.raw[j], self.bf[j]
            kk, nn = ap.shape
            kc = kk // 128
            s.op("pool", lambda e: e.tensor_copy(bf[:, 0:kc, 0:nn], raw[:, 0:kc, 0:nn]), reads=[raw.b], writes=[bf.b])
            self.taken += 1
            if self.taken == len(self.pending):
                self.i = (self.i + self.taken) % self.nbuf
            return bf, kc, nn


    def norm_block(self, xin, tok0, NT, BIG, aT, hT, xn, ss, rstd, ps, A, B, pctr):
        s, D = self.s, self.cfg["D"]
        KC = D // 128
        if True:
            if True:
                for t in range(NT):
                    s.dma(BIG[:, t, :], xin[tok0 + t * 128: tok0 + (t + 1) * 128, :], accs=[BIG.b] if t else (),
                          writes=() if t else [BIG.b], dsem=BIG.d)
                for t in range(NT):
                    s.op("dve", lambda e, t=t: e.tensor_tensor(out=xn, in0=BIG[:, t, :], in1=BIG[:, t, :], op=ALU.mult),
                         reads=[BIG.b], writes=[aT.b])
                    s.op("dve", lambda e, t=t: e.reduce_sum(out=ss[:, t:t + 1], in_=xn, axis=AX.X),
                         reads=[aT.b], writes=[ss.b] if t == 0 else (), accs=[ss.b] if t else ())
                    self.rsqrt_mean(rstd[:, t:t + 1], ss[:, t:t + 1], rstd.b, ss.b, D)
                    s.op("dve", lambda e, t=t: e.tensor_scalar(out=xn, in0=BIG[:, t, :], scalar1=rstd[:, t:t + 1], scalar2=None,
                                                          op0=ALU.mult), reads=[BIG.b, rstd.b], writes=[aT.b])
                    for g in range(0, KC, 8):
                        p = ps[pctr % 8]
                        pctr += 1
                        pb = p.t[:].bitcast(BF16)
                        nch = min(8, KC - g)
                        for c in range(nch):
                            s.op("pe", lambda e, c=c, g=g, pb=pb: e.transpose(pb[:, c * 128:(c + 1) * 128], xn[:, (g + c) * 128:(g + c + 1) * 128], self.identb[:]),
                                 reads=[aT.b, self.identb.b], writes=[p.b] if c == 0 else (), accs=[p.b] if c else ())
                        for c in range(nch):
                            s.op("act", lambda e, c=c, g=g, pb=pb, t=t: e.activation(out=hT[:, g + c, t * 128:(t + 1) * 128], in_=pb[:, c * 128:(c + 1) * 128],
                                                                                 func=AF.Identity, scale=A[:, g + c:g + c + 1], bias=B[:, g + c:g + c + 1]),
                                 reads=[p.b, A.b, B.b], writes=[hT.b] if (t == 0 and g == 0 and c == 0) else (),
                                 accs=() if (t == 0 and g == 0 and c == 0) else [hT.b])
        return pctr

    def ffn_phase(self, l, which, xin, xout, A, B, Gsrc):
        cfg, s = self.cfg, self.s
        D, DFF, S = cfg["D"], cfg["DFF"], cfg["S"]
        KC, FC = D // 128, DFF // 128
        TB = min(512, S)
        NT = TB // 128
        w_in = self.dram["ffn%d_in" % which][l]
        w_out = self.dram["ffn%d_out" % which][l]
        with contextlib.ExitStack() as ph:
            hT = T(s, ph, "hT", [128, KC, TB], BF16, dma=True)
            aT = T(s, ph, "aT", [128, FC, TB], BF16)
            BIG = T(s, ph, "BIG", [128, NT, D], F32, dma=True)
            G = T(s, ph, "G", [128, D], F32)
            grow = T(s, ph, "grow", [1, D], F32, dma=True)
            ss = T(s, ph, "ss", [128, NT], F32)
            rstd = T(s, ph, "rstd", [128, NT], F32)
            ssy = T(s, ph, "ssy", [128, NT, D // 512], F32)
            junk = T(s, ph, "junk", [128, 512], F32)
            su = [T(s, ph, "su%d" % i, [128, TB], F32) for i in range(2)]
            ps = [T(s, ph, "ps%d" % i, [128, 512], F32, psum=True) for i in range(8)]
            ws = K.WStream(self, ph)
            xn_v = aT.t[:, 0:KC // NT if False else FC, :]
            xn = aT.t[:].rearrange("p c t -> p (c t)")[:, 0:D]
            xh = hT.t[:].rearrange("p c t -> p (c t)").bitcast(F32)[:, 0:D]
            s.dma(grow[:], Gsrc, writes=[grow.b], dsem=grow.d)
            for n in range(D // 512):
                p = ps[n % 8]
                s.op("pe", lambda e, p=p, n=n: e.matmul(p[:], self.ones_row[:], grow[:, n * 512:(n + 1) * 512], start=True, stop=True),
                     reads=[grow.b, self.ones_row.b], writes=[p.b])
                s.op("act", lambda e, p=p, n=n: e.copy(G[:, n * 512:(n + 1) * 512], p[:]), reads=[p.b], accs=[G.b])
            if "dbg_G" in cfg.get("dbg", ()):
                dd = self.scratch("dbg_G", (128, D))
                s.dma(dd, G[:], reads=[G.b], dsem=BIG.d)
            pctr = 0
            for tb in range(S // TB):
                tok0 = tb * TB
                pctr = self.norm_block(xin, tok0, NT, BIG, aT, hT, xn, ss, rstd, ps, A, B, pctr)
                for fg in range(DFF // 512):
                    aps = []
                    for half in range(2):
                        for ks in range(D // 512):
                            aps.append(w_in[ks * 512:(ks + 1) * 512, half * DFF + fg * 512: half * DFF + (fg + 1) * 512])
                    ws.plan(aps)
                    for half in range(2):
                        for ks in range(D // 512):
                            bf, kc, nn = ws.next()
                            for k4 in range(kc):
                                kk = ks * 4 + k4
                                for c in range(4):
                                    p = ps[half * 4 + c]
                                    first = kk == 0
                                    s.op("pe", lambda e, p=p, bf=bf, k4=k4, c=c, kk=kk: e.matmul(p[:, 0:TB], bf[:, k4, c * 128:(c + 1) * 128], hT[:, kk, :],
                                                                                              start=(kk == 0), stop=(kk == KC - 1)),
                                         reads=[bf.b, hT.b], writes=[p.b] if first else (), accs=() if first else [p.b])
                    for c in range(4):
                        sb = su[c % 2]
                        s.op("act", lambda e, c=c, sb=sb: e.activation(out=sb[:], in_=ps[c][:, 0:TB], func=AF.Silu), reads=[ps[c].b], writes=[sb.b])
                        first = (fg == 0 and c == 0)
                        s.op("dve", lambda e, c=c, sb=sb, fg=fg: e.tensor_tensor(out=aT[:, fg * 4 + c, :], in0=sb[:], in1=ps[4 + c][:, 0:TB], op=ALU.mult),
                             reads=[sb.b, ps[4 + c].b], writes=[aT.b] if first else (), accs=() if first else [aT.b])
                for ng in range(D // 512):
                    ws.plan([w_out[ks * 512:(ks + 1) * 512, ng * 512:(ng + 1) * 512] for ks in range(DFF // 512)])
                    pb0 = (ng % 2) * 4
                    for ks in range(DFF // 512):
                        bf, kc, nn = ws.next()
                        for k4 in range(kc):
                            kk = ks * 4 + k4
                            for t in range(NT):
                                p = ps[pb0 + t]
                                first = kk == 0
                                s.op("pe", lambda e, p=p, bf=bf, k4=k4, t=t, kk=kk: e.matmul(p[:], aT[:, kk, t * 128:(t + 1) * 128], bf[:, k4, :],
                                                                                          start=(kk == 0), stop=(kk == FC - 1)),
                                     reads=[bf.b, aT.b], writes=[p.b] if first else (), accs=() if first else [p.b])
                    for t in range(NT):
                        p = ps[pb0 + t]
                        first = (ng == 0 and t == 0)
                        s.op("act", lambda e, p=p, t=t, ng=ng: e.copy(BIG[:, t, ng * 512:(ng + 1) * 512], p[:]), reads=[p.b],
                             writes=[BIG.b] if first else (), accs=() if first else [BIG.b])
                        s.op("dve", lambda e, t=t, ng=ng: e.tensor_tensor(out=junk[:], in0=BIG[:, t, ng * 512:(ng + 1) * 512],
                                                                       in1=BIG[:, t, ng * 512:(ng + 1) * 512], op=ALU.mult),
                             reads=[BIG.b], writes=[junk.b])
                        s.op("dve", lambda e, t=t, ng=ng: e.reduce_sum(out=ssy[:, t, ng:ng + 1], in_=junk[:], axis=AX.X),
                             reads=[junk.b], accs=[ssy.b])
                if "dbg_y" in cfg.get("dbg", ()) and tb == 0:
                    dd = self.scratch("dbg_y", (128, NT * D))
                    s.dma(dd, BIG[:].rearrange("p t d -> p (t d)"), reads=[BIG.b], dsem=BIG.d)
                    dd2 = self.scratch("dbg_a", (128, FC * TB), BF16)
                    s.dma(dd2, aT[:].rearrange("p c t -> p (c t)"), reads=[aT.b], dsem=BIG.d)
                for t in range(NT):
                    s.op("dve", lambda e, t=t: e.reduce_sum(out=ss[:, t:t + 1], in_=ssy[:, t, :], axis=AX.X), reads=[ssy.b], accs=[ss.b])
                    self.rsqrt_mean(rstd[:, t:t + 1], ss[:, t:t + 1], rstd.b, ss.b, D)
                    s.dma(xh, xin[tok0 + t * 128: tok0 + (t + 1) * 128, :], writes=[hT.b], dsem=hT.d)
                    s.op("dve", lambda e, t=t: e.scalar_tensor_tensor(out=BIG[:, t, :], in0=BIG[:, t, :], scalar=rstd[:, t:t + 1], in1=G[:],
                                                                 op0=ALU.mult, op1=ALU.mult), reads=[BIG.b, rstd.b, G.b], accs=[BIG.b])
                    s.op("dve", lambda e, t=t: e.tensor_tensor(out=BIG[:, t, :], in0=BIG[:, t, :], in1=xh, op=ALU.add),
                         reads=[BIG.b, hT.b], accs=[BIG.b])
                    s.dma(xout[tok0 + t * 128: tok0 + (t + 1) * 128, :], BIG[:, t, :], reads=[BIG.b], dsem=BIG.d)
            s.flush()


    def merge_phase(self, l, xin, xout, Gsrc):
        cfg, s = self.cfg, self.s
        D, S = cfg["D"], cfg["S"]
        KC = D // 128
        TB = min(512, S)
        NT = TB // 128
        w_out = self.dram["w_out"][l]
        w_br = self.dram["w_branch"][l]
        gT = self.mx["gT"]
        yT = self.mx["yT"]
        with contextlib.ExitStack() as ph:
            mT = T(s, ph, "mT", [128, KC, TB], BF16)
            BIG = T(s, ph, "BIG", [128, NT, D], F32, dma=True)
            G = T(s, ph, "G", [128, D], F32)
            grow = T(s, ph, "grow", [1, D], F32, dma=True)
            ss = T(s, ph, "ss", [128, NT], F32)
            rstd = T(s, ph, "rstd", [128, NT], F32)
            ssy = T(s, ph, "ssy", [128, NT, D // 512], F32)
            junk = T(s, ph, "junk", [128, 512], F32)
            ybuf = T(s, ph, "ybuf", [128, 3, 8, TB], BF16, dma=True)
            gts = [T(s, ph, "gts%d" % i, [128, TB], F32, dma=True) for i in range(4)]
            macc = T(s, ph, "macc", [128, 4, TB], F32)
            tmp = T(s, ph, "tmp", [128, TB], F32)
            ps = [T(s, ph, "ps%d" % i, [128, 512], F32, psum=True) for i in range(8)]
            ws = K.WStream(self, ph, nbuf=2)
            xh = ybuf.t[:].rearrange("p a c t -> p (a c t)").bitcast(F32)[:, 0:D]
            s.dma(grow[:], Gsrc, writes=[grow.b], dsem=grow.d)
            for n in range(D // 512):
                p = ps[n % 8]
                s.op("pe", lambda e, p=p, n=n: e.matmul(p[:], self.ones_row[:], grow[:, n * 512:(n + 1) * 512], start=True, stop=True),
                     reads=[grow.b, self.ones_row.b], writes=[p.b])
                s.op("act", lambda e, p=p, n=n: e.copy(G[:, n * 512:(n + 1) * 512], p[:]), reads=[p.b], accs=[G.b])
            gi = 0
            grp = 0
            for tb in range(S // TB):
                tok0 = tb * TB
                for br in range(3):
                    s.dma(ybuf[:, br, :, :], yT[br].rearrange("(c p) s -> p c s", p=128)[:, :, tok0:tok0 + TB],
                          writes=[ybuf.b] if br == 0 else (), accs=[ybuf.b] if br else (), dsem=ybuf.d)
                for dg in range(D // 512):
                    for br in range(3):
                        ws.plan([w_br[br, ks * 512:(ks + 1) * 512, dg * 512:(dg + 1) * 512] for ks in range(2)])
                        pb0 = (grp % 2) * 4
                        grp += 1
                        for ks in range(2):
                            bf, kc, nn = ws.next()
                            for k4 in range(kc):
                                kk = ks * 4 + k4
                                for c in range(4):
                                    p = ps[pb0 + c]
                                    first = kk == 0
                                    s.op("pe", lambda e, p=p, bf=bf, k4=k4, c=c, kk=kk, br=br: e.matmul(
                                        p[:, 0:TB], bf[:, k4, c * 128:(c + 1) * 128], ybuf[:, br, kk, :], start=(kk == 0), stop=(kk == 7)),
                                        reads=[bf.b, ybuf.b], writes=[p.b] if first else (), accs=() if first else [p.b])
                        for c in range(4):
                            p = ps[pb0 + c]
                            dch = dg * 4 + c
                            gt = gts[gi % 4]
                            gi += 1
                            s.dma(gt[:], gT[br * D + dch * 128: br * D + (dch + 1) * 128, tok0:tok0 + TB], writes=[gt.b], dsem=gt.d)
                            if br == 0:
                                s.op("dve", lambda e, p=p, c=c, gt=gt: e.tensor_tensor(out=macc[:, c, :], in0=p[:, 0:TB], in1=gt[:], op=ALU.mult),
                                     reads=[p.b, gt.b], writes=[macc.b] if c == 0 else (), accs=[macc.b] if c else ())
                            else:
                                s.op("dve", lambda e, p=p, gt=gt: e.tensor_tensor(out=tmp[:], in0=p[:, 0:TB], in1=gt[:], op=ALU.mult),
                                     reads=[p.b, gt.b], writes=[tmp.b])
                                s.op("dve", lambda e, c=c: e.tensor_tensor(out=macc[:, c, :], in0=macc[:, c, :], in1=tmp[:], op=ALU.add),
                                     reads=[macc.b, tmp.b], accs=[macc.b])
                            if br == 2:
                                first = (dg == 0 and c == 0)
                                s.op("act", lambda e, c=c, dch=dch: e.copy(mT[:, dch, :], macc[:, c, :]), reads=[macc.b],
                                     writes=[mT.b] if first else (), accs=() if first else [mT.b])
                for ng in range(D // 512):
                    ws.plan([w_out[ks * 512:(ks + 1) * 512, ng * 512:(ng + 1) * 512] for ks in range(D // 512)])
                    pb0 = (ng % 2) * 4
                    for ks in range(D // 512):
                        bf, kc, nn = ws.next()
                        for k4 in range(kc):
                            kk = ks * 4 + k4
                            for t in range(NT):
                                p = ps[pb0 + t]
                                first = kk == 0
                                s.op("pe", lambda e, p=p, bf=bf, k4=k4, t=t, kk=kk: e.matmul(p[:], mT[:, kk, t * 128:(t + 1) * 128], bf[:, k4, :],
                                                                                          start=(kk == 0), stop=(kk == KC - 1)),
                                     reads=[bf.b, mT.b], writes=[p.b] if first else (), accs=() if first else [p.b])
                    for t in range(NT):
                        p = ps[pb0 + t]
                        first = (ng == 0 and t == 0)
                        s.op("act", lambda e, p=p, t=t, ng=ng: e.copy(BIG[:, t, ng * 512:(ng + 1) * 512], p[:]), reads=[p.b],
                             writes=[BIG.b] if first else (), accs=() if first else [BIG.b])
                        s.op("dve", lambda e, t=t, ng=ng: e.tensor_tensor(out=junk[:], in0=BIG[:, t, ng * 512:(ng + 1) * 512],
                                                                       in1=BIG[:, t, ng * 512:(ng + 1) * 512], op=ALU.mult),
                             reads=[BIG.b], writes=[junk.b])
                        s.op("dve", lambda e, t=t, ng=ng: e.reduce_sum(out=ssy[:, t, ng:ng + 1], in_=junk[:], axis=AX.X),
                             reads=[junk.b], accs=[ssy.b])
                for t in range(NT):
                    s.op("dve", lambda e, t=t: e.reduce_sum(out=ss[:, t:t + 1], in_=ssy[:, t, :], axis=AX.X), reads=[ssy.b], accs=[ss.b])
                    self.rsqrt_mean(rstd[:, t:t + 1], ss[:, t:t + 1], rstd.b, ss.b, D)
                    s.dma(xh, xin[tok0 + t * 128: tok0 + (t + 1) * 128, :], writes=[ybuf.b], dsem=ybuf.d)
                    s.op("dve", lambda e, t=t: e.scalar_tensor_tensor(out=BIG[:, t, :], in0=BIG[:, t, :], scalar=rstd[:, t:t + 1], in1=G[:],
                                                                 op0=ALU.mult, op1=ALU.mult), reads=[BIG.b, rstd.b, G.b], accs=[BIG.b])
                    s.op("dve", lambda e, t=t: e.tensor_tensor(out=BIG[:, t, :], in0=BIG[:, t, :], in1=xh, op=ALU.add),
                         reads=[BIG.b, ybuf.b], accs=[BIG.b])
                    s.dma(xout[tok0 + t * 128: tok0 + (t + 1) * 128, :], BIG[:, t, :], reads=[BIG.b], dsem=BIG.d)
            s.flush()

    def rsqrt_mean(self, out, ssq, outb, ssb, n, npart=128):
        s = self.s
        s.op("act", lambda e: e.activation(out=out, in_=ssq, func=AF.Sqrt, scale=1.0 / n, bias=self.eps[0:npart, 0:1]), reads=[ssb, self.eps.b], accs=[outb])
        s.op("dve", lambda e: e.reciprocal(out, out), reads=[outb], accs=[outb])

    def colload(self, ph, dst_ap, src_vec, C, ps, name):
        s = self.s
        tmp = T(s, ph, "cl_" + name, [C, 128], F32, dma=True)
        s.dma(tmp[:], src_vec.rearrange("(c p) -> c p", p=128), writes=[tmp.b], dsem=tmp.d)
        s.op("pe", lambda e: e.transpose(ps[:, 0:C], tmp[:], self.identf[0:C, 0:C]), reads=[tmp.b, self.identf.b], writes=[ps.b])
        return ps

    def mod_setup(self):
        s, st, cfg = self.s, self.stack, self.cfg
        KC = cfg["D"] // 128
        self.AB = [T(s, st, "AB%d" % i, [128, KC], F32) for i in range(6)]
        self.condT = T(s, st, "condT", [128, 2], F32)

    def cond_phase(self, cvec):
        s, cfg = self.s, self.cfg
        D = cfg["D"]
        KC = D // 128
        wcd = self.dram["w_c_down"]
        with contextlib.ExitStack() as ph:
            ps = [T(s, ph, "ps%d" % i, [128, 512], F32, psum=True) for i in range(2)]
            cT = T(s, ph, "cT", [128, KC], F32)
            w = T(s, ph, "wcd", [128, KC, 256], F32, dma=True)
            self.colload(ph, None, cvec, KC, ps[0], "c")
            s.op("dve", lambda e: e.tensor_copy(cT[:], ps[0][:, 0:KC]), reads=[ps[0].b], writes=[cT.b])
            s.dma(w[:], wcd.rearrange("(c p) r -> p c r", p=128), writes=[w.b], dsem=w.d)
            for rc in range(2):
                for c in range(KC):
                    s.op("pe", lambda e, rc=rc, c=c: e.matmul(ps[1][:, rc:rc + 1], w[:, c, rc * 128:(rc + 1) * 128], cT[:, c:c + 1],
                                                         start=(c == 0), stop=(c == KC - 1)),
                         reads=[w.b, cT.b], writes=[ps[1].b] if (c == 0 and rc == 0) else (), accs=() if (c == 0 and rc == 0) else [ps[1].b])
            s.op("act", lambda e: e.activation(out=self.condT[:], in_=ps[1][:, 0:2], func=AF.Silu), reads=[ps[1].b], writes=[self.condT.b])
            s.flush()

    def mod_phase(self, l, grow_dram):
        s, cfg = self.s, self.cfg
        D = cfg["D"]
        KC = D // 128
        wcu = self.dram["w_c_up"][l]
        gains = self.dram["norm_gains"][l]
        PW = min(1024, D)
        with contextlib.ExitStack() as ph:
            ps = [T(s, ph, "ps%d" % i, [128, 512], F32, psum=True) for i in range(4)]
            piece = [T(s, ph, "wcu%d" % i, [128, 2, PW], F32, dma=True) for i in range(2)]
            gcol = T(s, ph, "gcol", [128, KC], F32)
            grow = T(s, ph, "grow", [1, D], F32, dma=True)
            gain_row = T(s, ph, "gain_row", [1, D], F32, dma=True)
            pi = 0
            for sub in range(3):
                self.colload(ph, None, gains[2 * sub], KC, ps[0], "g%d" % sub)
                s.op("dve", lambda e: e.tensor_copy(gcol[:], ps[0][:, 0:KC]), reads=[ps[0].b], writes=[gcol.b])
                for which in range(3):
                    m = 3 * sub + which
                    if which == 2:
                        s.dma(gain_row[:], gains[2 * sub + 1].rearrange("(o d) -> o d", o=1), writes=[gain_row.b], dsem=gain_row.d)
                    for pc in range(D // PW):
                        pt = piece[pi % 2]
                        pi += 1
                        s.dma(pt[:], wcu[:, m * D + pc * PW: m * D + (pc + 1) * PW].rearrange("(c p) n -> p c n", p=128),
                              writes=[pt.b], dsem=pt.d)
                        if which < 2:
                            pcol = ps[1 + which]
                            for cc in range(PW // 128):
                                c = pc * (PW // 128) + cc
                                for rc in range(2):
                                    first = (c == 0 and rc == 0)
                                    s.op("pe", lambda e, pt=pt, rc=rc, cc=cc, c=c, pcol=pcol: e.matmul(
                                        pcol[:, c:c + 1], pt[:, rc, cc * 128:(cc + 1) * 128], self.condT[:, rc:rc + 1], start=(rc == 0), stop=(rc == 1)),
                                        reads=[pt.b, self.condT.b], writes=[pcol.b] if first else (), accs=() if first else [pcol.b])
                        else:
                            for n in range(PW // 512):
                                for rc in range(2):
                                    s.op("pe", lambda e, pt=pt, rc=rc, n=n: e.matmul(ps[3][0:1, :], self.condT[:, rc:rc + 1], pt[:, rc, n * 512:(n + 1) * 512],
                                                                                 start=(rc == 0), stop=(rc == 1)),
                                         reads=[pt.b, self.condT.b], writes=[ps[3].b] if rc == 0 else (), accs=[ps[3].b] if rc else ())
                                col0 = pc * PW + n * 512
                                res = 0.5 if sub != 1 else 1.0
                                s.op("dve", lambda e, col0=col0, res=res: e.scalar_tensor_tensor(
                                    out=grow[:, col0:col0 + 512], in0=ps[3][0:1, :], scalar=res, in1=gain_row[:, col0:col0 + 512],
                                    op0=ALU.mult, op1=ALU.mult), reads=[ps[3].b, gain_row.b], accs=[grow.b])
                    if which == 0:
                        s.op("dve", lambda e, sub=sub: e.tensor_copy(self.AB[2 * sub + 1][:], ps[1][:, 0:KC]), reads=[ps[1].b], writes=[self.AB[2 * sub + 1].b])
                    elif which == 1:
                        s.op("dve", lambda e, sub=sub: e.scalar_tensor_tensor(out=self.AB[2 * sub][:], in0=ps[2][:, 0:KC], scalar=1.0, in1=gcol[:],
                                                                         op0=ALU.add, op1=ALU.mult),
                             reads=[ps[2].b, gcol.b], writes=[self.AB[2 * sub].b])
                    else:
                        s.dma(grow_dram[sub], grow[:], reads=[grow.b], dsem=grow.d)
            s.flush()

    def mixer_scratch(self):
        cfg = self.cfg
        S, D = cfg["S"], cfg["D"]
        sc = self.scratch
        self.mx = dict(
            aqT=sc("aqT", (1024, S)), afT=sc("afT", (1024, S)), af=sc("af", (S, 1024)), ai=sc("ai", (S, 1024)),
            ag=sc("ag", (S, 1024)), bqT=sc("bqT", (1024, S)), blat=sc("blat", (S, 512)), biqT=sc("biqT", (1024, S)),
            bikT=sc("bikT", (64, S)), biw=sc("biw", (S, 16)), cqT=sc("cqT", (1024, S)), ckT=sc("ckT", (128, S)),
            cv=sc("cv", (S, 128)), gT=sc("gT", (3 * D, S)),
            yT=sc("yT", (3, 1024, S), BF16),
        )

    def proj_phase(self, l, xin, A, B):
        cfg, s = self.cfg, self.s
        D, S = cfg["D"], cfg["S"]
        KC = D // 128
        TB = min(512, S)
        NT = TB // 128
        w_in = self.dram["w_in"][l]
        mx = self.mx
        jobs = []
        col = 0
        for name, ncols, modes in (("aq", 1024, ("fm",)), ("af", 1024, ("fm", "tm")), ("ai", 1024, ("tm",)), ("ag", 1024, ("tm",)),
                                   ("bq", 1024, ("fm",)), ("blat", 512, ("tm",)), ("biq", 1024, ("fm",)), ("bik", 64, ("fm",)),
                                   ("biw", 16, ("tm",)), ("cq", 1024, ("fm",)), ("ck", 128, ("fm",)), ("cv", 128, ("tm",)),
                                   ("g", 3 * D, ("fm",))):
            for m in modes:
                dst = mx[name + "T"] if m == "fm" else mx[name]
                jobs.append((col, ncols, m, dst, AF.Sigmoid if name == "g" else AF.Identity))
            col += ncols
        with contextlib.ExitStack() as ph:
            hT = T(s, ph, "hT", [128, KC, TB], BF16)
            xnT = T(s, ph, "xnT", [128, D], BF16)
            BIG = T(s, ph, "BIG", [128, NT, D], F32, dma=True)
            ss = T(s, ph, "ss", [128, NT], F32)
            rstd = T(s, ph, "rstd", [128, NT], F32)
            stage = [T(s, ph, "stg%d" % i, [128, 512], F32, dma=True) for i in range(4)]
            ps = [T(s, ph, "ps%d" % i, [128, 512], F32, psum=True) for i in range(8)]
            ws = K.WStream(self, ph)
            pctr = 0
            sctr = 0
            gctr = 0
            for tb in range(S // TB):
                tok0 = tb * TB
                pctr = self.norm_block(xin, tok0, NT, BIG, xnT, hT, xnT[:], ss, rstd, ps, A, B, pctr)
                if "dbg_AB" in cfg.get("dbg", ()) and tb == 0:
                    dd = self.scratch("dbg_AB", (128, 2 * KC + NT))
                    s.dma(dd[:, 0:KC], A[:], reads=[A.b], dsem=BIG.d)
                    s.dma(dd[:, KC:2 * KC], B[:], reads=[B.b], dsem=BIG.d)
                    s.dma(dd[:, 2 * KC:], rstd[:], reads=[rstd.b], dsem=BIG.d)
                if "dbg_hT" in cfg.get("dbg", ()) and tb == 0:
                    dd = self.scratch("dbg_hT", (128, KC * TB), BF16)
                    s.dma(dd, hT[:].rearrange("p c t -> p (c t)"), reads=[hT.b], dsem=BIG.d)
                for (col0, ncols, mode, dst, func) in jobs:
                    for c0 in range(0, ncols, 512):
                        w = min(512, ncols - c0)
                        ws.plan([w_in[ks * 512:(ks + 1) * 512, col0 + c0: col0 + c0 + w] for ks in range(D // 512)])
                        pb0 = (gctr % 2) * 4
                        gctr += 1
                        nchunk = (w + 127) // 128
                        for ks in range(D // 512):
                            bf, kc, nn = ws.next()
                            if "dbg_w" in cfg.get("dbg", ()) and gctr == 1 and tb == 0:
                                dd = self.scratch("dbg_w", (128, 4 * 512), BF16)
                                s.dma(dd, bf[:].rearrange("p c t -> p (c t)"), reads=[bf.b], dsem=BIG.d)
                            for k4 in range(kc):
                                kk = ks * 4 + k4
                                first = kk == 0
                                if mode == "fm":
                                    for c in range(nchunk):
                                        mc = min(128, w - c * 128)
                                        p = ps[pb0 + c]
                                        s.op("pe", lambda e, p=p, bf=bf, k4=k4, c=c, kk=kk, mc=mc: e.matmul(
                                            p[0:mc, 0:TB], bf[:, k4, c * 128:c * 128 + mc], hT[:, kk, :], start=(kk == 0), stop=(kk == KC - 1)),
                                            reads=[bf.b, hT.b], writes=[p.b] if first else (), accs=() if first else [p.b])
                                else:
                                    for t in range(NT):
                                        p = ps[pb0 + t]
                                        s.op("pe", lambda e, p=p, bf=bf, k4=k4, t=t, kk=kk, w=w: e.matmul(
                                            p[:, 0:w], hT[:, kk, t * 128:(t + 1) * 128], bf[:, k4, 0:w], start=(kk == 0), stop=(kk == KC - 1)),
                                            reads=[bf.b, hT.b], writes=[p.b] if first else (), accs=() if first else [p.b])
                        if mode == "fm":
                            for c in range(nchunk):
                                mc = min(128, w - c * 128)
                                p = ps[pb0 + c]
                                sg = stage[sctr % 4]
                                sctr += 1
                                s.op("act", lambda e, p=p, sg=sg, mc=mc, func=func: e.activation(out=sg[0:mc, 0:TB], in_=p[0:mc, 0:TB], func=func),
                                     reads=[p.b], writes=[sg.b])
                                s.dma(dst[c0 + c * 128: c0 + c * 128 + mc, tok0:tok0 + TB], sg[0:mc, 0:TB], reads=[sg.b], dsem=sg.d)
                        else:
                            for t in range(NT):
                                p = ps[pb0 + t]
                                sg = stage[sctr % 4]
                                sctr += 1
                                s.op("act", lambda e, p=p, sg=sg, w=w: e.copy(sg[:, 0:w], p[:, 0:w]), reads=[p.b], writes=[sg.b])
                                s.dma(dst[tok0 + t * 128: tok0 + (t + 1) * 128, c0:c0 + w], sg[:, 0:w], reads=[sg.b], dsem=sg.d)
            s.flush()

    PITCH = 512
    YSZ = 128 * 512 + 512

    def bcast_rows(self, dst_ap, row_ap, n, ps, npart, reads, dstb, first=True):
        s = self.s
        for c0 in range(0, n, 512):
            w = min(512, n - c0)
            s.op("pe", lambda e, c0=c0, w=w: e.matmul(ps[0:npart, 0:w], self.ones_row[0:1, 0:npart], row_ap[0:1, c0:c0 + w], start=True, stop=True),
                 reads=list(reads) + [self.ones_row.b], writes=[ps.b])
            s.op("act", lambda e, c0=c0, w=w: e.copy(dst_ap[0:npart, c0:c0 + w], ps[0:npart, 0:w]), reads=[ps.b],
                 writes=[dstb] if (first and c0 == 0) else (), accs=() if (first and c0 == 0) else [dstb])

    def setup_phase(self):
        s, st, cfg = self.s, self.stack, self.cfg
        DEPTH = cfg["DEPTH"]
        self.U = T(s, st, "U", [64, 64], F32)
        self.Mgt = T(s, st, "Mgt", [64, 64], F32)
        s.op("pool", lambda e: e.memset(self.U[:], 1.0), writes=[self.U.b])
        s.op("pool", lambda e: e.affine_select(out=self.U[:], in_=self.U[:], pattern=[[1, 64]], compare_op=ALU.is_ge, fill=0.0,
                                               base=0, channel_multiplier=-1), reads=[self.U.b], accs=[self.U.b])
        s.op("pool", lambda e: e.memset(self.Mgt[:], 1.0), writes=[self.Mgt.b])
        s.op("pool", lambda e: e.affine_select(out=self.Mgt[:], in_=self.Mgt[:], pattern=[[-1, 64]], compare_op=ALU.is_gt, fill=0.0,
                                               base=0, channel_multiplier=1), reads=[self.Mgt.b], accs=[self.Mgt.b])
        self.cmask = T(s, st, "cmask", [128, 128], F32)
        s.op("pool", lambda e: e.memset(self.cmask[:], 0.0), writes=[self.cmask.b])
        s.op("pool", lambda e: e.affine_select(out=self.cmask[:], in_=self.cmask[:], pattern=[[-1, 128]], compare_op=ALU.is_ge, fill=-1e30,
                                               base=0, channel_multiplier=1), reads=[self.cmask.b], accs=[self.cmask.b])
        lb_d = self.scratch("lb_d", (DEPTH, 1024))
        Z_d = self.scratch("Z_d", (2, 24, 384))
        self.Yc_d = self.nc.dram_tensor("Yc_d", [16 * K.YSZ], F32)
        self.Yb_d = self.nc.dram_tensor("Yb_d", [8 * K.YSZ], F32)
        with contextlib.ExitStack() as ph:
            lg = T(s, ph, "lg", [1, DEPTH, 1024], F32, dma=True)
            ex = T(s, ph, "ex", [1, DEPTH, 1024], F32)
            lbrow = T(s, ph, "lbrow", [1, DEPTH, 1024], F32, dma=True)
            mx = T(s, ph, "mx", [1, 1024], F32)
            sm = T(s, ph, "sm", [1, 1024], F32)
            cum = T(s, ph, "cum", [1, 1024], F32)
            s.dma(lg[:], self.dram["lb_logits"].rearrange("(o l) d -> o l d", o=1), writes=[lg.b], dsem=lg.d)
            s.op("dve", lambda e: e.tensor_copy(mx[:], lg[:, 0, :]), reads=[lg.b], writes=[mx.b])
            for l in range(1, DEPTH):
                s.op("dve", lambda e, l=l: e.tensor_tensor(out=mx[:], in0=mx[:], in1=lg[:, l, :], op=ALU.max), reads=[mx.b, lg.b], accs=[mx.b])
            for l in range(DEPTH):
                s.op("dve", lambda e, l=l: e.tensor_tensor(out=ex[:, l, :], in0=lg[:, l, :], in1=mx[:], op=ALU.subtract), reads=[lg.b, mx.b],
                     writes=[ex.b] if l == 0 else (), accs=[ex.b] if l else ())
            s.op("act", lambda e: e.activation(out=ex[:], in_=ex[:], func=AF.Exp), reads=[ex.b], accs=[ex.b])
            s.op("dve", lambda e: e.tensor_copy(sm[:], ex[:, 0, :]), reads=[ex.b], writes=[sm.b])
            for l in range(1, DEPTH):
                s.op("dve", lambda e, l=l: e.tensor_tensor(out=sm[:], in0=sm[:], in1=ex[:, l, :], op=ALU.add), reads=[sm.b, ex.b], accs=[sm.b])
            s.op("dve", lambda e: e.reciprocal(sm[:], sm[:]), reads=[sm.b], accs=[sm.b])
            s.op("dve", lambda e: e.memset(lbrow[:, 0, :], 0.0), writes=[lbrow.b])
            s.op("dve", lambda e: e.memset(cum[:], 0.0), writes=[cum.b])
            for l in range(1, DEPTH):
                s.op("dve", lambda e, l=l: e.tensor_tensor(out=ex[:, l, :], in0=ex[:, l, :], in1=sm[:], op=ALU.mult), reads=[ex.b, sm.b], accs=[ex.b])
                s.op("dve", lambda e, l=l: e.tensor_tensor(out=cum[:], in0=cum[:], in1=ex[:, l, :], op=ALU.add), reads=[cum.b, ex.b], accs=[cum.b])
                s.op("dve", lambda e, l=l: e.tensor_copy(lbrow[:, l, :], cum[:]), reads=[cum.b], accs=[lbrow.b])
            s.dma(lb_d.rearrange("(o l) d -> o l d", o=1), lbrow[:], reads=[lbrow.b], dsem=lbrow.d)
            relT = T(s, ph, "relT", [33, 24], F32, dma=True)
            oh = T(s, ph, "oh", [33, 2 * 383], F32, dma=True)
            Zsb = T(s, ph, "Zsb", [24, 2, 384], F32, dma=True)
            psz = [T(s, ph, "psz%d" % i, [128, 512], F32, psum=True) for i in range(2)]
            s.op("pool", lambda e: e.memset(relT[:], -1e30), writes=[relT.b])
            s.dma(relT[0:32, :], self.dram["rel_table"], reads=[relT.b], accs=[relT.b], dsem=relT.d)
            s.dma(oh[:], self.dram["t5_onehot"], writes=[oh.b], dsem=oh.d)
            s.op("pool", lambda e: e.memset(Zsb[:], 0.0), writes=[Zsb.b])
            for j in range(2):
                s.op("pe", lambda e, j=j: e.matmul(psz[j][0:24, 0:383], relT[:], oh[:, j * 383:(j + 1) * 383], start=True, stop=True),
                     reads=[relT.b, oh.b], writes=[psz[j].b])
                s.op("act", lambda e, j=j: e.copy(Zsb[:, j, 0:383], psz[j][0:24, 0:383]), reads=[psz[j].b], accs=[Zsb.b])
            s.dma(Z_d.rearrange("j h i -> h j i"), Zsb[:], reads=[Zsb.b], dsem=Zsb.d)
            s.flush()
        with contextlib.ExitStack() as ph:
            dummy = T(s, ph, "dummy", [1, 8], F32, dma=True)
            for j, (Y, nh, h0) in enumerate(((self.Yc_d, 16, 8), (self.Yb_d, 8, 0))):
                for h in range(nh):
                    src = bass.AP(tensor=Z_d.tensor, offset=(j * 24 + h0 + h) * 384, ap=[[0, 128], [1, 383]])
                    dst = bass.AP(tensor=Y, offset=h * K.YSZ, ap=[[K.PITCH + 1, 128], [1, 383]])
                    s.dma(dst, src, accs=[dummy.b], dsem=dummy.d)
            s.flush()

    def bias_view(self, Y, h):
        return bass.AP(tensor=Y, offset=h * K.YSZ + 127, ap=[[K.PITCH, 128], [1, 256]])

    def swa_phase(self, l):
        cfg, s = self.cfg, self.s
        S = cfg["S"]
        NB = S // 128
        mx_ = self.mx
        with contextlib.ExitStack() as ph:
            ps = [T(s, ph, "ps%d" % i, [128, 512], F32, psum=True) for i in range(8)]
            stg = T(s, ph, "stg", [128, S], F32, dma=True)
            qb = T(s, ph, "qb", [128, S], BF16)
            kdup = [T(s, ph, "kdup%d" % g, [128, S], BF16) for g in range(2)]
            vb = T(s, ph, "vb", [128, NB, 128], BF16)
            vst = T(s, ph, "vst", [128, NB, 128], F32, dma=True)
            biasC = T(s, ph, "biasC", [128, 16, 256], F32, dma=True)
            sinkB = T(s, ph, "sinkB", [128, 16], F32)
            srow = T(s, ph, "srow", [1, 16], F32, dma=True)
            lg = T(s, ph, "lg", [128, 256], F32)
            pe_ = T(s, ph, "pexp", [128, 256], F32)
            pn = T(s, ph, "pn", [128, 256], BF16)
            pT = T(s, ph, "pT", [128, 2, 128], BF16)
            sm = T(s, ph, "sm", [128, 4], F32)
            yc = T(s, ph, "yc", [128, NB, 1024], BF16)
            ycT = T(s, ph, "ycT", [128, 8, S], BF16, dma=True)
            for h in range(16):
                s.dma(biasC[:, h, :], self.bias_view(self.Yc_d, h), writes=[biasC.b] if h == 0 else (), accs=[biasC.b] if h else (), dsem=biasC.d)
            s.dma(srow[:], self.dram["sinks"][l].rearrange("(o h) -> o h", o=1), writes=[srow.b], dsem=srow.d)
            self.bcast_rows(sinkB, srow, 16, ps[0], 128, [srow.b], sinkB.b)
            for g in range(2):
                s.dma(stg[0:64, :], mx_["ckT"][g * 64:(g + 1) * 64, :], writes=[stg.b], dsem=stg.d)
                s.dma(stg[64:128, :], mx_["ckT"][g * 64:(g + 1) * 64, :], accs=[stg.b], dsem=stg.d)
                s.op("dve", lambda e, g=g: e.tensor_copy(kdup[g][:], stg[:]), reads=[stg.b], writes=[kdup[g].b])
            s.dma(vst[:], mx_["cv"].rearrange("(n s) d -> s n d", s=128), writes=[vst.b], dsem=vst.d)
            s.op("dve", lambda e: e.tensor_copy(vb[:], vst[:]), reads=[vst.b], writes=[vb.b])
            pc = 0
            for j in range(8):
                g = j // 4
                s.dma(stg[:], mx_["cqT"][j * 128:(j + 1) * 128, :], writes=[stg.b], dsem=stg.d)
                s.op("dve", lambda e: e.tensor_copy(qb[:], stg[:]), reads=[stg.b], writes=[qb.b])
                for hh in range(2):
                    h = 2 * j + hh
                    hb = hh * 64
                    for nb in range(NB):
                        k0 = max(nb - 1, 0) * 128
                        kw = 256 if nb > 0 else 128
                        b0 = 0 if nb > 0 else 128
                        pL = ps[pc % 8]; pc += 1
                        s.op("pe", lambda e, pL=pL, hb=hb, nb=nb, k0=k0, kw=kw, g=g: e.matmul(
                            pL[:, 0:kw], qb[hb:hb + 64, nb * 128:(nb + 1) * 128], kdup[g][hb:hb + 64, k0:k0 + kw], start=True, stop=True),
                            reads=[qb.b, kdup[g].b], writes=[pL.b])
                        s.op("dve", lambda e, pL=pL, kw=kw, b0=b0, h=h: e.scalar_tensor_tensor(
                            out=lg[:, 0:kw], in0=pL[:, 0:kw], scalar=0.125, in1=biasC[:, h, b0:b0 + kw], op0=ALU.mult, op1=ALU.add),
                            reads=[pL.b, biasC.b], writes=[lg.b])
                        s.op("dve", lambda e, kw=kw: e.reduce_max(out=sm[:, 0:1], in_=lg[:, 0:kw], axis=AX.X), reads=[lg.b], writes=[sm.b])
                        s.op("dve", lambda e, h=h: e.tensor_tensor(out=sm[:, 0:1], in0=sm[:, 0:1], in1=sinkB[:, h:h + 1], op=ALU.max),
                             reads=[sm.b, sinkB.b], accs=[sm.b])
                        s.op("dve", lambda e: e.tensor_scalar(out=sm[:, 1:2], in0=sm[:, 0:1], scalar1=-1.0, scalar2=None, op0=ALU.mult),
                             reads=[sm.b], accs=[sm.b])
                        s.op("act", lambda e, kw=kw: e.activation(out=pe_[:, 0:kw], in_=lg[:, 0:kw], func=AF.Exp, bias=sm[:, 1:2]),
                             reads=[lg.b, sm.b], writes=[pe_.b])
                        s.op("act", lambda e, h=h: e.activation(out=sm[:, 2:3], in_=sinkB[:, h:h + 1], func=AF.Exp, bias=sm[:, 1:2]),
                             reads=[sinkB.b, sm.b], accs=[sm.b])
                        s.op("dve", lambda e, kw=kw: e.reduce_sum(out=sm[:, 3:4], in_=pe_[:, 0:kw], axis=AX.X), reads=[pe_.b], accs=[sm.b])
                        s.op("dve", lambda e: e.tensor_tensor(out=sm[:, 3:4], in0=sm[:, 3:4], in1=sm[:, 2:3], op=ALU.add), reads=[sm.b], accs=[sm.b])
                        s.op("dve", lambda e: e.reciprocal(sm[:, 3:4], sm[:, 3:4]), reads=[sm.b], accs=[sm.b])
                        s.op("dve", lambda e, kw=kw: e.tensor_scalar(out=pn[:, 0:kw], in0=pe_[:, 0:kw], scalar1=sm[:, 3:4], scalar2=None, op0=ALU.mult),
                             reads=[pe_.b, sm.b], writes=[pn.b])
                        nkb = kw // 128
                        pt = ps[pc % 8]; pc += 1
                        ptb = pt.t[:].bitcast(BF16)
                        for kb in range(nkb):
                            s.op("pe", lambda e, kb=kb, ptb=ptb: e.transpose(ptb[:, kb * 128:(kb + 1) * 128], pn[:, kb * 128:(kb + 1) * 128], self.identb[:]),
                                 reads=[pn.b, self.identb.b], writes=[pt.b] if kb == 0 else (), accs=[pt.b] if kb else ())
                        s.op("act", lambda e, ptb=ptb, kw=kw: e.copy(pT[:].rearrange("p a b -> p (a b)")[:, 0:kw], ptb[:, 0:kw]), reads=[pt.b], writes=[pT.b])
                        pO = ps[pc % 8]; pc += 1
                        for kb in range(nkb):
                            blk = k0 // 128 + kb
                            s.op("pe", lambda e, kb=kb, blk=blk, pO=pO, g=g, nkb=nkb: e.matmul(
                                pO[:, 0:64], pT[:, kb, :], vb[:, blk, g * 64:(g + 1) * 64], start=(kb == 0), stop=(kb == nkb - 1)),
                                reads=[pT.b, vb.b], writes=[pO.b] if kb == 0 else (), accs=[pO.b] if kb else ())
                        first = (j == 0 and hh == 0 and nb == 0)
                        s.op("act", lambda e, pO=pO, nb=nb, h=h: e.copy(yc[:, nb, h * 64:(h + 1) * 64], pO[:, 0:64]), reads=[pO.b],
                             writes=[yc.b] if first else (), accs=() if first else [yc.b])
            for nb in range(NB):
                pt = ps[pc % 8]; pc += 1
                ptb = pt.t[:].bitcast(BF16)
                for c in range(8):
                    s.op("pe", lambda e, c=c, nb=nb, ptb=ptb: e.transpose(ptb[:, c * 128:(c + 1) * 128], yc[:, nb, c * 128:(c + 1) * 128], self.identb[:]),
                         reads=[yc.b, self.identb.b], writes=[pt.b] if c == 0 else (), accs=[pt.b] if c else ())
                s.op("act", lambda e, nb=nb, ptb=ptb: e.copy(ycT[:, :, nb * 128:(nb + 1) * 128], ptb.rearrange("p (c t) -> p c t", c=8)), reads=[pt.b],
                     writes=[ycT.b] if nb == 0 else (), accs=[ycT.b] if nb else ())
            s.dma(self.mx["yT"][2].rearrange("(c p) s -> p c s", p=128), ycT[:], reads=[ycT.b], dsem=ycT.d)
            s.flush()

    def dsa_phase(self, l):
        cfg, s = self.cfg, self.s
        S, TOPK = cfg["S"], cfg["TOPK"]
        NB = S // 128
        mx_ = self.mx
        SCALE = 128 ** -0.5
        with contextlib.ExitStack() as ph:
            ps = [T(s, ph, "ps%d" % i, [128, 512], F32, psum=True) for i in range(8)]
            stg = T(s, ph, "stg", [128, S], F32, dma=True)
            lat = T(s, ph, "lat", [128, 512], F32, dma=True)
            latn = T(s, ph, "latn", [128, 512], BF16)
            latT = T(s, ph, "latT", [128, 4, S], BF16)
            sq = T(s, ph, "sq", [128, 512], F32)
            sm = T(s, ph, "sm", [128, 8], F32)
            kvg = T(s, ph, "kvg", [128, 512], F32)
            grow_ = T(s, ph, "kvgrow", [1, 512], F32, dma=True)
            wkv = T(s, ph, "wkv", [128, 4, 256], F32, dma=True)
            wkvb = T(s, ph, "wkvb", [128, 4, 256], BF16)
            kT = T(s, ph, "kT", [128, S], BF16)
            vb = T(s, ph, "vb", [128, NB, 128], BF16)
            s.dma(grow_[:], self.dram["kv_norm"][l].rearrange("(o d) -> o d", o=1), writes=[grow_.b], dsem=grow_.d)
            self.bcast_rows(kvg, grow_, 512, ps[0], 128, [grow_.b], kvg.b)
            s.dma(wkv[:], self.dram["w_kv_up"][l].rearrange("(c p) n -> p c n", p=128), writes=[wkv.b], dsem=wkv.d)
            s.op("dve", lambda e: e.tensor_copy(wkvb[:], wkv[:]), reads=[wkv.b], writes=[wkvb.b])
            pc = 0
            for nb in range(NB):
                s.dma(lat[:], mx_["blat"][nb * 128:(nb + 1) * 128, :], writes=[lat.b], dsem=lat.d)
                s.op("dve", lambda e, nb=nb: e.tensor_tensor(out=sq[:], in0=lat[:], in1=lat[:], op=ALU.mult), reads=[lat.b], writes=[sq.b])
                s.op("dve", lambda e: e.reduce_sum(out=sm[:, 0:1], in_=sq[:], axis=AX.X), reads=[sq.b], writes=[sm.b])
                self.rsqrt_mean(sm[:, 1:2], sm[:, 0:1], sm.b, sm.b, 512)
                s.op("dve", lambda e, nb=nb: e.scalar_tensor_tensor(out=latn[:], in0=lat[:], scalar=sm[:, 1:2], in1=kvg[:], op0=ALU.mult, op1=ALU.mult),
                     reads=[lat.b, sm.b, kvg.b], writes=[latn.b])
                pt = ps[pc % 8]; pc += 1
                ptb = pt.t[:].bitcast(BF16)
                for c in range(4):
                    s.op("pe", lambda e, c=c, ptb=ptb: e.transpose(ptb[:, c * 128:(c + 1) * 128], latn[:, c * 128:(c + 1) * 128], self.identb[:]),
                         reads=[latn.b, self.identb.b], writes=[pt.b] if c == 0 else (), accs=[pt.b] if c else ())
                s.op("act", lambda e, nb=nb, ptb=ptb: e.copy(latT[:, :, nb * 128:(nb + 1) * 128], ptb[:, 0:512].rearrange("p (c t) -> p c t", c=4)),
                     reads=[pt.b], writes=[latT.b] if nb == 0 else (), accs=[latT.b] if nb else ())
            for t0 in range(0, S, 512):
                p = ps[pc % 8]; pc += 1
                for c in range(4):
                    s.op("pe", lambda e, c=c, p=p, t0=t0: e.matmul(p[:], wkvb[:, c, 0:128], latT[:, c, t0:t0 + 512], start=(c == 0), stop=(c == 3)),
                         reads=[wkvb.b, latT.b], writes=[p.b] if c == 0 else (), accs=[p.b] if c else ())
                s.op("act", lambda e, p=p, t0=t0: e.copy(kT[:, t0:t0 + 512], p[:]), reads=[p.b], writes=[kT.b] if t0 == 0 else (), accs=[kT.b] if t0 else ())
            for nb in range(NB):
                p = ps[pc % 8]; pc += 1
                for c in range(4):
                    s.op("pe", lambda e, c=c, p=p, nb=nb: e.matmul(p[:, 0:128], latT[:, c, nb * 128:(nb + 1) * 128], wkvb[:, c, 128:256], start=(c == 0), stop=(c == 3)),
                         reads=[wkvb.b, latT.b], writes=[p.b] if c == 0 else (), accs=[p.b] if c else ())
                s.op("act", lambda e, p=p, nb=nb: e.copy(vb[:, nb, :], p[:, 0:128]), reads=[p.b], writes=[vb.b] if nb == 0 else (), accs=[vb.b] if nb else ())
            ikd = T(s, ph, "ikd", [128, S], BF16)
            iqb = T(s, ph, "iqb", [128, 8, S], BF16)
            qb = T(s, ph, "qb", [128, 8, S], BF16)
            iw = T(s, ph, "iw", [128, NB, 16], F32, dma=True)
            biasB = T(s, ph, "biasB", [128, 8, 256], F32, dma=True)
            cB = T(s, ph, "cB", [128, 24], F32)
            crow = T(s, ph, "crow", [1, 24], F32, dma=True)
            s.dma(stg[0:64, :], mx_["bikT"], writes=[stg.b], dsem=stg.d)
            s.dma(stg[64:128, :], mx_["bikT"], accs=[stg.b], dsem=stg.d)
            s.op("dve", lambda e: e.tensor_copy(ikd[:], stg[:]), reads=[stg.b], writes=[ikd.b])
            for c in range(8):
                s.dma(stg[:], mx_["biqT"][c * 128:(c + 1) * 128, :], writes=[stg.b], dsem=stg.d)
                s.op("dve", lambda e, c=c: e.tensor_copy(iqb[:, c, :], stg[:]), reads=[stg.b], writes=[iqb.b] if c == 0 else (), accs=[iqb.b] if c else ())
            for c in range(8):
                s.dma(stg[:], mx_["bqT"][c * 128:(c + 1) * 128, :], writes=[stg.b], dsem=stg.d)
                s.op("dve", lambda e, c=c: e.tensor_copy(qb[:, c, :], stg[:]), reads=[stg.b], writes=[qb.b] if c == 0 else (), accs=[qb.b] if c else ())
            s.dma(iw[:], mx_["biw"].rearrange("(n t) h -> t n h", t=128), writes=[iw.b], dsem=iw.d)
            for h in range(8):
                s.dma(biasB[:, h, :], self.bias_view(self.Yb_d, h), writes=[biasB.b] if h == 0 else (), accs=[biasB.b] if h else (), dsem=biasB.d)
            s.dma(crow[:], self.dram["rel_table"][31:32, :], writes=[crow.b], dsem=crow.d)
            self.bcast_rows(cB, crow, 24, ps[0], 128, [crow.b], cB.b)
            acc = T(s, ph, "acc", [128, S], F32)
            work = T(s, ph, "work", [128, S], F32)
            rl = T(s, ph, "rl", [128, S], F32)
            mb = work
            m8 = T(s, ph, "m8", [128, 8], F32)
            lg = T(s, ph, "lg", [128, S], F32)
            pn = T(s, ph, "pn", [128, S], BF16)
            pT = T(s, ph, "pT", [128, NB, 128], BF16)
            ybT = T(s, ph, "ybT", [128, 8, S], BF16, dma=True)
            for qt in range(NB):
                kw = (qt + 1) * 128
                q0 = qt * 128
                use_topk = kw > TOPK
                if use_topk:
                    for ih in range(16):
                        c, hb = ih // 2, (ih % 2) * 64
                        pb = (ih % 2) * 4
                        for n0 in range(0, kw, 512):
                            w = min(512, kw - n0)
                            p = ps[pb + n0 // 512]
                            s.op("pe", lambda e, p=p, c=c, hb=hb, n0=n0, w=w, q0=q0: e.matmul(
                                p[:, 0:w], iqb[hb:hb + 64, c, q0:q0 + 128], ikd[hb:hb + 64, n0:n0 + w], start=True, stop=True),
                                reads=[iqb.b, ikd.b], writes=[p.b])
                            s.op("act", lambda e, p=p, n0=n0, w=w: e.activation(out=rl[:, n0:n0 + w], in_=p[:, 0:w], func=AF.Relu),
                                 reads=[p.b], writes=[rl.b] if n0 == 0 else (), accs=[rl.b] if n0 else ())
                        if ih == 0:
                            s.op("dve", lambda e, kw=kw, qt=qt: e.tensor_scalar(out=acc[:, 0:kw], in0=rl[:, 0:kw], scalar1=iw[:, qt, 0:1], scalar2=None, op0=ALU.mult),
                                 reads=[rl.b, iw.b], writes=[acc.b])
                        else:
                            s.op("dve", lambda e, kw=kw, qt=qt, ih=ih: e.scalar_tensor_tensor(
                                out=acc[:, 0:kw], in0=rl[:, 0:kw], scalar=iw[:, qt, ih:ih + 1], in1=acc[:, 0:kw], op0=ALU.mult, op1=ALU.add),
                                reads=[rl.b, iw.b, acc.b], accs=[acc.b])
                    s.op("dve", lambda e, q0=q0: e.tensor_tensor(out=acc[:, q0:q0 + 128], in0=acc[:, q0:q0 + 128], in1=self.cmask[:], op=ALU.add),
                         reads=[acc.b, self.cmask.b], accs=[acc.b])
                    s.op("dve", lambda e, kw=kw: e.tensor_copy(work[:, 0:kw], acc[:, 0:kw]), reads=[acc.b], writes=[work.b])
                    for r in range(TOPK // 8):
                        s.op("dve", lambda e, kw=kw: e.max(out=m8[:], in_=work[:, 0:kw]), reads=[work.b], writes=[m8.b])
                        if r < TOPK // 8 - 1:
                            s.op("dve", lambda e, kw=kw: e.match_replace(out=work[:, 0:kw], in_to_replace=m8[:], in_values=work[:, 0:kw], imm_value=-3e38),
                                 reads=[work.b, m8.b], accs=[work.b])
                    s.op("dve", lambda e, kw=kw: e.tensor_scalar(out=mb[:, 0:kw], in0=acc[:, 0:kw], scalar1=m8[:, 7:8], scalar2=-1e30,
                                                            op0=ALU.is_lt, op1=ALU.mult), reads=[acc.b, m8.b], writes=[mb.b])
                for h in range(8):
                    pb = (h % 2) * 4
                    for n0 in range(0, kw, 512):
                        w = min(512, kw - n0)
                        p = ps[pb + n0 // 512]
                        s.op("pe", lambda e, p=p, h=h, n0=n0, w=w, q0=q0: e.matmul(p[:, 0:w], qb[:, h, q0:q0 + 128], kT[:, n0:n0 + w], start=True, stop=True),
                             reads=[qb.b, kT.b], writes=[p.b])
                    w0 = max(kw - 256, 0)
                    first = True
                    for n0 in range(0, w0, 512):
                        w = min(512, w0 - n0)
                        p = ps[pb + n0 // 512]
                        s.op("dve", lambda e, p=p, n0=n0, w=w, h=h: e.tensor_scalar(out=lg[:, n0:n0 + w], in0=p[:, 0:w], scalar1=SCALE, scalar2=cB[:, h:h + 1],
                                                                               op0=ALU.mult, op1=ALU.add), reads=[p.b, cB.b],
                             writes=[lg.b] if first else (), accs=() if first else [lg.b])
                        first = False
                    ww = kw - w0
                    b0 = 256 - ww
                    for n0 in range(w0, kw, 128):
                        p = ps[pb + n0 // 512]
                        o = n0 % 512
                        bo = b0 + (n0 - w0)
                        s.op("dve", lambda e, p=p, n0=n0, o=o, bo=bo, h=h: e.scalar_tensor_tensor(
                            out=lg[:, n0:n0 + 128], in0=p[:, o:o + 128], scalar=SCALE, in1=biasB[:, h, bo:bo + 128], op0=ALU.mult, op1=ALU.add),
                            reads=[p.b, biasB.b], writes=[lg.b] if first else (), accs=() if first else [lg.b])
                        first = False
                    if use_topk:
                        s.op("dve", lambda e, kw=kw: e.tensor_tensor(out=lg[:, 0:kw], in0=lg[:, 0:kw], in1=mb[:, 0:kw], op=ALU.add), reads=[lg.b, mb.b], accs=[lg.b])
                    s.op("dve", lambda e, kw=kw: e.reduce_max(out=sm[:, 2:3], in_=lg[:, 0:kw], axis=AX.X), reads=[lg.b], writes=[sm.b])
                    s.op("dve", lambda e: e.tensor_scalar(out=sm[:, 3:4], in0=sm[:, 2:3], scalar1=-1.0, scalar2=None, op0=ALU.mult), reads=[sm.b], accs=[sm.b])
                    s.op("act", lambda e, kw=kw: e.activation(out=lg[:, 0:kw], in_=lg[:, 0:kw], func=AF.Exp, bias=sm[:, 3:4]), reads=[lg.b, sm.b], accs=[lg.b])
                    s.op("dve", lambda e, kw=kw: e.reduce_sum(out=sm[:, 4:5], in_=lg[:, 0:kw], axis=AX.X), reads=[lg.b], accs=[sm.b])
                    s.op("dve", lambda e: e.reciprocal(sm[:, 4:5], sm[:, 4:5]), reads=[sm.b], accs=[sm.b])
                    s.op("dve", lambda e, kw=kw: e.tensor_scalar(out=pn[:, 0:kw], in0=lg[:, 0:kw], scalar1=sm[:, 4:5], scalar2=None, op0=ALU.mult),
                         reads=[lg.b, sm.b], writes=[pn.b])
                    nkb = kw // 128
                    for k8 in range(0, nkb, 8):
                        pt = ps[pc % 8]; pc += 1
                        ptb = pt.t[:].bitcast(BF16)
                        nn = min(8, nkb - k8)
                        for kb in range(nn):
                            s.op("pe", lambda e, kb=kb, k8=k8, ptb=ptb: e.transpose(ptb[:, kb * 128:(kb + 1) * 128], pn[:, (k8 + kb) * 128:(k8 + kb + 1) * 128], self.identb[:]),
                                 reads=[pn.b, self.identb.b], writes=[pt.b] if kb == 0 else (), accs=[pt.b] if kb else ())
                        s.op("act", lambda e, ptb=ptb, k8=k8, nn=nn: e.copy(pT[:, k8:k8 + nn, :], ptb[:, 0:nn * 128].rearrange("p (a b) -> p a b", b=128)),
                             reads=[pt.b], writes=[pT.b] if k8 == 0 else (), accs=[pT.b] if k8 else ())
                    pO = ps[pc % 8]; pc += 1
                    for kb in range(nkb):
                        s.op("pe", lambda e, kb=kb, pO=pO, nkb=nkb: e.matmul(pO[:, 0:128], vb[:, kb, :], pT[:, kb, :], start=(kb == 0), stop=(kb == nkb - 1)),
                             reads=[pT.b, vb.b], writes=[pO.b] if kb == 0 else (), accs=[pO.b] if kb else ())
                    first = (qt == 0 and h == 0)
                    s.op("act", lambda e, pO=pO, h=h, q0=q0: e.copy(ybT[:, h, q0:q0 + 128], pO[:, 0:128]), reads=[pO.b],
                         writes=[ybT.b] if first else (), accs=() if first else [ybT.b])
            s.dma(self.mx["yT"][1].rearrange("(c p) s -> p c s", p=128), ybT[:], reads=[ybT.b], dsem=ybT.d)
            s.flush()

    def hgrn_phase(self, l):
        cfg, s = self.cfg, self.s
        S = cfg["S"]
        CS = 32
        SEG = cfg.get("HSEG", min(512, S))
        NCHT = S // CS
        NCH = SEG // CS
        mx_ = self.mx
        lb_d = self.dram["lb_d"]
        with contextlib.ExitStack() as ph:
            ps = [T(s, ph, "ps%d" % i, [128, 512], F32, psum=True) for i in range(8)]
            lbc = T(s, ph, "lbc", [128, 8], F32)
            omc = T(s, ph, "omc", [128, 8], F32)
            lbB = T(s, ph, "lbB", [CS, 1024], F32)
            omB = T(s, ph, "omB", [CS, 1024], F32)
            hgB = T(s, ph, "hgB", [CS, 128], F32)
            row = T(s, ph, "row", [1, 1024], F32, dma=True)
            row2 = T(s, ph, "row2", [1, 128], F32, dma=True)
            self.colload(ph, None, lb_d[l], 8, ps[0], "lb")
            s.op("dve", lambda e: e.tensor_copy(lbc[:], ps[0][:, 0:8]), reads=[ps[0].b], writes=[lbc.b])
            s.op("dve", lambda e: e.tensor_scalar(out=omc[:], in0=lbc[:], scalar1=-1.0, scalar2=1.0, op0=ALU.mult, op1=ALU.add), reads=[lbc.b], writes=[omc.b])
            s.dma(row[:], lb_d[l:l + 1, :], writes=[row.b], dsem=row.d)
            self.bcast_rows(lbB, row, 1024, ps[1], CS, [row.b], lbB.b)
            s.op("dve", lambda e: e.tensor_scalar(out=omB[:], in0=lbB[:], scalar1=-1.0, scalar2=1.0, op0=ALU.mult, op1=ALU.add), reads=[lbB.b], writes=[omB.b])
            s.dma(row2[:], self.dram["hgrn_norm"][l].rearrange("(o d) -> o d", o=1), writes=[row2.b], dsem=row2.d)
            self.bcast_rows(hgB, row2, 128, ps[1], CS, [row2.b], hgB.b)
            qT = T(s, ph, "qT", [128, NCH, CS], F32, dma=True)
            kTf = T(s, ph, "kTf", [128, NCH, CS], F32, dma=True)
            bT = T(s, ph, "bT", [128, NCH, CS], F32)
            d1 = T(s, ph, "d1", [128, NCH, CS], F32)
            bmid = T(s, ph, "bmid", [128, NCH], F32)
            bend = T(s, ph, "bend", [128, NCH], F32)
            qtil = T(s, ph, "qtil", [128, NCH, CS], BF16)
            ktil = T(s, ph, "ktil", [128, NCH, CS], BF16)
            qb = T(s, ph, "qbb", [128, NCH, CS], BF16)
            f_tm = T(s, ph, "f_tm", [CS, NCH, 128], F32, dma=True)
            lf_tm = T(s, ph, "lf_tm", [CS, NCH, 128], F32)
            k_tm = T(s, ph, "k_tm", [CS, NCH, 128], F32)
            i_tm = T(s, ph, "i_tm", [CS, NCH, 128], F32, dma=True)
            g_tm = T(s, ph, "g_tm", [CS, NCH, 128], F32, dma=True)
            kend = T(s, ph, "kend", [CS, NCH, 128], BF16)
            vb = T(s, ph, "vb", [CS, NCH, 128], BF16)
            o_sb = T(s, ph, "o_sb", [CS, NCH, 128], F32)
            ssq = T(s, ph, "ssq", [CS, NCH], F32)
            att = T(s, ph, "att", [CS, CS], BF16)
            state = T(s, ph, "state", [128, 128], F32)
            stb = T(s, ph, "stb", [128, 128], BF16)
            yaT = T(s, ph, "yaT", [128, SEG], BF16, dma=True)
            pc = 2
            for h, sg in [(h_, g_) for h_ in range(8) for g_ in range(S // SEG)]:
                hs = slice(h * 128, (h + 1) * 128)
                ts = slice(sg * SEG, (sg + 1) * SEG)
                cs_ = slice(sg * NCH, (sg + 1) * NCH)
                bc = lambda t, hs=hs: t[:, hs].unsqueeze(1).to_broadcast([CS, NCH, 128])
                s.dma(qT[:].rearrange("p n t -> p (n t)"), mx_["aqT"][hs, ts], writes=[qT.b], dsem=qT.d)
                s.dma(kTf[:].rearrange("p n t -> p (n t)"), mx_["afT"][hs, ts], writes=[kTf.b], dsem=kTf.d)
                s.dma(f_tm[:], mx_["af"].rearrange("(n t) d -> t n d", t=CS)[:, cs_, hs], writes=[f_tm.b], dsem=f_tm.d)
                s.dma(i_tm[:], mx_["ai"].rearrange("(n t) d -> t n d", t=CS)[:, cs_, hs], writes=[i_tm.b], dsem=i_tm.d)
                s.dma(g_tm[:], mx_["ag"].rearrange("(n t) d -> t n d", t=CS)[:, cs_, hs], writes=[g_tm.b], dsem=g_tm.d)
                s.op("act", lambda e: e.activation(out=kTf[:], in_=kTf[:], func=AF.Sigmoid, scale=-1.0), reads=[kTf.b], accs=[kTf.b])
                s.op("dve", lambda e, h=h: e.tensor_scalar(out=kTf[:], in0=kTf[:], scalar1=omc[:, h:h + 1], scalar2=None, op0=ALU.mult), reads=[kTf.b, omc.b], accs=[kTf.b])
                s.op("act", lambda e: e.activation(out=k_tm[:], in_=f_tm[:], func=AF.Sigmoid, scale=-1.0), reads=[f_tm.b], writes=[k_tm.b])
                s.op("dve", lambda e, bc=bc: e.tensor_tensor(out=k_tm[:], in0=k_tm[:], in1=bc(omB), op=ALU.mult), reads=[k_tm.b, omB.b], accs=[k_tm.b])
                s.op("act", lambda e: e.activation(out=lf_tm[:], in_=f_tm[:], func=AF.Sigmoid), reads=[f_tm.b], writes=[lf_tm.b])
                s.op("dve", lambda e, bc=bc: e.tensor_tensor(out=lf_tm[:], in0=lf_tm[:], in1=bc(omB), op=ALU.mult), reads=[lf_tm.b, omB.b], accs=[lf_tm.b])
                s.op("dve", lambda e, bc=bc: e.tensor_tensor(out=lf_tm[:], in0=lf_tm[:], in1=bc(lbB), op=ALU.add), reads=[lf_tm.b, lbB.b], accs=[lf_tm.b])
                s.op("dve", lambda e: e.tensor_scalar(out=lf_tm[:], in0=lf_tm[:], scalar1=1e-20, scalar2=None, op0=ALU.max), reads=[lf_tm.b], accs=[lf_tm.b])
                s.op("act", lambda e: e.activation(out=lf_tm[:], in_=lf_tm[:], func=AF.Ln), reads=[lf_tm.b], accs=[lf_tm.b])
                s.op("act", lambda e: e.activation(out=vb[:], in_=i_tm[:], func=AF.Silu), reads=[i_tm.b], writes=[vb.b])
                s.op("act", lambda e: e.activation(out=g_tm[:], in_=g_tm[:], func=AF.Silu), reads=[g_tm.b], accs=[g_tm.b])
                NPB = 512 // CS
                for n8 in range(0, NCH, NPB):
                    p = ps[pc % 8]; pc += 1
                    for n in range(n8, min(n8 + NPB, NCH)):
                        s.op("pe", lambda e, p=p, n=n, n8=n8: e.matmul(p[:, (n - n8) * CS:(n - n8 + 1) * CS], lf_tm[:, n, :], self.U[0:CS, 0:CS], start=True, stop=True),
                             reads=[lf_tm.b, self.U.b], writes=[p.b] if n == n8 else (), accs=[p.b] if n != n8 else ())
                    nn = min(NPB, NCH - n8)
                    s.op("act", lambda e, p=p, n8=n8, nn=nn: e.copy(bT[:, n8:n8 + nn, :], p[:, 0:nn * CS].rearrange("p (a b) -> p a b", b=CS)),
                         reads=[p.b], writes=[bT.b] if n8 == 0 else (), accs=[bT.b] if n8 else ())
                for n4 in range(0, NCH, 4):
                    p = ps[pc % 8]; pc += 1
                    for n in range(n4, min(n4 + 4, NCH)):
                        s.op("pe", lambda e, p=p, n=n, n4=n4: e.matmul(p[0:CS, (n - n4) * 128:(n - n4 + 1) * 128], self.Mgt[0:CS, 0:CS], lf_tm[:, n, :], start=True, stop=True),
                             reads=[lf_tm.b, self.Mgt.b], writes=[p.b] if n == n4 else (), accs=[p.b] if n != n4 else ())
                    nn = min(4, NCH - n4)
                    s.op("act", lambda e, p=p, n4=n4, nn=nn: e.activation(out=o_sb[:, n4:n4 + nn, :], in_=p[0:CS, 0:nn * 128].rearrange("p (a b) -> p a b", b=128), func=AF.Exp),
                         reads=[p.b], writes=[o_sb.b] if n4 == 0 else (), accs=[o_sb.b] if n4 else ())
                s.op("dve", lambda e: e.tensor_tensor(out=kend[:], in0=k_tm[:], in1=o_sb[:], op=ALU.mult), reads=[k_tm.b, o_sb.b], writes=[kend.b])
                s.op("dve", lambda e: e.tensor_copy(bmid[:], bT[:, :, CS // 2 - 1]), reads=[bT.b], writes=[bmid.b])
                s.op("dve", lambda e: e.tensor_tensor(out=d1[:], in0=bT[:], in1=bmid[:].unsqueeze(2).to_broadcast([128, NCH, CS]), op=ALU.subtract),
                     reads=[bT.b, bmid.b], writes=[d1.b])
                s.op("act", lambda e: e.activation(out=bT[:], in_=bT[:], func=AF.Exp), reads=[bT.b], accs=[bT.b])
                s.op("dve", lambda e: e.tensor_copy(bend[:], bT[:, :, CS - 1]), reads=[bT.b], writes=[bend.b])
                s.op("dve", lambda e: e.tensor_tensor(out=qb[:], in0=qT[:], in1=bT[:], op=ALU.mult), reads=[qT.b, bT.b], writes=[qb.b])
                s.op("act", lambda e: e.activation(out=bT[:], in_=d1[:], func=AF.Exp), reads=[d1.b], writes=[bT.b])
                s.op("dve", lambda e: e.tensor_tensor(out=qtil[:], in0=qT[:], in1=bT[:], op=ALU.mult), reads=[qT.b, bT.b], writes=[qtil.b])
                s.op("act", lambda e: e.activation(out=bT[:], in_=d1[:], func=AF.Exp, scale=-1.0), reads=[d1.b], writes=[bT.b])
                s.op("dve", lambda e: e.tensor_tensor(out=ktil[:], in0=kTf[:], in1=bT[:], op=ALU.mult), reads=[kTf.b, bT.b], writes=[ktil.b])
                for n in range(NCH):
                    gn = sg * NCH + n
                    pA = ps[pc % 8]; pc += 1
                    s.op("pe", lambda e, pA=pA, n=n: e.matmul(pA[0:CS, 0:CS], ktil[:, n, :], qtil[:, n, :], start=True, stop=True),
                         reads=[ktil.b, qtil.b], writes=[pA.b])
                    s.op("dve", lambda e, pA=pA: e.tensor_tensor(out=att[:], in0=pA[0:CS, 0:CS], in1=self.U[0:CS, 0:CS], op=ALU.mult), reads=[pA.b, self.U.b], writes=[att.b])
                    pO = ps[pc % 8]; pc += 1
                    s.op("pe", lambda e, pO=pO, n=n, gn=gn: e.matmul(pO[0:CS, 0:128], att[:], vb[:, n, :], start=True, stop=(gn == 0)),
                         reads=[att.b, vb.b], writes=[pO.b])
                    if gn > 0:
                        s.op("pe", lambda e, pO=pO, n=n: e.matmul(pO[0:CS, 0:128], qb[:, n, :], stb[:], start=False, stop=True),
                             reads=[qb.b, stb.b], accs=[pO.b])
                    s.op("act", lambda e, pO=pO, n=n: e.copy(o_sb[:, n, :], pO[0:CS, 0:128]), reads=[pO.b], writes=[o_sb.b] if n == 0 else (), accs=[o_sb.b] if n else ())
                    if gn < NCHT - 1:
                        pS = ps[pc % 8]; pc += 1
                        s.op("pe", lambda e, pS=pS, n=n: e.matmul(pS[:, 0:128], kend[:, n, :], vb[:, n, :], start=True, stop=True),
                             reads=[kend.b, vb.b], writes=[pS.b])
                        if gn == 0:
                            s.op("dve", lambda e, pS=pS: e.tensor_copy(state[:], pS[:, 0:128]), reads=[pS.b], writes=[state.b])
                        else:
                            s.op("dve", lambda e, pS=pS, n=n: e.scalar_tensor_tensor(out=state[:], in0=state[:], scalar=bend[:, n:n + 1], in1=pS[:, 0:128],
                                                                                 op0=ALU.mult, op1=ALU.add), reads=[state.b, bend.b, pS.b], accs=[state.b])
                        s.op("act", lambda e: e.copy(stb[:], state[:]), reads=[state.b], writes=[stb.b])
                s.op("dve", lambda e: e.tensor_tensor(out=lf_tm[:], in0=o_sb[:], in1=o_sb[:], op=ALU.mult), reads=[o_sb.b], writes=[lf_tm.b])
                s.op("dve", lambda e: e.tensor_reduce(out=ssq[:], in_=lf_tm[:], axis=AX.X, op=ALU.add), reads=[lf_tm.b], writes=[ssq.b])
                self.rsqrt_mean(ssq[:], ssq[:], ssq.b, ssq.b, 128, npart=CS)
                s.op("dve", lambda e: e.tensor_tensor(out=o_sb[:], in0=o_sb[:], in1=ssq[:].unsqueeze(2).to_broadcast([CS, NCH, 128]), op=ALU.mult),
                     reads=[o_sb.b, ssq.b], accs=[o_sb.b])
                s.op("dve", lambda e: e.tensor_tensor(out=o_sb[:], in0=o_sb[:], in1=hgB[:].unsqueeze(1).to_broadcast([CS, NCH, 128]), op=ALU.mult),
                     reads=[o_sb.b, hgB.b], accs=[o_sb.b])
                s.op("dve", lambda e: e.tensor_tensor(out=o_sb[:], in0=o_sb[:], in1=g_tm[:], op=ALU.mult), reads=[o_sb.b, g_tm.b], accs=[o_sb.b])
                for n8 in range(0, NCH, NPB):
                    p = ps[pc % 8]; pc += 1
                    nn = min(NPB, NCH - n8)
                    for n in range(n8, n8 + nn):
                        s.op("pe", lambda e, p=p, n=n, n8=n8: e.transpose(p[:, (n - n8) * CS:(n - n8 + 1) * CS], o_sb[:, n, :], self.identf[0:CS, 0:CS]),
                             reads=[o_sb.b, self.identf.b], writes=[p.b] if n == n8 else (), accs=[p.b] if n != n8 else ())
                    s.op("act", lambda e, p=p, n8=n8, nn=nn: e.copy(yaT[:, n8 * CS:(n8 + nn) * CS], p[:, 0:nn * CS]), reads=[p.b],
                         writes=[yaT.b] if n8 == 0 else (), accs=[yaT.b] if n8 else ())
                s.dma(self.mx["yT"][0][hs, ts], yaT[:], reads=[yaT.b], dsem=yaT.d)
            s.flush()


def build_program(cfg):
    k = K(cfg)
    D, S, DEPTH, DFF, NBE = cfg["D"], cfg["S"], cfg["DEPTH"], cfg["DFF"], cfg["NBE"]
    DIN = 8016 + 3 * D
    for name, shape in (("x", (NBE, S, D)), ("c", (NBE, D)), ("w_c_down", (D, 256)), ("w_c_up", (DEPTH, 256, 9 * D)),
                        ("norm_gains", (DEPTH, 6, D)), ("w_in", (DEPTH, D, DIN)), ("lb_logits", (DEPTH, 1024)),
                        ("hgrn_norm", (DEPTH, 128)), ("kv_norm", (DEPTH, 512)), ("w_kv_up", (DEPTH, 512, 256)),
                        ("rel_table", (32, 24)), ("sinks", (DEPTH, 16)), ("w_branch", (DEPTH, 3, 1024, D)),
                        ("w_out", (DEPTH, D, D)), ("ffn1_in", (DEPTH, D, 2 * DFF)), ("ffn1_out", (DEPTH, DFF, D)),
                        ("ffn2_in", (DEPTH, D, 2 * DFF)), ("ffn2_out", (DEPTH, DFF, D)), ("t5_onehot", (33, 766))):
        k.ext(name, shape)
    out = k.ext("out", (NBE, S, D), kind="ExternalOutput")
    xres = k.scratch("xres", (S, D))
    growd = k.scratch("grow_d", (3, 1, D))
    k.mixer_scratch()
    k.consts()
    k.mod_setup()
    k.setup_phase()
    for b in range(NBE):
        k.cond_phase(k.dram["c"][b])
        for l in range(DEPTH):
            k.mod_phase(l, growd)
            k.ffn_phase(l, 1, k.dram["x"][b] if l == 0 else xres, xres, k.AB[0], k.AB[1], growd[0])
            k.proj_phase(l, xres, k.AB[2], k.AB[3])
            k.hgrn_phase(l)
            k.dsa_phase(l)
            k.swa_phase(l)
            k.merge_phase(l, xres, xres, growd[1])
            k.ffn_phase(l, 2, xres, out[b] if l == DEPTH - 1 else xres, k.AB[4], k.AB[5], growd[2])
    return k


def t5_bucket_np(d):
    d = np.maximum(d, 0)
    df = np.maximum(d, 1).astype(np.float32)
    large = 16 + (np.log(df / np.float32(16)) / np.float32(math.log(128 / 16)) * np.float32(16)).astype(np.int32)
    large = np.minimum(large, 31)
    return np.where(d < 16, d, large)
def t5_onehot():
    oh = np.zeros((33, 2, 383), np.float32)
    for i in range(383):
        dist = 255 - i
        b = int(t5_bucket_np(np.array([dist]))[0])
        if 0 <= dist < 128:
            oh[b, 0, i] = 1
        else:
            oh[32, 0, i] = 1
        if dist >= 0:
            oh[b, 1, i] = 1
        else:
            oh[32, 1, i] = 1
    return oh.reshape(33, 766)


N_CORES = 8


def run_cores(inputs, n_cores, batch_ids):
    cfg = dict(FULL)
    cfg["NBE"] = len(batch_ids[0])
    Sched.SERIAL = True
    prog = build_program(cfg)
    oh = t5_onehot()
    shared = {kk: np.ascontiguousarray(v, dtype=np.float32) for kk, v in inputs.items() if kk not in ("x", "c")}
    in_maps = []
    for ids in batch_ids:
        m = dict(shared)
        m["x"] = np.ascontiguousarray(inputs["x"][ids], dtype=np.float32)
        m["c"] = np.ascontiguousarray(inputs["c"][ids], dtype=np.float32)
        m["t5_onehot"] = oh
        in_maps.append(m)
    res = run_bass_kernel_spmd(prog.nc, in_maps, core_ids=list(range(n_cores)))
    return [np.asarray(r["out"]) for r in res.results]


def kernel(**inputs):
    B = inputs["x"].shape[0]
    per = B // N_CORES
    batch_ids = [list(range(c * per, (c + 1) * per)) for c in range(N_CORES)]
    outs = run_cores(inputs, N_CORES, batch_ids)
    return np.concatenate(outs, axis=0).astype(np.float32)
```

```python
import math
import contextlib
import numpy as np
import concourse.bass as bass
import concourse.mybir as mybir
from concourse.bass_utils import run_bass_kernel_spmd

DT = mybir.dt
F32, BF16 = DT.float32, DT.bfloat16
ALU = mybir.AluOpType
AF = mybir.ActivationFunctionType
AX = mybir.AxisListType

FULL = dict(D=4096, B=8, S=2048, DEPTH=4, DFF=4096, TOPK=256, NCORES=8)


class DSem:
    def __init__(self, sem):
        self.sem = sem
        self.count = 0


class Buf:
    def __init__(self, name):
        self.name = name
        self.writes = {}
        self.reads = {}


ENGS = ("pe", "act", "dve", "pool", "sp")


class Sched:
    SERIAL = False
    DMA_WINDOW = 8

    def __init__(self, nc, stack):
        self.nc = nc
        self.stack = stack
        self.h = dict(pe=nc.tensor, act=nc.scalar, dve=nc.vector, pool=nc.gpsimd, sp=nc.sync)
        self.q = {e: [] for e in ENGS}
        self.sem = {e: stack.enter_context(nc.semaphore("c_" + e)) for e in ENGS if e != "sp"}
        self.cnt = {e: 0 for e in ENGS}
        self.waited = {e: {} for e in ENGS}
        self.dsems = []
        self.dnext = 0
        self.ninst = 0
        self.dma_hist = []

    def get_dsem(self):
        if self.dnext == len(self.dsems):
            self.dsems.append(DSem(self.stack.enter_context(self.nc.semaphore("d%d" % self.dnext))))
        self.dnext += 1
        return self.dsems[self.dnext - 1]

    def _wait(self, eng, key, val):
        if isinstance(key, DSem):
            val = key.count
            sem = key.sem
        else:
            if key == eng and eng in ("pe", "sp"):
                return
            sem = self.sem[key]
        w = self.waited[eng]
        if w.get(key, 0) >= val:
            return
        w[key] = val
        h = self.h[eng]
        self.q[eng].append(lambda e, sem=sem, val=val: e.wait_ge(sem, val))

    def op(self, eng, fn, reads=(), writes=(), accs=(), dsem=None):
        deps = []
        for b in reads:
            deps.extend(b.writes.items())
        for b in writes:
            deps.extend(b.writes.items())
            deps.extend(b.reads.items())
        for b in accs:
            deps.extend(b.reads.items())
        is_dma = dsem is not None
        if Sched.SERIAL is True or (Sched.SERIAL and eng in Sched.SERIAL):
            for k2 in ENGS:
                if k2 != "sp" and self.cnt[k2] > 0:
                    self._wait(eng, k2, self.cnt[k2])
            for d in self.dsems:
                if d.count:
                    self._wait(eng, d, d.count)
        for key, val in deps:
            if not is_dma and key == eng:
                if not any(key in b.writes for b in reads):
                    continue
            self._wait(eng, key, val)
        h = self.h[eng]
        if is_dma:
            if len(self.dma_hist) >= Sched.DMA_WINDOW:
                self._wait(eng, self.dma_hist[-Sched.DMA_WINDOW], 0)
            self.dma_hist.append(dsem)
            del self.dma_hist[:-32]
            dsem.count += 16
            tok = (dsem, dsem.count)
            sem, inc = dsem.sem, 16
        else:
            self.cnt[eng] += 1
            tok = (eng, self.cnt[eng])
            sem, inc = self.sem[eng], 1
        self.q[eng].append(lambda e, fn=fn, sem=sem, inc=inc: fn(e).then_inc(sem, inc))
        self.ninst += 1
        for b in reads:
            b.reads[tok[0]] = tok[1]
        for b in writes:
            b.writes = {tok[0]: tok[1]}
            b.reads = {}
        for b in accs:
            b.writes[tok[0]] = tok[1]
            b.reads = {}

    def dma(self, out, in_, reads=(), writes=(), accs=(), dsem=None, eng="sp", **kw):
        self.op(eng, lambda h: h.dma_start(out=out, in_=in_, **kw), reads=reads, writes=writes,
                accs=accs, dsem=dsem)

    def barrier(self):
        for e in ENGS:
            for k in ENGS:
                if k != "sp" and k != e and self.cnt[k] > 0:
                    self._wait(e, k, self.cnt[k])
            for d in self.dsems:
                if d.count:
                    self._wait(e, d, d.count)

    def flush(self):
        self.barrier()
        with self.nc.Block() as block:
            self._flush(block)
        self.q = {e: [] for e in ENGS}
        self.dnext = 0

    def _flush(self, block):
        q = self.q

        @block.tensor
        def _(e):
            for f in q["pe"]:
                f(e)

        @block.scalar
        def _(e):
            for f in q["act"]:
                f(e)

        @block.vector
        def _(e):
            for f in q["dve"]:
                f(e)

        @block.gpsimd
        def _(e):
            for f in q["pool"]:
                f(e)

        @block.sync
        def _(e):
            for f in q["sp"]:
                f(e)


class T:
    uid = 0

    def __init__(self, sched, stack, name, shape, dtype, psum=False, dma=False):
        nc = sched.nc
        alloc = nc.psum_tensor if psum else nc.sbuf_tensor
        T.uid += 1
        name = "%s_%d" % (name, T.uid)
        self.t = stack.enter_context(alloc(name, shape, dtype))
        self.b = Buf(name)
        self.d = sched.get_dsem() if dma else None

    def __getitem__(self, idx):
        return self.t[idx]


class K:
    def __init__(self, cfg):
        self.cfg = cfg
        self.nc = bass.Bass("TRN2", target_bir_lowering=False)
        self.stack = contextlib.ExitStack()
        self.s = Sched(self.nc, self.stack)
        self.dram = {}

    def ext(self, name, shape, kind="ExternalInput"):
        self.dram[name] = self.nc.dram_tensor(name, list(shape), F32, kind=kind).ap()
        return self.dram[name]

    def scratch(self, name, shape, dtype=F32):
        if name in self.cfg.get("dbg", ()):
            ap = self.nc.dram_tensor(name, list(shape), dtype, kind="ExternalOutput").ap()
        elif name in self.cfg.get("ext_in", ()):
            ap = self.nc.dram_tensor(name, list(shape), dtype, kind="ExternalInput").ap()
        else:
            ap = self.nc.dram_tensor(name, list(shape), dtype).ap()
        self.dram[name] = ap
        return ap

    def consts(self):
        s, st = self.s, self.stack
        self.identb = T(s, st, "identb", [128, 128], BF16)
        self.identf = T(s, st, "identf", [128, 128], F32)
        self.ones_row = T(s, st, "ones_row", [1, 128], F32)
        s.op("pool", lambda e: e.memset(self.identf[:], 0.0), writes=[self.identf.b])
        s.op("pool", lambda e: e.affine_select(out=self.identf[:], in_=self.identf[:], pattern=[[-1, 128]],
                                               compare_op=ALU.not_equal, fill=1.0, base=0, channel_multiplier=1),
             reads=[self.identf.b], accs=[self.identf.b])
        s.op("dve", lambda e: e.tensor_copy(self.identb[:], self.identf[:]), reads=[self.identf.b], writes=[self.identb.b])
        s.op("pool", lambda e: e.memset(self.ones_row[:], 1.0), writes=[self.ones_row.b])
        self.eps = T(s, st, "eps", [128, 1], F32)
        s.op("pool", lambda e: e.memset(self.eps[:], 1e-6), writes=[self.eps.b])
        s.flush()

    def scramble(self):
        s = self.s
        with contextlib.ExitStack() as ph:
            big = T(s, ph, "scr", [128, 40000], F32)
            ps = [T(s, ph, "scrp%d" % i, [128, 512], F32, psum=True) for i in range(8)]
            s.op("pool", lambda e: e.memset(big[:, 0:20000], 12345.0), writes=[big.b])
            s.op("dve", lambda e: e.memset(big[:, 20000:40000], -54321.0), accs=[big.b])
            for p in ps:
                s.op("dve", lambda e, p=p: e.memset(p[:], 777.0), writes=[p.b])
            s.flush()

    class WStream:
        ncast = 0

        def __init__(self, k, ph, nbuf=3, kc=4, n=512):
            self.k = k
            s = k.s
            self.raw = [T(s, ph, "wraw%d" % i, [128, kc, n], F32, dma=True) for i in range(nbuf)]
            self.bf = [T(s, ph, "wbf%d" % i, [128, kc, n], BF16) for i in range(nbuf)]
            self.i = 0
            self.pending = []
            self.loaded = 0
            self.nbuf = nbuf

        def plan(self, aps):
            self.pending = list(aps)
            self.issued = 0
            self.taken = 0
            for _ in range(min(self.nbuf - 1, len(self.pending))):
                self._issue()

        def _issue(self):
            ap = self.pending[self.issued]
            j = (self.i + self.issued) % self.nbuf
            raw = self.raw[j]
            kk, nn = ap.shape
            kc = kk // 128
            self.k.s.dma(raw[:, 0:kc, 0:nn], ap.rearrange("(c p) n -> p c n", p=128), writes=[raw.b], dsem=raw.d)
            self.issued += 1

        def next(self):
            s = self.k.s
            if self.issued < len(self.pending):
                self._issue()
            ap = self.pending[self.taken]
            j = (self.i + self.taken) % self.nbuf
            raw, bf = self.raw[j], self.bf[j]
            kk, nn = ap.shape
            kc = kk // 128
            K.WStream.ncast += 1
            if K.WStream.ncast % 2:
                s.op("dve", lambda e: e.tensor_copy(bf[:, 0:kc, 0:nn], raw[:, 0:kc, 0:nn]), reads=[raw.b], writes=[bf.b])
            else:
                s.op("act", lambda e: e.copy(bf[:, 0:kc, 0:nn], raw[:, 0:kc, 0:nn]), reads=[raw.b], writes=[bf.b])
            self.taken += 1
            if self.taken == len(self.pending):
                self.i = (self.i + self.taken) % self.nbuf
            return bf, kc, nn


    def norm_block(self, xin, tok0, NT, BIG, aT, hT, xn, ss, rstd, ps, A, B, pctr):
        s, D = self.s, self.cfg["D"]
        KC = D // 128
        if True:
            if True:
                for t in range(NT):
                    s.dma(BIG[:, t, :], xin[tok0 + t * 128: tok0 + (t + 1) * 128, :], accs=[BIG.b] if t else (),
                          writes=() if t else [BIG.b], dsem=BIG.d)
                for t in range(NT):
                    s.op("dve", lambda e, t=t: e.tensor_tensor(out=xn, in0=BIG[:, t, :], in1=BIG[:, t, :], op=ALU.mult),
                         reads=[BIG.b], writes=[aT.b])
                    s.op("dve", lambda e, t=t: e.reduce_sum(out=ss[:, t:t + 1], in_=xn, axis=AX.X),
                         reads=[aT.b], writes=[ss.b] if t == 0 else (), accs=[ss.b] if t else ())
                    self.rsqrt_mean(rstd[:, t:t + 1], ss[:, t:t + 1], rstd.b, ss.b, D)
                    s.op("dve", lambda e, t=t: e.tensor_scalar(out=xn, in0=BIG[:, t, :], scalar1=rstd[:, t:t + 1], scalar2=None,
                                                          op0=ALU.mult), reads=[BIG.b, rstd.b], writes=[aT.b])
                    for g in range(0, KC, 8):
                        p = ps[pctr % 8]
                        pctr += 1
                        pb = p.t[:].bitcast(BF16)
                        nch = min(8, KC - g)
                        for c in range(nch):
                            s.op("pe", lambda e, c=c, g=g, pb=pb: e.transpose(pb[:, c * 128:(c + 1) * 128], xn[:, (g + c) * 128:(g + c + 1) * 128], self.identb[:]),
                                 reads=[aT.b, self.identb.b], writes=[p.b] if c == 0 else (), accs=[p.b] if c else ())
                        for c in range(nch):
                            s.op("act", lambda e, c=c, g=g, pb=pb, t=t: e.activation(out=hT[:, g + c, t * 128:(t + 1) * 128], in_=pb[:, c * 128:(c + 1) * 128],
                                                                                 func=AF.Identity, scale=A[:, g + c:g + c + 1], bias=B[:, g + c:g + c + 1]),
                                 reads=[p.b, A.b, B.b], writes=[hT.b] if (t == 0 and g == 0 and c == 0) else (),
                                 accs=() if (t == 0 and g == 0 and c == 0) else [hT.b])
        return pctr

    def ffn_phase(self, l, which, xin, xout, A, B, Gsrc):
        cfg, s = self.cfg, self.s
        D, DFF, S = cfg["D"], cfg["DFF"], cfg["S"]
        KC, FC = D // 128, DFF // 128
        TB = min(512, S)
        NT = TB // 128
        w_in = self.dram["ffn%d_in" % which][l]
        w_out = self.dram["ffn%d_out" % which][l]
        with contextlib.ExitStack() as ph:
            hT = T(s, ph, "hT", [128, KC, TB], BF16, dma=True)
            aT = T(s, ph, "aT", [128, FC, TB], BF16)
            BIG = T(s, ph, "BIG", [128, NT, D], F32, dma=True)
            G = T(s, ph, "G", [128, D], F32)
            grow = T(s, ph, "grow", [1, D], F32, dma=True)
            ss = T(s, ph, "ss", [128, NT], F32)
            rstd = T(s, ph, "rstd", [128, NT], F32)
            ssy = T(s, ph, "ssy", [128, NT, D // 512], F32)
            junk = T(s, ph, "junk", [128, 512], F32)
            su = [T(s, ph, "su%d" % i, [128, TB], F32) for i in range(2)]
            ps = [T(s, ph, "ps%d" % i, [128, 512], F32, psum=True) for i in range(8)]
            ws = K.WStream(self, ph)
            xn_v = aT.t[:, 0:KC // NT if False else FC, :]
            xn = aT.t[:].rearrange("p c t -> p (c t)")[:, 0:D]
            xh = hT.t[:].rearrange("p c t -> p (c t)").bitcast(F32)[:, 0:D]
            s.dma(grow[:], Gsrc, writes=[grow.b], dsem=grow.d)
            for n in range(D // 512):
                p = ps[n % 8]
                s.op("pe", lambda e, p=p, n=n: e.matmul(p[:], self.ones_row[:], grow[:, n * 512:(n + 1) * 512], start=True, stop=True),
                     reads=[grow.b, self.ones_row.b], writes=[p.b])
                s.op("act", lambda e, p=p, n=n: e.copy(G[:, n * 512:(n + 1) * 512], p[:]), reads=[p.b], accs=[G.b])
            if "dbg_G" in cfg.get("dbg", ()):
                dd = self.scratch("dbg_G", (128, D))
                s.dma(dd, G[:], reads=[G.b], dsem=BIG.d)
            pctr = 0
            for tb in range(S // TB):
                tok0 = tb * TB
                pctr = self.norm_block(xin, tok0, NT, BIG, aT, hT, xn, ss, rstd, ps, A, B, pctr)
                for fg in range(DFF // 512):
                    aps = []
                    for half in range(2):
                        for ks in range(D // 512):
                            aps.append(w_in[ks * 512:(ks + 1) * 512, half * DFF + fg * 512: half * DFF + (fg + 1) * 512])
                    ws.plan(aps)
                    for half in range(2):
                        for ks in range(D // 512):
                            bf, kc, nn = ws.next()
                            for k4 in range(kc):
                                kk = ks * 4 + k4
                                for c in range(4):
                                    p = ps[half * 4 + c]
                                    first = kk == 0
                                    s.op("pe", lambda e, p=p, bf=bf, k4=k4, c=c, kk=kk: e.matmul(p[:, 0:TB], bf[:, k4, c * 128:(c + 1) * 128], hT[:, kk, :],
                                                                                              start=(kk == 0), stop=(kk == KC - 1)),
                                         reads=[bf.b, hT.b], writes=[p.b] if first else (), accs=() if first else [p.b])
                    for c in range(4):
                        sb = su[c % 2]
                        s.op("act", lambda e, c=c, sb=sb: e.activation(out=sb[:], in_=ps[c][:, 0:TB], func=AF.Silu), reads=[ps[c].b], writes=[sb.b])
                        first = (fg == 0 and c == 0)
                        s.op("dve", lambda e, c=c, sb=sb, fg=fg: e.tensor_tensor(out=aT[:, fg * 4 + c, :], in0=sb[:], in1=ps[4 + c][:, 0:TB], op=ALU.mult),
                             reads=[sb.b, ps[4 + c].b], writes=[aT.b] if first else (), accs=() if first else [aT.b])
                for ng in range(D // 512):
                    ws.plan([w_out[ks * 512:(ks + 1) * 512, ng * 512:(ng + 1) * 512] for ks in range(DFF // 512)])
                    pb0 = (ng % 2) * 4
                    for ks in range(DFF // 512):
                        bf, kc, nn = ws.next()
                        for k4 in range(kc):
                            kk = ks * 4 + k4
                            for t in range(NT):
                                p = ps[pb0 + t]
                                first = kk == 0
                                s.op("pe", lambda e, p=p, bf=bf, k4=k4, t=t, kk=kk: e.matmul(p[:], aT[:, kk, t * 128:(t + 1) * 128], bf[:, k4, :],
                                                                                          start=(kk == 0), stop=(kk == FC - 1)),
                                     reads=[bf.b, aT.b], writes=[p.b] if first else (), accs=() if first else [p.b])
                    for t in range(NT):
                        p = ps[pb0 + t]
                        first = (ng == 0 and t == 0)
                        s.op("act", lambda e, p=p, t=t, ng=ng: e.copy(BIG[:, t, ng * 512:(ng + 1) * 512], p[:]), reads=[p.b],
                             writes=[BIG.b] if first else (), accs=() if first else [BIG.b])
                        s.op("dve", lambda e, t=t, ng=ng: e.tensor_tensor(out=junk[:], in0=BIG[:, t, ng * 512:(ng + 1) * 512],
                                                                       in1=BIG[:, t, ng * 512:(ng + 1) * 512], op=ALU.mult),
                             reads=[BIG.b], writes=[junk.b])
                        s.op("dve", lambda e, t=t, ng=ng: e.reduce_sum(out=ssy[:, t, ng:ng + 1], in_=junk[:], axis=AX.X),
                             reads=[junk.b], accs=[ssy.b])
                if "dbg_y" in cfg.get("dbg", ()) and tb == 0:
                    dd = self.scratch("dbg_y", (128, NT * D))
                    s.dma(dd, BIG[:].rearrange("p t d -> p (t d)"), reads=[BIG.b], dsem=BIG.d)
                    dd2 = self.scratch("dbg_a", (128, FC * TB), BF16)
                    s.dma(dd2, aT[:].rearrange("p c t -> p (c t)"), reads=[aT.b], dsem=BIG.d)
                for t in range(NT):
                    s.op("dve", lambda e, t=t: e.reduce_sum(out=ss[:, t:t + 1], in_=ssy[:, t, :], axis=AX.X), reads=[ssy.b], accs=[ss.b])
                    self.rsqrt_mean(rstd[:, t:t + 1], ss[:, t:t + 1], rstd.b, ss.b, D)
                    s.dma(xh, xin[tok0 + t * 128: tok0 + (t + 1) * 128, :], writes=[hT.b], dsem=hT.d)
                    s.op("dve", lambda e, t=t: e.scalar_tensor_tensor(out=BIG[:, t, :], in0=BIG[:, t, :], scalar=rstd[:, t:t + 1], in1=G[:],
                                                                 op0=ALU.mult, op1=ALU.mult), reads=[BIG.b, rstd.b, G.b], accs=[BIG.b])
                    s.op("dve", lambda e, t=t: e.tensor_tensor(out=BIG[:, t, :], in0=BIG[:, t, :], in1=xh, op=ALU.add),
                         reads=[BIG.b, hT.b], accs=[BIG.b])
                    s.dma(xout[tok0 + t * 128: tok0 + (t + 1) * 128, :], BIG[:, t, :], reads=[BIG.b], dsem=BIG.d)
            s.flush()


    def merge_phase(self, l, xin, xout, Gsrc):
        cfg, s = self.cfg, self.s
        D, S = cfg["D"], cfg["S"]
        KC = D // 128
        TB = min(512, S)
        NT = TB // 128
        w_out = self.dram["w_out"][l]
        w_br = self.dram["w_branch"][l]
        gT = self.mx["gT"]
        yT = self.mx["yT"]
        with contextlib.ExitStack() as ph:
            mT = T(s, ph, "mT", [128, KC, TB], BF16)
            BIG = T(s, ph, "BIG", [128, NT, D], F32, dma=True)
            G = T(s, ph, "G", [128, D], F32)
            grow = T(s, ph, "grow", [1, D], F32, dma=True)
            ss = T(s, ph, "ss", [128, NT], F32)
            rstd = T(s, ph, "rstd", [128, NT], F32)
            ssy = T(s, ph, "ssy", [128, NT, D // 512], F32)
            junk = T(s, ph, "junk", [128, 512], F32)
            ybuf = T(s, ph, "ybuf", [128, 3, 8, TB], BF16, dma=True)
            gts = [T(s, ph, "gts%d" % i, [128, TB], F32, dma=True) for i in range(4)]
            macc = T(s, ph, "macc", [128, 4, TB], F32)
            tmp = T(s, ph, "tmp", [128, TB], F32)
            ps = [T(s, ph, "ps%d" % i, [128, 512], F32, psum=True) for i in range(8)]
            ws = K.WStream(self, ph, nbuf=2)
            xh = ybuf.t[:].rearrange("p a c t -> p (a c t)").bitcast(F32)[:, 0:D]
            s.dma(grow[:], Gsrc, writes=[grow.b], dsem=grow.d)
            for n in range(D // 512):
                p = ps[n % 8]
                s.op("pe", lambda e, p=p, n=n: e.matmul(p[:], self.ones_row[:], grow[:, n * 512:(n + 1) * 512], start=True, stop=True),
                     reads=[grow.b, self.ones_row.b], writes=[p.b])
                s.op("act", lambda e, p=p, n=n: e.copy(G[:, n * 512:(n + 1) * 512], p[:]), reads=[p.b], accs=[G.b])
            gi = 0
            grp = 0
            for tb in range(S // TB):
                tok0 = tb * TB
                for br in range(3):
                    s.dma(ybuf[:, br, :, :], yT[br].rearrange("(c p) s -> p c s", p=128)[:, :, tok0:tok0 + TB],
                          writes=[ybuf.b] if br == 0 else (), accs=[ybuf.b] if br else (), dsem=ybuf.d)
                for dg in range(D // 512):
                    for br in range(3):
                        ws.plan([w_br[br, ks * 512:(ks + 1) * 512, dg * 512:(dg + 1) * 512] for ks in range(2)])
                        pb0 = (grp % 2) * 4
                        grp += 1
                        for ks in range(2):
                            bf, kc, nn = ws.next()
                            for k4 in range(kc):
                                kk = ks * 4 + k4
                                for c in range(4):
                                    p = ps[pb0 + c]
                                    first = kk == 0
                                    s.op("pe", lambda e, p=p, bf=bf, k4=k4, c=c, kk=kk, br=br: e.matmul(
                                        p[:, 0:TB], bf[:, k4, c * 128:(c + 1) * 128], ybuf[:, br, kk, :], start=(kk == 0), stop=(kk == 7)),
                                        reads=[bf.b, ybuf.b], writes=[p.b] if first else (), accs=() if first else [p.b])
                        for c in range(4):
                            p = ps[pb0 + c]
                            dch = dg * 4 + c
                            gt = gts[gi % 4]
                            gi += 1
                            s.dma(gt[:], gT[br * D + dch * 128: br * D + (dch + 1) * 128, tok0:tok0 + TB], writes=[gt.b], dsem=gt.d)
                            if br == 0:
                                s.op("dve", lambda e, p=p, c=c, gt=gt: e.tensor_tensor(out=macc[:, c, :], in0=p[:, 0:TB], in1=gt[:], op=ALU.mult),
                                     reads=[p.b, gt.b], writes=[macc.b] if c == 0 else (), accs=[macc.b] if c else ())
                            else:
                                s.op("dve", lambda e, p=p, gt=gt: e.tensor_tensor(out=tmp[:], in0=p[:, 0:TB], in1=gt[:], op=ALU.mult),
                                     reads=[p.b, gt.b], writes=[tmp.b])
                                s.op("dve", lambda e, c=c: e.tensor_tensor(out=macc[:, c, :], in0=macc[:, c, :], in1=tmp[:], op=ALU.add),
                                     reads=[macc.b, tmp.b], accs=[macc.b])
                            if br == 2:
                                first = (dg == 0 and c == 0)
                                s.op("act", lambda e, c=c, dch=dch: e.copy(mT[:, dch, :], macc[:, c, :]), reads=[macc.b],
                                     writes=[mT.b] if first else (), accs=() if first else [mT.b])
                for ng in range(D // 512):
                    ws.plan([w_out[ks * 512:(ks + 1) * 512, ng * 512:(ng + 1) * 512] for ks in range(D // 512)])
                    pb0 = (ng % 2) * 4
                    for ks in range(D // 512):
                        bf, kc, nn = ws.next()
                        for k4 in range(kc):
                            kk = ks * 4 + k4
                            for t in range(NT):
                                p = ps[pb0 + t]
                                first = kk == 0
                                s.op("pe", lambda e, p=p, bf=bf, k4=k4, t=t, kk=kk: e.matmul(p[:], mT[:, kk, t * 128:(t + 1) * 128], bf[:, k4, :],
                                                                                          start=(kk == 0), stop=(kk == KC - 1)),
                                     reads=[bf.b, mT.b], writes=[p.b] if first else (), accs=() if first else [p.b])
                    for t in range(NT):
                        p = ps[pb0 + t]
                        first = (ng == 0 and t == 0)
                        s.op("act", lambda e, p=p, t=t, ng=ng: e.copy(BIG[:, t, ng * 512:(ng + 1) * 512], p[:]), reads=[p.b],
                             writes=[BIG.b] if first else (), accs=() if first else [BIG.b])
                        s.op("dve", lambda e, t=t, ng=ng: e.tensor_tensor(out=junk[:], in0=BIG[:, t, ng * 512:(ng + 1) * 512],
                                                                       in1=BIG[:, t, ng * 512:(ng + 1) * 512], op=ALU.mult),
                             reads=[BIG.b], writes=[junk.b])
                        s.op("dve", lambda e, t=t, ng=ng: e.reduce_sum(out=ssy[:, t, ng:ng + 1], in_=junk[:], axis=AX.X),
                             reads=[junk.b], accs=[ssy.b])
                for t in range(NT):
                    s.op("dve", lambda e, t=t: e.reduce_sum(out=ss[:, t:t + 1], in_=ssy[:, t, :], axis=AX.X), reads=[ssy.b], accs=[ss.b])
                    self.rsqrt_mean(rstd[:, t:t + 1], ss[:, t:t + 1], rstd.b, ss.b, D)
                    s.dma(xh, xin[tok0 + t * 128: tok0 + (t + 1) * 128, :], writes=[ybuf.b], dsem=ybuf.d)
                    s.op("dve", lambda e, t=t: e.scalar_tensor_tensor(out=BIG[:, t, :], in0=BIG[:, t, :], scalar=rstd[:, t:t + 1], in1=G[:],
                                                                 op0=ALU.mult, op1=ALU.mult), reads=[BIG.b, rstd.b, G.b], accs=[BIG.b])
                    s.op("dve", lambda e, t=t: e.tensor_tensor(out=BIG[:, t, :], in0=BIG[:, t, :], in1=xh, op=ALU.add),
                         reads=[BIG.b, ybuf.b], accs=[BIG.b])
                    s.dma(xout[tok0 + t * 128: tok0 + (t + 1) * 128, :], BIG[:, t, :], reads=[BIG.b], dsem=BIG.d)
            s.flush()

    def rsqrt_mean(self, out, ssq, outb, ssb, n, npart=128):
        s = self.s
        s.op("act", lambda e: e.activation(out=out, in_=ssq, func=AF.Sqrt, scale=1.0 / n, bias=self.eps[0:npart, 0:1]), reads=[ssb, self.eps.b], accs=[outb])
        s.op("dve", lambda e: e.reciprocal(out, out), reads=[outb], accs=[outb])

    def colload(self, ph, dst_ap, src_vec, C, ps, name):
        s = self.s
        tmp = T(s, ph, "cl_" + name, [C, 128], F32, dma=True)
        s.dma(tmp[:], src_vec.rearrange("(c p) -> c p", p=128), writes=[tmp.b], dsem=tmp.d)
        s.op("pe", lambda e: e.transpose(ps[:, 0:C], tmp[:], self.identf[0:C, 0:C]), reads=[tmp.b, self.identf.b], writes=[ps.b])
        return ps

    def mod_setup(self):
        s, st, cfg = self.s, self.stack, self.cfg
        KC = cfg["D"] // 128
        self.AB = [T(s, st, "AB%d" % i, [128, KC], F32) for i in range(6)]
        self.condT = T(s, st, "condT", [128, 2], F32)

    def cond_phase(self, cvec):
        s, cfg = self.s, self.cfg
        D = cfg["D"]
        KC = D // 128
        wcd = self.dram["w_c_down"]
        with contextlib.ExitStack() as ph:
            ps = [T(s, ph, "ps%d" % i, [128, 512], F32, psum=True) for i in range(2)]
            cT = T(s, ph, "cT", [128, KC], F32)
            w = T(s, ph, "wcd", [128, KC, 256], F32, dma=True)
            self.colload(ph, None, cvec, KC, ps[0], "c")
            s.op("dve", lambda e: e.tensor_copy(cT[:], ps[0][:, 0:KC]), reads=[ps[0].b], writes=[cT.b])
            s.dma(w[:], wcd.rearrange("(c p) r -> p c r", p=128), writes=[w.b], dsem=w.d)
            for rc in range(2):
                for c in range(KC):
                    s.op("pe", lambda e, rc=rc, c=c: e.matmul(ps[1][:, rc:rc + 1], w[:, c, rc * 128:(rc + 1) * 128], cT[:, c:c + 1],
                                                         start=(c == 0), stop=(c == KC - 1)),
                         reads=[w.b, cT.b], writes=[ps[1].b] if (c == 0 and rc == 0) else (), accs=() if (c == 0 and rc == 0) else [ps[1].b])
            s.op("act", lambda e: e.activation(out=self.condT[:], in_=ps[1][:, 0:2], func=AF.Silu), reads=[ps[1].b], writes=[self.condT.b])
            s.flush()

    def mod_phase(self, l, grow_dram):
        s, cfg = self.s, self.cfg
        D = cfg["D"]
        KC = D // 128
        wcu = self.dram["w_c_up"][l]
        gains = self.dram["norm_gains"][l]
        PW = min(1024, D)
        with contextlib.ExitStack() as ph:
            ps = [T(s, ph, "ps%d" % i, [128, 512], F32, psum=True) for i in range(4)]
            piece = [T(s, ph, "wcu%d" % i, [128, 2, PW], F32, dma=True) for i in range(2)]
            gcol = T(s, ph, "gcol", [128, KC], F32)
            grow = T(s, ph, "grow", [1, D], F32, dma=True)
            gain_row = T(s, ph, "gain_row", [1, D], F32, dma=True)
            pi = 0
            for sub in range(3):
                self.colload(ph, None, gains[2 * sub], KC, ps[0], "g%d" % sub)
                s.op("dve", lambda e: e.tensor_copy(gcol[:], ps[0][:, 0:KC]), reads=[ps[0].b], writes=[gcol.b])
                for which in range(3):
                    m = 3 * sub + which
                    if which == 2:
                        s.dma(gain_row[:], gains[2 * sub + 1].rearrange("(o d) -> o d", o=1), writes=[gain_row.b], dsem=gain_row.d)
                    for pc in range(D // PW):
                        pt = piece[pi % 2]
                        pi += 1
                        s.dma(pt[:], wcu[:, m * D + pc * PW: m * D + (pc + 1) * PW].rearrange("(c p) n -> p c n", p=128),
                              writes=[pt.b], dsem=pt.d)
                        if which < 2:
                            pcol = ps[1 + which]
                            for cc in range(PW // 128):
                                c = pc * (PW // 128) + cc
                                for rc in range(2):
                                    first = (c == 0 and rc == 0)
                                    s.op("pe", lambda e, pt=pt, rc=rc, cc=cc, c=c, pcol=pcol: e.matmul(
                                        pcol[:, c:c + 1], pt[:, rc, cc * 128:(cc + 1) * 128], self.condT[:, rc:rc + 1], start=(rc == 0), stop=(rc == 1)),
                                        reads=[pt.b, self.condT.b], writes=[pcol.b] if first else (), accs=() if first else [pcol.b])
                        else:
                            for n in range(PW // 512):
                                for rc in range(2):
                                    s.op("pe", lambda e, pt=pt, rc=rc, n=n: e.matmul(ps[3][0:1, :], self.condT[:, rc:rc + 1], pt[:, rc, n * 512:(n + 1) * 512],
                                                                                 start=(rc == 0), stop=(rc == 1)),
                                         reads=[pt.b, self.condT.b], writes=[ps[3].b] if rc == 0 else (), accs=[ps[3].b] if rc else ())
                                col0 = pc * PW + n * 512
                                res = 0.5 if sub != 1 else 1.0
                                s.op("dve", lambda e, col0=col0, res=res: e.scalar_tensor_tensor(
                                    out=grow[:, col0:col0 + 512], in0=ps[3][0:1, :], scalar=res, in1=gain_row[:, col0:col0 + 512],
                                    op0=ALU.mult, op1=ALU.mult), reads=[ps[3].b, gain_row.b], accs=[grow.b])
                    if which == 0:
                        s.op("dve", lambda e, sub=sub: e.tensor_copy(self.AB[2 * sub + 1][:], ps[1][:, 0:KC]), reads=[ps[1].b], writes=[self.AB[2 * sub + 1].b])
                    elif which == 1:
                        s.op("dve", lambda e, sub=sub: e.scalar_tensor_tensor(out=self.AB[2 * sub][:], in0=ps[2][:, 0:KC], scalar=1.0, in1=gcol[:],
                                                                         op0=ALU.add, op1=ALU.mult),
                             reads=[ps[2].b, gcol.b], writes=[self.AB[2 * sub].b])
                    else:
                        s.dma(grow_dram[sub], grow[:], reads=[grow.b], dsem=grow.d)
            s.flush()

    def mixer_scratch(self):
        cfg = self.cfg
        S, D = cfg["S"], cfg["D"]
        sc = self.scratch
        self.mx = dict(
            aqT=sc("aqT", (1024, S)), afT=sc("afT", (1024, S)), af=sc("af", (S, 1024)), ai=sc("ai", (S, 1024)),
            ag=sc("ag", (S, 1024)), bqT=sc("bqT", (1024, S)), blat=sc("blat", (S, 512)), biqT=sc("biqT", (1024, S)),
            bikT=sc("bikT", (64, S)), biw=sc("biw", (S, 16)), cqT=sc("cqT", (1024, S)), ckT=sc("ckT", (128, S)),
            cv=sc("cv", (S, 128)), gT=sc("gT", (3 * D, S)),
            yT=sc("yT", (3, 1024, S), BF16),
        )

    def proj_phase(self, l, xin, A, B):
        cfg, s = self.cfg, self.s
        D, S = cfg["D"], cfg["S"]
        KC = D // 128
        TB = min(512, S)
        NT = TB // 128
        w_in = self.dram["w_in"][l]
        mx = self.mx
        jobs = []
        col = 0
        for name, ncols, modes in (("aq", 1024, ("fm",)), ("af", 1024, ("fm", "tm")), ("ai", 1024, ("tm",)), ("ag", 1024, ("tm",)),
                                   ("bq", 1024, ("fm",)), ("blat", 512, ("tm",)), ("biq", 1024, ("fm",)), ("bik", 64, ("fm",)),
                                   ("biw", 16, ("tm",)), ("cq", 1024, ("fm",)), ("ck", 128, ("fm",)), ("cv", 128, ("tm",)),
                                   ("g", 3 * D, ("fm",))):
            for m in modes:
                dst = mx[name + "T"] if m == "fm" else mx[name]
                jobs.append((col, ncols, m, dst, AF.Sigmoid if name == "g" else AF.Identity))
            col += ncols
        with contextlib.ExitStack() as ph:
            hT = T(s, ph, "hT", [128, KC, TB], BF16)
            xnT = T(s, ph, "xnT", [128, D], BF16)
            BIG = T(s, ph, "BIG", [128, NT, D], F32, dma=True)
            ss = T(s, ph, "ss", [128, NT], F32)
            rstd = T(s, ph, "rstd", [128, NT], F32)
            stage = [T(s, ph, "stg%d" % i, [128, 512], F32, dma=True) for i in range(4)]
            ps = [T(s, ph, "ps%d" % i, [128, 512], F32, psum=True) for i in range(8)]
            ws = K.WStream(self, ph)
            pctr = 0
            sctr = 0
            gctr = 0
            for tb in range(S // TB):
                tok0 = tb * TB
                pctr = self.norm_block(xin, tok0, NT, BIG, xnT, hT, xnT[:], ss, rstd, ps, A, B, pctr)
                if "dbg_AB" in cfg.get("dbg", ()) and tb == 0:
                    dd = self.scratch("dbg_AB", (128, 2 * KC + NT))
                    s.dma(dd[:, 0:KC], A[:], reads=[A.b], dsem=BIG.d)
                    s.dma(dd[:, KC:2 * KC], B[:], reads=[B.b], dsem=BIG.d)
                    s.dma(dd[:, 2 * KC:], rstd[:], reads=[rstd.b], dsem=BIG.d)
                if "dbg_hT" in cfg.get("dbg", ()) and tb == 0:
                    dd = self.scratch("dbg_hT", (128, KC * TB), BF16)
                    s.dma(dd, hT[:].rearrange("p c t -> p (c t)"), reads=[hT.b], dsem=BIG.d)
                for (col0, ncols, mode, dst, func) in jobs:
                    for c0 in range(0, ncols, 512):
                        w = min(512, ncols - c0)
                        ws.plan([w_in[ks * 512:(ks + 1) * 512, col0 + c0: col0 + c0 + w] for ks in range(D // 512)])
                        pb0 = (gctr % 2) * 4
                        gctr += 1
                        nchunk = (w + 127) // 128
                        for ks in range(D // 512):
                            bf, kc, nn = ws.next()
                            if "dbg_w" in cfg.get("dbg", ()) and gctr == 1 and tb == 0:
                                dd = self.scratch("dbg_w", (128, 4 * 512), BF16)
                                s.dma(dd, bf[:].rearrange("p c t -> p (c t)"), reads=[bf.b], dsem=BIG.d)
                            for k4 in range(kc):
                                kk = ks * 4 + k4
                                first = kk == 0
                                if mode == "fm":
                                    for c in range(nchunk):
                                        mc = min(128, w - c * 128)
                                        p = ps[pb0 + c]
                                        s.op("pe", lambda e, p=p, bf=bf, k4=k4, c=c, kk=kk, mc=mc: e.matmul(
                                            p[0:mc, 0:TB], bf[:, k4, c * 128:c * 128 + mc], hT[:, kk, :], start=(kk == 0), stop=(kk == KC - 1)),
                                            reads=[bf.b, hT.b], writes=[p.b] if first else (), accs=() if first else [p.b])
                                else:
                                    for t in range(NT):
                                        p = ps[pb0 + t]
                                        s.op("pe", lambda e, p=p, bf=bf, k4=k4, t=t, kk=kk, w=w: e.matmul(
                                            p[:, 0:w], hT[:, kk, t * 128:(t + 1) * 128], bf[:, k4, 0:w], start=(kk == 0), stop=(kk == KC - 1)),
                                            reads=[bf.b, hT.b], writes=[p.b] if first else (), accs=() if first else [p.b])
                        if mode == "fm":
                            for c in range(nchunk):
                                mc = min(128, w - c * 128)
                                p = ps[pb0 + c]
                                sg = stage[sctr % 4]
                                sctr += 1
                                s.op("act", lambda e, p=p, sg=sg, mc=mc, func=func: e.activation(out=sg[0:mc, 0:TB], in_=p[0:mc, 0:TB], func=func),
                                     reads=[p.b], writes=[sg.b])
                                s.dma(dst[c0 + c * 128: c0 + c * 128 + mc, tok0:tok0 + TB], sg[0:mc, 0:TB], reads=[sg.b], dsem=sg.d)
                        else:
                            for t in range(NT):
                                p = ps[pb0 + t]
                                sg = stage[sctr % 4]
                                sctr += 1
                                s.op("act", lambda e, p=p, sg=sg, w=w: e.copy(sg[:, 0:w], p[:, 0:w]), reads=[p.b], writes=[sg.b])
                                s.dma(dst[tok0 + t * 128: tok0 + (t + 1) * 128, c0:c0 + w], sg[:, 0:w], reads=[sg.b], dsem=sg.d)
            s.flush()

    PITCH = 512
    YSZ = 128 * 512 + 512

    def bcast_rows(self, dst_ap, row_ap, n, ps, npart, reads, dstb, first=True):
        s = self.s
        for c0 in range(0, n, 512):
            w = min(512, n - c0)
            s.op("pe", lambda e, c0=c0, w=w: e.matmul(ps[0:npart, 0:w], self.ones_row[0:1, 0:npart], row_ap[0:1, c0:c0 + w], start=True, stop=True),
                 reads=list(reads) + [self.ones_row.b], writes=[ps.b])
            s.op("act", lambda e, c0=c0, w=w: e.copy(dst_ap[0:npart, c0:c0 + w], ps[0:npart, 0:w]), reads=[ps.b],
                 writes=[dstb] if (first and c0 == 0) else (), accs=() if (first and c0 == 0) else [dstb])

    def setup_phase(self):
        s, st, cfg = self.s, self.stack, self.cfg
        DEPTH = cfg["DEPTH"]
        self.U = T(s, st, "U", [64, 64], F32)
        self.Mgt = T(s, st, "Mgt", [64, 64], F32)
        s.op("pool", lambda e: e.memset(self.U[:], 1.0), writes=[self.U.b])
        s.op("pool", lambda e: e.affine_select(out=self.U[:], in_=self.U[:], pattern=[[1, 64]], compare_op=ALU.is_ge, fill=0.0,
                                               base=0, channel_multiplier=-1), reads=[self.U.b], accs=[self.U.b])
        s.op("pool", lambda e: e.memset(self.Mgt[:], 1.0), writes=[self.Mgt.b])
        s.op("pool", lambda e: e.affine_select(out=self.Mgt[:], in_=self.Mgt[:], pattern=[[-1, 64]], compare_op=ALU.is_gt, fill=0.0,
                                               base=0, channel_multiplier=1), reads=[self.Mgt.b], accs=[self.Mgt.b])
        self.cmask = T(s, st, "cmask", [128, 128], F32)
        s.op("pool", lambda e: e.memset(self.cmask[:], 0.0), writes=[self.cmask.b])
        s.op("pool", lambda e: e.affine_select(out=self.cmask[:], in_=self.cmask[:], pattern=[[-1, 128]], compare_op=ALU.is_ge, fill=-1e30,
                                               base=0, channel_multiplier=1), reads=[self.cmask.b], accs=[self.cmask.b])
        lb_d = self.scratch("lb_d", (DEPTH, 1024))
        Z_d = self.scratch("Z_d", (2, 24, 384))
        self.Yc_d = self.nc.dram_tensor("Yc_d", [16 * K.YSZ], F32)
        self.Yb_d = self.nc.dram_tensor("Yb_d", [8 * K.YSZ], F32)
        with contextlib.ExitStack() as ph:
            lg = T(s, ph, "lg", [1, DEPTH, 1024], F32, dma=True)
            ex = T(s, ph, "ex", [1, DEPTH, 1024], F32)
            lbrow = T(s, ph, "lbrow", [1, DEPTH, 1024], F32, dma=True)
            mx = T(s, ph, "mx", [1, 1024], F32)
            sm = T(s, ph, "sm", [1, 1024], F32)
            cum = T(s, ph, "cum", [1, 1024], F32)
            s.dma(lg[:], self.dram["lb_logits"].rearrange("(o l) d -> o l d", o=1), writes=[lg.b], dsem=lg.d)
            s.op("dve", lambda e: e.tensor_copy(mx[:], lg[:, 0, :]), reads=[lg.b], writes=[mx.b])
            for l in range(1, DEPTH):
                s.op("dve", lambda e, l=l: e.tensor_tensor(out=mx[:], in0=mx[:], in1=lg[:, l, :], op=ALU.max), reads=[mx.b, lg.b], accs=[mx.b])
            for l in range(DEPTH):
                s.op("dve", lambda e, l=l: e.tensor_tensor(out=ex[:, l, :], in0=lg[:, l, :], in1=mx[:], op=ALU.subtract), reads=[lg.b, mx.b],
                     writes=[ex.b] if l == 0 else (), accs=[ex.b] if l else ())
            s.op("act", lambda e: e.activation(out=ex[:], in_=ex[:], func=AF.Exp), reads=[ex.b], accs=[ex.b])
            s.op("dve", lambda e: e.tensor_copy(sm[:], ex[:, 0, :]), reads=[ex.b], writes=[sm.b])
            for l in range(1, DEPTH):
                s.op("dve", lambda e, l=l: e.tensor_tensor(out=sm[:], in0=sm[:], in1=ex[:, l, :], op=ALU.add), reads=[sm.b, ex.b], accs=[sm.b])
            s.op("dve", lambda e: e.reciprocal(sm[:], sm[:]), reads=[sm.b], accs=[sm.b])
            s.op("dve", lambda e: e.memset(lbrow[:, 0, :], 0.0), writes=[lbrow.b])
            s.op("dve", lambda e: e.memset(cum[:], 0.0), writes=[cum.b])
            for l in range(1, DEPTH):
                s.op("dve", lambda e, l=l: e.tensor_tensor(out=ex[:, l, :], in0=ex[:, l, :], in1=sm[:], op=ALU.mult), reads=[ex.b, sm.b], accs=[ex.b])
                s.op("dve", lambda e, l=l: e.tensor_tensor(out=cum[:], in0=cum[:], in1=ex[:, l, :], op=ALU.add), reads=[cum.b, ex.b], accs=[cum.b])
                s.op("dve", lambda e, l=l: e.tensor_copy(lbrow[:, l, :], cum[:]), reads=[cum.b], accs=[lbrow.b])
            s.dma(lb_d.rearrange("(o l) d -> o l d", o=1), lbrow[:], reads=[lbrow.b], dsem=lbrow.d)
            relT = T(s, ph, "relT", [33, 24], F32, dma=True)
            oh = T(s, ph, "oh", [33, 2 * 383], F32, dma=True)
            Zsb = T(s, ph, "Zsb", [24, 2, 384], F32, dma=True)
            psz = [T(s, ph, "psz%d" % i, [128, 512], F32, psum=True) for i in range(2)]
            s.op("pool", lambda e: e.memset(relT[:], -1e30), writes=[relT.b])
            s.dma(relT[0:32, :], self.dram["rel_table"], reads=[relT.b], accs=[relT.b], dsem=relT.d)
            s.dma(oh[:], self.dram["t5_onehot"], writes=[oh.b], dsem=oh.d)
            s.op("pool", lambda e: e.memset(Zsb[:], 0.0), writes=[Zsb.b])
            for j in range(2):
                s.op("pe", lambda e, j=j: e.matmul(psz[j][0:24, 0:383], relT[:], oh[:, j * 383:(j + 1) * 383], start=True, stop=True),
                     reads=[relT.b, oh.b], writes=[psz[j].b])
                s.op("act", lambda e, j=j: e.copy(Zsb[:, j, 0:383], psz[j][0:24, 0:383]), reads=[psz[j].b], accs=[Zsb.b])
            s.dma(Z_d.rearrange("j h i -> h j i"), Zsb[:], reads=[Zsb.b], dsem=Zsb.d)
            s.flush()
        with contextlib.ExitStack() as ph:
            dummy = T(s, ph, "dummy", [1, 8], F32, dma=True)
            for j, (Y, nh, h0) in enumerate(((self.Yc_d, 16, 8), (self.Yb_d, 8, 0))):
                for h in range(nh):
                    src = bass.AP(tensor=Z_d.tensor, offset=(j * 24 + h0 + h) * 384, ap=[[0, 128], [1, 383]])
                    dst = bass.AP(tensor=Y, offset=h * K.YSZ, ap=[[K.PITCH + 1, 128], [1, 383]])
                    s.dma(dst, src, accs=[dummy.b], dsem=dummy.d)
            s.flush()

    def bias_view(self, Y, h):
        return bass.AP(tensor=Y, offset=h * K.YSZ + 127, ap=[[K.PITCH, 128], [1, 256]])

    def swa_phase(self, l):
        cfg, s = self.cfg, self.s
        S = cfg["S"]
        NB = S // 128
        mx_ = self.mx
        with contextlib.ExitStack() as ph:
            ps = [T(s, ph, "ps%d" % i, [128, 512], F32, psum=True) for i in range(8)]
            stg = T(s, ph, "stg", [128, S], F32, dma=True)
            qb = T(s, ph, "qb", [128, S], BF16)
            kdup = [T(s, ph, "kdup%d" % g, [128, S], BF16) for g in range(2)]
            vb = T(s, ph, "vb", [128, NB, 128], BF16)
            vst = T(s, ph, "vst", [128, NB, 128], F32, dma=True)
            biasC = T(s, ph, "biasC", [128, 16, 256], F32, dma=True)
            sinkB = T(s, ph, "sinkB", [128, 16], F32)
            srow = T(s, ph, "srow", [1, 16], F32, dma=True)
            lg = T(s, ph, "lg", [128, 256], F32)
            pe_ = T(s, ph, "pexp", [128, 256], F32)
            pn = T(s, ph, "pn", [128, 256], BF16)
            pT = T(s, ph, "pT", [128, 2, 128], BF16)
            sm = T(s, ph, "sm", [128, 4], F32)
            yc = T(s, ph, "yc", [128, NB, 1024], BF16)
            ycT = T(s, ph, "ycT", [128, 8, S], BF16, dma=True)
            for h in range(16):
                s.dma(biasC[:, h, :], self.bias_view(self.Yc_d, h), writes=[biasC.b] if h == 0 else (), accs=[biasC.b] if h else (), dsem=biasC.d)
            s.dma(srow[:], self.dram["sinks"][l].rearrange("(o h) -> o h", o=1), writes=[srow.b], dsem=srow.d)
            self.bcast_rows(sinkB, srow, 16, ps[0], 128, [srow.b], sinkB.b)
            for g in range(2):
                s.dma(stg[0:64, :], mx_["ckT"][g * 64:(g + 1) * 64, :], writes=[stg.b], dsem=stg.d)
                s.dma(stg[64:128, :], mx_["ckT"][g * 64:(g + 1) * 64, :], accs=[stg.b], dsem=stg.d)
                s.op("dve", lambda e, g=g: e.tensor_copy(kdup[g][:], stg[:]), reads=[stg.b], writes=[kdup[g].b])
            s.dma(vst[:], mx_["cv"].rearrange("(n s) d -> s n d", s=128), writes=[vst.b], dsem=vst.d)
            s.op("dve", lambda e: e.tensor_copy(vb[:], vst[:]), reads=[vst.b], writes=[vb.b])
            pc = 0
            for j in range(8):
                g = j // 4
                s.dma(stg[:], mx_["cqT"][j * 128:(j + 1) * 128, :], writes=[stg.b], dsem=stg.d)
                s.op("dve", lambda e: e.tensor_copy(qb[:], stg[:]), reads=[stg.b], writes=[qb.b])
                for hh in range(2):
                    h = 2 * j + hh
                    hb = hh * 64
                    for nb in range(NB):
                        k0 = max(nb - 1, 0) * 128
                        kw = 256 if nb > 0 else 128
                        b0 = 0 if nb > 0 else 128
                        pL = ps[pc % 8]; pc += 1
                        s.op("pe", lambda e, pL=pL, hb=hb, nb=nb, k0=k0, kw=kw, g=g: e.matmul(
                            pL[:, 0:kw], qb[hb:hb + 64, nb * 128:(nb + 1) * 128], kdup[g][hb:hb + 64, k0:k0 + kw], start=True, stop=True),
                            reads=[qb.b, kdup[g].b], writes=[pL.b])
                        s.op("dve", lambda e, pL=pL, kw=kw, b0=b0, h=h: e.scalar_tensor_tensor(
                            out=lg[:, 0:kw], in0=pL[:, 0:kw], scalar=0.125, in1=biasC[:, h, b0:b0 + kw], op0=ALU.mult, op1=ALU.add),
                            reads=[pL.b, biasC.b], writes=[lg.b])
                        s.op("dve", lambda e, kw=kw: e.reduce_max(out=sm[:, 0:1], in_=lg[:, 0:kw], axis=AX.X), reads=[lg.b], writes=[sm.b])
                        s.op("dve", lambda e, h=h: e.tensor_tensor(out=sm[:, 0:1], in0=sm[:, 0:1], in1=sinkB[:, h:h + 1], op=ALU.max),
                             reads=[sm.b, sinkB.b], accs=[sm.b])
                        s.op("dve", lambda e: e.tensor_scalar(out=sm[:, 1:2], in0=sm[:, 0:1], scalar1=-1.0, scalar2=None, op0=ALU.mult),
                             reads=[sm.b], accs=[sm.b])
                        s.op("act", lambda e, kw=kw: e.activation(out=pe_[:, 0:kw], in_=lg[:, 0:kw], func=AF.Exp, bias=sm[:, 1:2]),
                             reads=[lg.b, sm.b], writes=[pe_.b])
                        s.op("act", lambda e, h=h: e.activation(out=sm[:, 2:3], in_=sinkB[:, h:h + 1], func=AF.Exp, bias=sm[:, 1:2]),
                             reads=[sinkB.b, sm.b], accs=[sm.b])
                        s.op("dve", lambda e, kw=kw: e.reduce_sum(out=sm[:, 3:4], in_=pe_[:, 0:kw], axis=AX.X), reads=[pe_.b], accs=[sm.b])
                        s.op("dve", lambda e: e.tensor_tensor(out=sm[:, 3:4], in0=sm[:, 3:4], in1=sm[:, 2:3], op=ALU.add), reads=[sm.b], accs=[sm.b])
                        s.op("dve", lambda e: e.reciprocal(sm[:, 3:4], sm[:, 3:4]), reads=[sm.b], accs=[sm.b])
                        s.op("dve", lambda e, kw=kw: e.tensor_scalar(out=pn[:, 0:kw], in0=pe_[:, 0:kw], scalar1=sm[:, 3:4], scalar2=None, op0=ALU.mult),
                             reads=[pe_.b, sm.b], writes=[pn.b])
                        nkb = kw // 128
                        pt = ps[pc % 8]; pc += 1
                        ptb = pt.t[:].bitcast(BF16)
                        for kb in range(nkb):
                            s.op("pe", lambda e, kb=kb, ptb=ptb: e.transpose(ptb[:, kb * 128:(kb + 1) * 128], pn[:, kb * 128:(kb + 1) * 128], self.identb[:]),
                                 reads=[pn.b, self.identb.b], writes=[pt.b] if kb == 0 else (), accs=[pt.b] if kb else ())
                        s.op("act", lambda e, ptb=ptb, kw=kw: e.copy(pT[:].rearrange("p a b -> p (a b)")[:, 0:kw], ptb[:, 0:kw]), reads=[pt.b], writes=[pT.b])
                        pO = ps[pc % 8]; pc += 1
                        for kb in range(nkb):
                            blk = k0 // 128 + kb
                            s.op("pe", lambda e, kb=kb, blk=blk, pO=pO, g=g, nkb=nkb: e.matmul(
                                pO[:, 0:64], pT[:, kb, :], vb[:, blk, g * 64:(g + 1) * 64], start=(kb == 0), stop=(kb == nkb - 1)),
                                reads=[pT.b, vb.b], writes=[pO.b] if kb == 0 else (), accs=[pO.b] if kb else ())
                        first = (j == 0 and hh == 0 and nb == 0)
                        s.op("act", lambda e, pO=pO, nb=nb, h=h: e.copy(yc[:, nb, h * 64:(h + 1) * 64], pO[:, 0:64]), reads=[pO.b],
                             writes=[yc.b] if first else (), accs=() if first else [yc.b])
            for nb in range(NB):
                pt = ps[pc % 8]; pc += 1
                ptb = pt.t[:].bitcast(BF16)
                for c in range(8):
                    s.op("pe", lambda e, c=c, nb=nb, ptb=ptb: e.transpose(ptb[:, c * 128:(c + 1) * 128], yc[:, nb, c * 128:(c + 1) * 128], self.identb[:]),
                         reads=[yc.b, self.identb.b], writes=[pt.b] if c == 0 else (), accs=[pt.b] if c else ())
                s.op("act", lambda e, nb=nb, ptb=ptb: e.copy(ycT[:, :, nb * 128:(nb + 1) * 128], ptb.rearrange("p (c t) -> p c t", c=8)), reads=[pt.b],
                     writes=[ycT.b] if nb == 0 else (), accs=[ycT.b] if nb else ())
            s.dma(self.mx["yT"][2].rearrange("(c p) s -> p c s", p=128), ycT[:], reads=[ycT.b], dsem=ycT.d)
            s.flush()

    def dsa_phase(self, l):
        cfg, s = self.cfg, self.s
        S, TOPK = cfg["S"], cfg["TOPK"]
        NB = S // 128
        mx_ = self.mx
        SCALE = 128 ** -0.5
        with contextlib.ExitStack() as ph:
            ps = [T(s, ph, "ps%d" % i, [128, 512], F32, psum=True) for i in range(8)]
            stg = T(s, ph, "stg", [128, S], F32, dma=True)
            lat = T(s, ph, "lat", [128, 512], F32, dma=True)
            latn = T(s, ph, "latn", [128, 512], BF16)
            latT = T(s, ph, "latT", [128, 4, S], BF16)
            sq = T(s, ph, "sq", [128, 512], F32)
            sm = T(s, ph, "sm", [128, 8], F32)
            kvg = T(s, ph, "kvg", [128, 512], F32)
            grow_ = T(s, ph, "kvgrow", [1, 512], F32, dma=True)
            wkv = T(s, ph, "wkv", [128, 4, 256], F32, dma=True)
            wkvb = T(s, ph, "wkvb", [128, 4, 256], BF16)
            kT = T(s, ph, "kT", [128, S], BF16)
            vb = T(s, ph, "vb", [128, NB, 128], BF16)
            s.dma(grow_[:], self.dram["kv_norm"][l].rearrange("(o d) -> o d", o=1), writes=[grow_.b], dsem=grow_.d)
            self.bcast_rows(kvg, grow_, 512, ps[0], 128, [grow_.b], kvg.b)
            s.dma(wkv[:], self.dram["w_kv_up"][l].rearrange("(c p) n -> p c n", p=128), writes=[wkv.b], dsem=wkv.d)
            s.op("dve", lambda e: e.tensor_copy(wkvb[:], wkv[:]), reads=[wkv.b], writes=[wkvb.b])
            pc = 0
            for nb in range(NB):
                s.dma(lat[:], mx_["blat"][nb * 128:(nb + 1) * 128, :], writes=[lat.b], dsem=lat.d)
                s.op("dve", lambda e, nb=nb: e.tensor_tensor(out=sq[:], in0=lat[:], in1=lat[:], op=ALU.mult), reads=[lat.b], writes=[sq.b])
                s.op("dve", lambda e: e.reduce_sum(out=sm[:, 0:1], in_=sq[:], axis=AX.X), reads=[sq.b], writes=[sm.b])
                self.rsqrt_mean(sm[:, 1:2], sm[:, 0:1], sm.b, sm.b, 512)
                s.op("dve", lambda e, nb=nb: e.scalar_tensor_tensor(out=latn[:], in0=lat[:], scalar=sm[:, 1:2], in1=kvg[:], op0=ALU.mult, op1=ALU.mult),
                     reads=[lat.b, sm.b, kvg.b], writes=[latn.b])
                pt = ps[pc % 8]; pc += 1
                ptb = pt.t[:].bitcast(BF16)
                for c in range(4):
                    s.op("pe", lambda e, c=c, ptb=ptb: e.transpose(ptb[:, c * 128:(c + 1) * 128], latn[:, c * 128:(c + 1) * 128], self.identb[:]),
                         reads=[latn.b, self.identb.b], writes=[pt.b] if c == 0 else (), accs=[pt.b] if c else ())
                s.op("act", lambda e, nb=nb, ptb=ptb: e.copy(latT[:, :, nb * 128:(nb + 1) * 128], ptb[:, 0:512].rearrange("p (c t) -> p c t", c=4)),
                     reads=[pt.b], writes=[latT.b] if nb == 0 else (), accs=[latT.b] if nb else ())
            for t0 in range(0, S, 512):
                p = ps[pc % 8]; pc += 1
                for c in range(4):
                    s.op("pe", lambda e, c=c, p=p, t0=t0: e.matmul(p[:], wkvb[:, c, 0:128], latT[:, c, t0:t0 + 512], start=(c == 0), stop=(c == 3)),
                         reads=[wkvb.b, latT.b], writes=[p.b] if c == 0 else (), accs=[p.b] if c else ())
                s.op("act", lambda e, p=p, t0=t0: e.copy(kT[:, t0:t0 + 512], p[:]), reads=[p.b], writes=[kT.b] if t0 == 0 else (), accs=[kT.b] if t0 else ())
            for nb in range(NB):
                p = ps[pc % 8]; pc += 1
                for c in range(4):
                    s.op("pe", lambda e, c=c, p=p, nb=nb: e.matmul(p[:, 0:128], latT[:, c, nb * 128:(nb + 1) * 128], wkvb[:, c, 128:256], start=(c == 0), stop=(c == 3)),
                         reads=[wkvb.b, latT.b], writes=[p.b] if c == 0 else (), accs=[p.b] if c else ())
                s.op("act", lambda e, p=p, nb=nb: e.copy(vb[:, nb, :], p[:, 0:128]), reads=[p.b], writes=[vb.b] if nb == 0 else (), accs=[vb.b] if nb else ())
            ikd = T(s, ph, "ikd", [128, S], BF16)
            iqb = T(s, ph, "iqb", [128, 8, S], BF16)
            qb = T(s, ph, "qb", [128, 8, S], BF16)
            iw = T(s, ph, "iw", [128, NB, 16], F32, dma=True)
            biasB = T(s, ph, "biasB", [128, 8, 256], F32, dma=True)
            cB = T(s, ph, "cB", [128, 24], F32)
            crow = T(s, ph, "crow", [1, 24], F32, dma=True)
            s.dma(stg[0:64, :], mx_["bikT"], writes=[stg.b], dsem=stg.d)
            s.dma(stg[64:128, :], mx_["bikT"], accs=[stg.b], dsem=stg.d)
            s.op("dve", lambda e: e.tensor_copy(ikd[:], stg[:]), reads=[stg.b], writes=[ikd.b])
            for c in range(8):
                s.dma(stg[:], mx_["biqT"][c * 128:(c + 1) * 128, :], writes=[stg.b], dsem=stg.d)
                s.op("dve", lambda e, c=c: e.tensor_copy(iqb[:, c, :], stg[:]), reads=[stg.b], writes=[iqb.b] if c == 0 else (), accs=[iqb.b] if c else ())
            for c in range(8):
                s.dma(stg[:], mx_["bqT"][c * 128:(c + 1) * 128, :], writes=[stg.b], dsem=stg.d)
                s.op("dve", lambda e, c=c: e.tensor_copy(qb[:, c, :], stg[:]), reads=[stg.b], writes=[qb.b] if c == 0 else (), accs=[qb.b] if c else ())
            s.dma(iw[:], mx_["biw"].rearrange("(n t) h -> t n h", t=128), writes=[iw.b], dsem=iw.d)
            for h in range(8):
                s.dma(biasB[:, h, :], self.bias_view(self.Yb_d, h), writes=[biasB.b] if h == 0 else (), accs=[biasB.b] if h else (), dsem=biasB.d)
            s.dma(crow[:], self.dram["rel_table"][31:32, :], writes=[crow.b], dsem=crow.d)
            self.bcast_rows(cB, crow, 24, ps[0], 128, [crow.b], cB.b)
            acc = T(s, ph, "acc", [128, S], F32)
            work = T(s, ph, "work", [128, S], F32)
            rl = T(s, ph, "rl", [128, S], F32)
            mb = work
            m8 = T(s, ph, "m8", [128, 8], F32)
            lg = T(s, ph, "lg", [128, S], F32)
            pn = T(s, ph, "pn", [128, S], BF16)
            pT = T(s, ph, "pT", [128, NB, 128], BF16)
            ybT = T(s, ph, "ybT", [128, 8, S], BF16, dma=True)
            for qt in range(NB):
                kw = (qt + 1) * 128
                q0 = qt * 128
                use_topk = kw > TOPK
                if use_topk:
                    for ih in range(16):
                        c, hb = ih // 2, (ih % 2) * 64
                        pb = (ih % 2) * 4
                        for n0 in range(0, kw, 512):
                            w = min(512, kw - n0)
                            p = ps[pb + n0 // 512]
                            s.op("pe", lambda e, p=p, c=c, hb=hb, n0=n0, w=w, q0=q0: e.matmul(
                                p[:, 0:w], iqb[hb:hb + 64, c, q0:q0 + 128], ikd[hb:hb + 64, n0:n0 + w], start=True, stop=True),
                                reads=[iqb.b, ikd.b], writes=[p.b])
                            s.op("act", lambda e, p=p, n0=n0, w=w: e.activation(out=rl[:, n0:n0 + w], in_=p[:, 0:w], func=AF.Relu),
                                 reads=[p.b], writes=[rl.b] if n0 == 0 else (), accs=[rl.b] if n0 else ())
                        if ih == 0:
                            s.op("dve", lambda e, kw=kw, qt=qt: e.tensor_scalar(out=acc[:, 0:kw], in0=rl[:, 0:kw], scalar1=iw[:, qt, 0:1], scalar2=None, op0=ALU.mult),
                                 reads=[rl.b, iw.b], writes=[acc.b])
                        else:
                            s.op("dve", lambda e, kw=kw, qt=qt, ih=ih: e.scalar_tensor_tensor(
                                out=acc[:, 0:kw], in0=rl[:, 0:kw], scalar=iw[:, qt, ih:ih + 1], in1=acc[:, 0:kw], op0=ALU.mult, op1=ALU.add),
                                reads=[rl.b, iw.b, acc.b], accs=[acc.b])
                    s.op("dve", lambda e, q0=q0: e.tensor_tensor(out=acc[:, q0:q0 + 128], in0=acc[:, q0:q0 + 128], in1=self.cmask[:], op=ALU.add),
                         reads=[acc.b, self.cmask.b], accs=[acc.b])
                    s.op("dve", lambda e, kw=kw: e.tensor_copy(work[:, 0:kw], acc[:, 0:kw]), reads=[acc.b], writes=[work.b])
                    for r in range(TOPK // 8):
                        s.op("dve", lambda e, kw=kw: e.max(out=m8[:], in_=work[:, 0:kw]), reads=[work.b], writes=[m8.b])
                        if r < TOPK // 8 - 1:
                            s.op("dve", lambda e, kw=kw: e.match_replace(out=work[:, 0:kw], in_to_replace=m8[:], in_values=work[:, 0:kw], imm_value=-3e38),
                                 reads=[work.b, m8.b], accs=[work.b])
                    s.op("dve", lambda e, kw=kw: e.tensor_scalar(out=mb[:, 0:kw], in0=acc[:, 0:kw], scalar1=m8[:, 7:8], scalar2=-1e30,
                                                            op0=ALU.is_lt, op1=ALU.mult), reads=[acc.b, m8.b], writes=[mb.b])
                for h in range(8):
                    pb = (h % 2) * 4
                    for n0 in range(0, kw, 512):
                        w = min(512, kw - n0)
                        p = ps[pb + n0 // 512]
                        s.op("pe", lambda e, p=p, h=h, n0=n0, w=w, q0=q0: e.matmul(p[:, 0:w], qb[:, h, q0:q0 + 128], kT[:, n0:n0 + w], start=True, stop=True),
                             reads=[qb.b, kT.b], writes=[p.b])
                    w0 = max(kw - 256, 0)
                    first = True
                    for n0 in range(0, w0, 512):
                        w = min(512, w0 - n0)
                        p = ps[pb + n0 // 512]
                        s.op("dve", lambda e, p=p, n0=n0, w=w, h=h: e.tensor_scalar(out=lg[:, n0:n0 + w], in0=p[:, 0:w], scalar1=SCALE, scalar2=cB[:, h:h + 1],
                                                                               op0=ALU.mult, op1=ALU.add), reads=[p.b, cB.b],
                             writes=[lg.b] if first else (), accs=() if first else [lg.b])
                        first = False
                    ww = kw - w0
                    b0 = 256 - ww
                    for n0 in range(w0, kw, 128):
                        p = ps[pb + n0 // 512]
                        o = n0 % 512
                        bo = b0 + (n0 - w0)
                        s.op("dve", lambda e, p=p, n0=n0, o=o, bo=bo, h=h: e.scalar_tensor_tensor(
                            out=lg[:, n0:n0 + 128], in0=p[:, o:o + 128], scalar=SCALE, in1=biasB[:, h, bo:bo + 128], op0=ALU.mult, op1=ALU.add),
                            reads=[p.b, biasB.b], writes=[lg.b] if first else (), accs=() if first else [lg.b])
                        first = False
                    if use_topk:
                        s.op("dve", lambda e, kw=kw: e.tensor_tensor(out=lg[:, 0:kw], in0=lg[:, 0:kw], in1=mb[:, 0:kw], op=ALU.add), reads=[lg.b, mb.b], accs=[lg.b])
                    s.op("dve", lambda e, kw=kw: e.reduce_max(out=sm[:, 2:3], in_=lg[:, 0:kw], axis=AX.X), reads=[lg.b], writes=[sm.b])
                    s.op("dve", lambda e: e.tensor_scalar(out=sm[:, 3:4], in0=sm[:, 2:3], scalar1=-1.0, scalar2=None, op0=ALU.mult), reads=[sm.b], accs=[sm.b])
                    s.op("act", lambda e, kw=kw: e.activation(out=lg[:, 0:kw], in_=lg[:, 0:kw], func=AF.Exp, bias=sm[:, 3:4]), reads=[lg.b, sm.b], accs=[lg.b])
                    s.op("dve", lambda e, kw=kw: e.reduce_sum(out=sm[:, 4:5], in_=lg[:, 0:kw], axis=AX.X), reads=[lg.b], accs=[sm.b])
                    s.op("dve", lambda e: e.reciprocal(sm[:, 4:5], sm[:, 4:5]), reads=[sm.b], accs=[sm.b])
                    s.op("dve", lambda e, kw=kw: e.tensor_scalar(out=pn[:, 0:kw], in0=lg[:, 0:kw], scalar1=sm[:, 4:5], scalar2=None, op0=ALU.mult),
                         reads=[lg.b, sm.b], writes=[pn.b])
                    nkb = kw // 128
                    for k8 in range(0, nkb, 8):
                        pt = ps[pc % 8]; pc += 1
                        ptb = pt.t[:].bitcast(BF16)
                        nn = min(8, nkb - k8)
                        for kb in range(nn):
                            s.op("pe", lambda e, kb=kb, k8=k8, ptb=ptb: e.transpose(ptb[:, kb * 128:(kb + 1) * 128], pn[:, (k8 + kb) * 128:(k8 + kb + 1) * 128], self.identb[:]),
                                 reads=[pn.b, self.identb.b], writes=[pt.b] if kb == 0 else (), accs=[pt.b] if kb else ())
                        s.op("act", lambda e, ptb=ptb, k8=k8, nn=nn: e.copy(pT[:, k8:k8 + nn, :], ptb[:, 0:nn * 128].rearrange("p (a b) -> p a b", b=128)),
                             reads=[pt.b], writes=[pT.b] if k8 == 0 else (), accs=[pT.b] if k8 else ())
                    pO = ps[pc % 8]; pc += 1
                    for kb in range(nkb):
                        s.op("pe", lambda e, kb=kb, pO=pO, nkb=nkb: e.matmul(pO[:, 0:128], vb[:, kb, :], pT[:, kb, :], start=(kb == 0), stop=(kb == nkb - 1)),
                             reads=[pT.b, vb.b], writes=[pO.b] if kb == 0 else (), accs=[pO.b] if kb else ())
                    first = (qt == 0 and h == 0)
                    s.op("act", lambda e, pO=pO, h=h, q0=q0: e.copy(ybT[:, h, q0:q0 + 128], pO[:, 0:128]), reads=[pO.b],
                         writes=[ybT.b] if first else (), accs=() if first else [ybT.b])
            s.dma(self.mx["yT"][1].rearrange("(c p) s -> p c s", p=128), ybT[:], reads=[ybT.b], dsem=ybT.d)
            s.flush()

    def hgrn_phase(self, l):
        cfg, s = self.cfg, self.s
        S = cfg["S"]
        CS = 32
        SEG = cfg.get("HSEG", min(512, S))
        NCHT = S // CS
        NCH = SEG // CS
        mx_ = self.mx
        lb_d = self.dram["lb_d"]
        with contextlib.ExitStack() as ph:
            ps = [T(s, ph, "ps%d" % i, [128, 512], F32, psum=True) for i in range(8)]
            lbc = T(s, ph, "lbc", [128, 8], F32)
            omc = T(s, ph, "omc", [128, 8], F32)
            lbB = T(s, ph, "lbB", [CS, 1024], F32)
            omB = T(s, ph, "omB", [CS, 1024], F32)
            hgB = T(s, ph, "hgB", [CS, 128], F32)
            row = T(s, ph, "row", [1, 1024], F32, dma=True)
            row2 = T(s, ph, "row2", [1, 128], F32, dma=True)
            self.colload(ph, None, lb_d[l], 8, ps[0], "lb")
            s.op("dve", lambda e: e.tensor_copy(lbc[:], ps[0][:, 0:8]), reads=[ps[0].b], writes=[lbc.b])
            s.op("dve", lambda e: e.tensor_scalar(out=omc[:], in0=lbc[:], scalar1=-1.0, scalar2=1.0, op0=ALU.mult, op1=ALU.add), reads=[lbc.b], writes=[omc.b])
            s.dma(row[:], lb_d[l:l + 1, :], writes=[row.b], dsem=row.d)
            self.bcast_rows(lbB, row, 1024, ps[1], CS, [row.b], lbB.b)
            s.op("dve", lambda e: e.tensor_scalar(out=omB[:], in0=lbB[:], scalar1=-1.0, scalar2=1.0, op0=ALU.mult, op1=ALU.add), reads=[lbB.b], writes=[omB.b])
            s.dma(row2[:], self.dram["hgrn_norm"][l].rearrange("(o d) -> o d", o=1), writes=[row2.b], dsem=row2.d)
            self.bcast_rows(hgB, row2, 128, ps[1], CS, [row2.b], hgB.b)
            qT = T(s, ph, "qT", [128, NCH, CS], F32, dma=True)
            kTf = T(s, ph, "kTf", [128, NCH, CS], F32, dma=True)
            bT = T(s, ph, "bT", [128, NCH, CS], F32)
            d1 = T(s, ph, "d1", [128, NCH, CS], F32)
            bmid = T(s, ph, "bmid", [128, NCH], F32)
            bend = T(s, ph, "bend", [128, NCH], F32)
            qtil = T(s, ph, "qtil", [128, NCH, CS], BF16)
            ktil = T(s, ph, "ktil", [128, NCH, CS], BF16)
            qb = T(s, ph, "qbb", [128, NCH, CS], BF16)
            f_tm = T(s, ph, "f_tm", [CS, NCH, 128], F32, dma=True)
            lf_tm = T(s, ph, "lf_tm", [CS, NCH, 128], F32)
            k_tm = T(s, ph, "k_tm", [CS, NCH, 128], F32)
            i_tm = T(s, ph, "i_tm", [CS, NCH, 128], F32, dma=True)
            g_tm = T(s, ph, "g_tm", [CS, NCH, 128], F32, dma=True)
            kend = T(s, ph, "kend", [CS, NCH, 128], BF16)
            vb = T(s, ph, "vb", [CS, NCH, 128], BF16)
            o_sb = T(s, ph, "o_sb", [CS, NCH, 128], F32)
            ssq = T(s, ph, "ssq", [CS, NCH], F32)
            att = T(s, ph, "att", [CS, CS], BF16)
            state = T(s, ph, "state", [128, 128], F32)
            stb = T(s, ph, "stb", [128, 128], BF16)
            yaT = T(s, ph, "yaT", [128, SEG], BF16, dma=True)
            pc = 2
            for h, sg in [(h_, g_) for h_ in range(8) for g_ in range(S // SEG)]:
                hs = slice(h * 128, (h + 1) * 128)
                ts = slice(sg * SEG, (sg + 1) * SEG)
                cs_ = slice(sg * NCH, (sg + 1) * NCH)
                bc = lambda t, hs=hs: t[:, hs].unsqueeze(1).to_broadcast([CS, NCH, 128])
                s.dma(qT[:].rearrange("p n t -> p (n t)"), mx_["aqT"][hs, ts], writes=[qT.b], dsem=qT.d)
                s.dma(kTf[:].rearrange("p n t -> p (n t)"), mx_["afT"][hs, ts], writes=[kTf.b], dsem=kTf.d)
                s.dma(f_tm[:], mx_["af"].rearrange("(n t) d -> t n d", t=CS)[:, cs_, hs], writes=[f_tm.b], dsem=f_tm.d)
                s.dma(i_tm[:], mx_["ai"].rearrange("(n t) d -> t n d", t=CS)[:, cs_, hs], writes=[i_tm.b], dsem=i_tm.d)
                s.dma(g_tm[:], mx_["ag"].rearrange("(n t) d -> t n d", t=CS)[:, cs_, hs], writes=[g_tm.b], dsem=g_tm.d)
                s.op("act", lambda e: e.activation(out=kTf[:], in_=kTf[:], func=AF.Sigmoid, scale=-1.0), reads=[kTf.b], accs=[kTf.b])
                s.op("dve", lambda e, h=h: e.tensor_scalar(out=kTf[:], in0=kTf[:], scalar1=omc[:, h:h + 1], scalar2=None, op0=ALU.mult), reads=[kTf.b, omc.b], accs=[kTf.b])
                s.op("act", lambda e: e.activation(out=k_tm[:], in_=f_tm[:], func=AF.Sigmoid, scale=-1.0), reads=[f_tm.b], writes=[k_tm.b])
                s.op("dve", lambda e, bc=bc: e.tensor_tensor(out=k_tm[:], in0=k_tm[:], in1=bc(omB), op=ALU.mult), reads=[k_tm.b, omB.b], accs=[k_tm.b])
                s.op("act", lambda e: e.activation(out=lf_tm[:], in_=f_tm[:], func=AF.Sigmoid), reads=[f_tm.b], writes=[lf_tm.b])
                s.op("dve", lambda e, bc=bc: e.tensor_tensor(out=lf_tm[:], in0=lf_tm[:], in1=bc(omB), op=ALU.mult), reads=[lf_tm.b, omB.b], accs=[lf_tm.b])
                s.op("dve", lambda e, bc=bc: e.tensor_tensor(out=lf_tm[:], in0=lf_tm[:], in1=bc(lbB), op=ALU.add), reads=[lf_tm.b, lbB.b], accs=[lf_tm.b])
                s.op("dve", lambda e: e.tensor_scalar(out=lf_tm[:], in0=lf_tm[:], scalar1=1e-20, scalar2=None, op0=ALU.max), reads=[lf_tm.b], accs=[lf_tm.b])
                s.op("act", lambda e: e.activation(out=lf_tm[:], in_=lf_tm[:], func=AF.Ln), reads=[lf_tm.b], accs=[lf_tm.b])
                s.op("act", lambda e: e.activation(out=vb[:], in_=i_tm[:], func=AF.Silu), reads=[i_tm.b], writes=[vb.b])
                s.op("act", lambda e: e.activation(out=g_tm[:], in_=g_tm[:], func=AF.Silu), reads=[g_tm.b], accs=[g_tm.b])
                NPB = 512 // CS
                for n8 in range(0, NCH, NPB):
                    p = ps[pc % 8]; pc += 1
                    for n in range(n8, min(n8 + NPB, NCH)):
                        s.op("pe", lambda e, p=p, n=n, n8=n8: e.matmul(p[:, (n - n8) * CS:(n - n8 + 1) * CS], lf_tm[:, n, :], self.U[0:CS, 0:CS], start=True, stop=True),
                             reads=[lf_tm.b, self.U.b], writes=[p.b] if n == n8 else (), accs=[p.b] if n != n8 else ())
                    nn = min(NPB, NCH - n8)
                    s.op("act", lambda e, p=p, n8=n8, nn=nn: e.copy(bT[:, n8:n8 + nn, :], p[:, 0:nn * CS].rearrange("p (a b) -> p a b", b=CS)),
                         reads=[p.b], writes=[bT.b] if n8 == 0 else (), accs=[bT.b] if n8 else ())
                for n4 in range(0, NCH, 4):
                    p = ps[pc % 8]; pc += 1
                    for n in range(n4, min(n4 + 4, NCH)):
                        s.op("pe", lambda e, p=p, n=n, n4=n4: e.matmul(p[0:CS, (n - n4) * 128:(n - n4 + 1) * 128], self.Mgt[0:CS, 0:CS], lf_tm[:, n, :], start=True, stop=True),
                             reads=[lf_tm.b, self.Mgt.b], writes=[p.b] if n == n4 else (), accs=[p.b] if n != n4 else ())
                    nn = min(4, NCH - n4)
                    s.op("act", lambda e, p=p, n4=n4, nn=nn: e.activation(out=o_sb[:, n4:n4 + nn, :], in_=p[0:CS, 0:nn * 128].rearrange("p (a b) -> p a b", b=128), func=AF.Exp),
                         reads=[p.b], writes=[o_sb.b] if n4 == 0 else (), accs=[o_sb.b] if n4 else ())
                s.op("dve", lambda e: e.tensor_tensor(out=kend[:], in0=k_tm[:], in1=o_sb[:], op=ALU.mult), reads=[k_tm.b, o_sb.b], writes=[kend.b])
                s.op("dve", lambda e: e.tensor_copy(bmid[:], bT[:, :, CS // 2 - 1]), reads=[bT.b], writes=[bmid.b])
                s.op("dve", lambda e: e.tensor_tensor(out=d1[:], in0=bT[:], in1=bmid[:].unsqueeze(2).to_broadcast([128, NCH, CS]), op=ALU.subtract),
                     reads=[bT.b, bmid.b], writes=[d1.b])
                s.op("act", lambda e: e.activation(out=bT[:], in_=bT[:], func=AF.Exp), reads=[bT.b], accs=[bT.b])
                s.op("dve", lambda e: e.tensor_copy(bend[:], bT[:, :, CS - 1]), reads=[bT.b], writes=[bend.b])
                s.op("dve", lambda e: e.tensor_tensor(out=qb[:], in0=qT[:], in1=bT[:], op=ALU.mult), reads=[qT.b, bT.b], writes=[qb.b])
                s.op("act", lambda e: e.activation(out=bT[:], in_=d1[:], func=AF.Exp), reads=[d1.b], writes=[bT.b])
                s.op("dve", lambda e: e.tensor_tensor(out=qtil[:], in0=qT[:], in1=bT[:], op=ALU.mult), reads=[qT.b, bT.b], writes=[qtil.b])
                s.op("act", lambda e: e.activation(out=bT[:], in_=d1[:], func=AF.Exp, scale=-1.0), reads=[d1.b], writes=[bT.b])
                s.op("dve", lambda e: e.tensor_tensor(out=ktil[:], in0=kTf[:], in1=bT[:], op=ALU.mult), reads=[kTf.b, bT.b], writes=[ktil.b])
                for n in range(NCH):
                    gn = sg * NCH + n
                    pA = ps[pc % 8]; pc += 1
                    s.op("pe", lambda e, pA=pA, n=n: e.matmul(pA[0:CS, 0:CS], ktil[:, n, :], qtil[:, n, :], start=True, stop=True),
                         reads=[ktil.b, qtil.b], writes=[pA.b])
                    s.op("dve", lambda e, pA=pA: e.tensor_tensor(out=att[:], in0=pA[0:CS, 0:CS], in1=self.U[0:CS, 0:CS], op=ALU.mult), reads=[pA.b, self.U.b], writes=[att.b])
                    pO = ps[pc % 8]; pc += 1
                    s.op("pe", lambda e, pO=pO, n=n, gn=gn: e.matmul(pO[0:CS, 0:128], att[:], vb[:, n, :], start=True, stop=(gn == 0)),
                         reads=[att.b, vb.b], writes=[pO.b])
                    if gn > 0:
                        s.op("pe", lambda e, pO=pO, n=n: e.matmul(pO[0:CS, 0:128], qb[:, n, :], stb[:], start=False, stop=True),
                             reads=[qb.b, stb.b], accs=[pO.b])
                    s.op("act", lambda e, pO=pO, n=n: e.copy(o_sb[:, n, :], pO[0:CS, 0:128]), reads=[pO.b], writes=[o_sb.b] if n == 0 else (), accs=[o_sb.b] if n else ())
                    if gn < NCHT - 1:
                        pS = ps[pc % 8]; pc += 1
                        s.op("pe", lambda e, pS=pS, n=n: e.matmul(pS[:, 0:128], kend[:, n, :], vb[:, n, :], start=True, stop=True),
                             reads=[kend.b, vb.b], writes=[pS.b])
                        if gn == 0:
                            s.op("dve", lambda e, pS=pS: e.tensor_copy(state[:], pS[:, 0:128]), reads=[pS.b], writes=[state.b])
                        else:
                            s.op("dve", lambda e, pS=pS, n=n: e.scalar_tensor_tensor(out=state[:], in0=state[:], scalar=bend[:, n:n + 1], in1=pS[:, 0:128],
                                                                                 op0=ALU.mult, op1=ALU.add), reads=[state.b, bend.b, pS.b], accs=[state.b])
                        s.op("act", lambda e: e.copy(stb[:], state[:]), reads=[state.b], writes=[stb.b])
                s.op("dve", lambda e: e.tensor_tensor(out=lf_tm[:], in0=o_sb[:], in1=o_sb[:], op=ALU.mult), reads=[o_sb.b], writes=[lf_tm.b])
                s.op("dve", lambda e: e.tensor_reduce(out=ssq[:], in_=lf_tm[:], axis=AX.X, op=ALU.add), reads=[lf_tm.b], writes=[ssq.b])
                self.rsqrt_mean(ssq[:], ssq[:], ssq.b, ssq.b, 128, npart=CS)
                s.op("dve", lambda e: e.tensor_tensor(out=o_sb[:], in0=o_sb[:], in1=ssq[:].unsqueeze(2).to_broadcast([CS, NCH, 128]), op=ALU.mult),
                     reads=[o_sb.b, ssq.b], accs=[o_sb.b])
                s.op("dve", lambda e: e.tensor_tensor(out=o_sb[:], in0=o_sb[:], in1=hgB[:].unsqueeze(1).to_broadcast([CS, NCH, 128]), op=ALU.mult),
                     reads=[o_sb.b, hgB.b], accs=[o_sb.b])
                s.op("dve", lambda e: e.tensor_tensor(out=o_sb[:], in0=o_sb[:], in1=g_tm[:], op=ALU.mult), reads=[o_sb.b, g_tm.b], accs=[o_sb.b])
                for n8 in range(0, NCH, NPB):
                    p = ps[pc % 8]; pc += 1
                    nn = min(NPB, NCH - n8)
                    for n in range(n8, n8 + nn):
                        s.op("pe", lambda e, p=p, n=n, n8=n8: e.transpose(p[:, (n - n8) * CS:(n - n8 + 1) * CS], o_sb[:, n, :], self.identf[0:CS, 0:CS]),
                             reads=[o_sb.b, self.identf.b], writes=[p.b] if n == n8 else (), accs=[p.b] if n != n8 else ())
                    s.op("act", lambda e, p=p, n8=n8, nn=nn: e.copy(yaT[:, n8 * CS:(n8 + nn) * CS], p[:, 0:nn * CS]), reads=[p.b],
                         writes=[yaT.b] if n8 == 0 else (), accs=[yaT.b] if n8 else ())
                s.dma(self.mx["yT"][0][hs, ts], yaT[:], reads=[yaT.b], dsem=yaT.d)
            s.flush()


def build_program(cfg):
    k = K(cfg)
    D, S, DEPTH, DFF, NBE = cfg["D"], cfg["S"], cfg["DEPTH"], cfg["DFF"], cfg["NBE"]
    DIN = 8016 + 3 * D
    for name, shape in (("x", (NBE, S, D)), ("c", (NBE, D)), ("w_c_down", (D, 256)), ("w_c_up", (DEPTH, 256, 9 * D)),
                        ("norm_gains", (DEPTH, 6, D)), ("w_in", (DEPTH, D, DIN)), ("lb_logits", (DEPTH, 1024)),
                        ("hgrn_norm", (DEPTH, 128)), ("kv_norm", (DEPTH, 512)), ("w_kv_up", (DEPTH, 512, 256)),
                        ("rel_table", (32, 24)), ("sinks", (DEPTH, 16)), ("w_branch", (DEPTH, 3, 1024, D)),
                        ("w_out", (DEPTH, D, D)), ("ffn1_in", (DEPTH, D, 2 * DFF)), ("ffn1_out", (DEPTH, DFF, D)),
                        ("ffn2_in", (DEPTH, D, 2 * DFF)), ("ffn2_out", (DEPTH, DFF, D)), ("t5_onehot", (33, 766))):
        k.ext(name, shape)
    out = k.ext("out", (NBE, S, D), kind="ExternalOutput")
    xres = k.scratch("xres", (S, D))
    growd = k.scratch("grow_d", (3, 1, D))
    k.mixer_scratch()
    k.consts()
    k.mod_setup()
    k.setup_phase()
    for b in range(NBE):
        k.cond_phase(k.dram["c"][b])
        for l in range(DEPTH):
            k.mod_phase(l, growd)
            k.ffn_phase(l, 1, k.dram["x"][b] if l == 0 else xres, xres, k.AB[0], k.AB[1], growd[0])
            k.proj_phase(l, xres, k.AB[2], k.AB[3])
            k.hgrn_phase(l)
            k.dsa_phase(l)
            k.swa_phase(l)
            k.merge_phase(l, xres, xres, growd[1])
            k.ffn_phase(l, 2, xres, out[b] if l == DEPTH - 1 else xres, k.AB[4], k.AB[5], growd[2])
    return k


def t5_bucket_np(d):
    d = np.maximum(d, 0)
    df = np.maximum(d, 1).astype(np.float32)
    large = 16 + (np.log(df / np.float32(16)) / np.float32(math.log(128 / 16)) * np.float32(16)).astype(np.int32)
    large = np.minimum(large, 31)
    return np.where(d < 16, d, large)
def t5_onehot():
    oh = np.zeros((33, 2, 383), np.float32)
    for i in range(383):
        dist = 255 - i
        b = int(t5_bucket_np(np.array([dist]))[0])
        if 0 <= dist < 128:
            oh[b, 0, i] = 1
        else:
            oh[32, 0, i] = 1
        if dist >= 0:
            oh[b, 1, i] = 1
        else:
            oh[32, 1, i] = 1
    return oh.reshape(33, 766)


N_CORES = 8


def run_cores(inputs, n_cores, batch_ids):
    cfg = dict(FULL)
    cfg["NBE"] = len(batch_ids[0])
    Sched.SERIAL = False
    prog = build_program(cfg)
    oh = t5_onehot()
    shared = {kk: np.ascontiguousarray(v, dtype=np.float32) for kk, v in inputs.items() if kk not in ("x", "c")}
    in_maps = []
    for ids in batch_ids:
        m = dict(shared)
        m["x"] = np.ascontiguousarray(inputs["x"][ids], dtype=np.float32)
        m["c"] = np.ascontiguousarray(inputs["c"][ids], dtype=np.float32)
        m["t5_onehot"] = oh
        in_maps.append(m)
    res = run_bass_kernel_spmd(prog.nc, in_maps, core_ids=list(range(n_cores)))
    return [np.asarray(r["out"]) for r in res.results]


def kernel(**inputs):
    B = inputs["x"].shape[0]
    per = B // N_CORES
    batch_ids = [list(range(c * per, (c + 1) * per)) for c in range(N_CORES)]
    outs = run_cores(inputs, N_CORES, batch_ids)
    return np.concatenate(outs, axis=0).astype(np.float32)
```

```python
import math
import contextlib
import numpy as np
import concourse.bass as bass
import concourse.mybir as mybir
from concourse.bass_utils import run_bass_kernel_spmd

DT = mybir.dt
F32, BF16 = DT.float32, DT.bfloat16
ALU = mybir.AluOpType
AF = mybir.ActivationFunctionType
AX = mybir.AxisListType

FULL = dict(D=4096, B=8, S=2048, DEPTH=4, DFF=4096, TOPK=256, NCORES=8)


class DSem:
    def __init__(self, sem):
        self.sem = sem
        self.count = 0


class Buf:
    def __init__(self, name):
        self.name = name
        self.writes = {}
        self.reads = {}


ENGS = ("pe", "act", "dve", "pool", "sp")


class Sched:
    SERIAL = False
    DMA_WINDOW = 8

    def __init__(self, nc, stack):
        self.nc = nc
        self.stack = stack
        self.h = dict(pe=nc.tensor, act=nc.scalar, dve=nc.vector, pool=nc.gpsimd, sp=nc.sync)
        self.q = {e: [] for e in ENGS}
        self.sem = {e: stack.enter_context(nc.semaphore("c_" + e)) for e in ENGS if e != "sp"}
        self.cnt = {e: 0 for e in ENGS}
        self.waited = {e: {} for e in ENGS}
        self.dsems = []
        self.dnext = 0
        self.ninst = 0
        self.dma_hist = []

    def get_dsem(self):
        if self.dnext == len(self.dsems):
            self.dsems.append(DSem(self.stack.enter_context(self.nc.semaphore("d%d" % self.dnext))))
        self.dnext += 1
        return self.dsems[self.dnext - 1]

    def _wait(self, eng, key, val):
        if isinstance(key, DSem):
            val = key.count
            sem = key.sem
        else:
            if key == eng and eng in ("pe", "sp"):
                return
            sem = self.sem[key]
        w = self.waited[eng]
        if w.get(key, 0) >= val:
            return
        w[key] = val
        h = self.h[eng]
        self.q[eng].append(lambda e, sem=sem, val=val: e.wait_ge(sem, val))

    def op(self, eng, fn, reads=(), writes=(), accs=(), dsem=None):
        deps = []
        for b in reads:
            deps.extend(b.writes.items())
        for b in writes:
            deps.extend(b.writes.items())
            deps.extend(b.reads.items())
        for b in accs:
            deps.extend(b.reads.items())
        is_dma = dsem is not None
        if Sched.SERIAL is True or (Sched.SERIAL and eng in Sched.SERIAL):
            for k2 in ENGS:
                if k2 != "sp" and self.cnt[k2] > 0:
                    self._wait(eng, k2, self.cnt[k2])
            for d in self.dsems:
                if d.count:
                    self._wait(eng, d, d.count)
        for key, val in deps:
            if not is_dma and key == eng:
                if not any(key in b.writes for b in reads):
                    continue
            self._wait(eng, key, val)
        h = self.h[eng]
        if is_dma:
            if len(self.dma_hist) >= Sched.DMA_WINDOW:
                self._wait(eng, self.dma_hist[-Sched.DMA_WINDOW], 0)
            self.dma_hist.append(dsem)
            del self.dma_hist[:-32]
            dsem.count += 16
            tok = (dsem, dsem.count)
            sem, inc = dsem.sem, 16
        else:
            self.cnt[eng] += 1
            tok = (eng, self.cnt[eng])
            sem, inc = self.sem[eng], 1
        self.q[eng].append(lambda e, fn=fn, sem=sem, inc=inc: fn(e).then_inc(sem, inc))
        self.ninst += 1
        for b in reads:
            b.reads[tok[0]] = tok[1]
        for b in writes:
            b.writes = {tok[0]: tok[1]}
            b.reads = {}
        for b in accs:
            b.writes[tok[0]] = tok[1]
            b.reads = {}

    def dma(self, out, in_, reads=(), writes=(), accs=(), dsem=None, eng="sp", **kw):
        self.op(eng, lambda h: h.dma_start(out=out, in_=in_, **kw), reads=reads, writes=writes,
                accs=accs, dsem=dsem)

    def barrier(self):
        for e in ENGS:
            for k in ENGS:
                if k != "sp" and k != e and self.cnt[k] > 0:
                    self._wait(e, k, self.cnt[k])
            for d in self.dsems:
                if d.count:
                    self._wait(e, d, d.count)

    def flush(self):
        self.barrier()
        with self.nc.Block() as block:
            self._flush(block)
        self.q = {e: [] for e in ENGS}
        self.dnext = 0

    def _flush(self, block):
        q = self.q

        @block.tensor
        def _(e):
            for f in q["pe"]:
                f(e)

        @block.scalar
        def _(e):
            for f in q["act"]:
                f(e)

        @block.vector
        def _(e):
            for f in q["dve"]:
                f(e)

        @block.gpsimd
        def _(e):
            for f in q["pool"]:
                f(e)

        @block.sync
        def _(e):
            for f in q["sp"]:
                f(e)


class T:
    uid = 0

    def __init__(self, sched, stack, name, shape, dtype, psum=False, dma=False):
        nc = sched.nc
        alloc = nc.psum_tensor if psum else nc.sbuf_tensor
        T.uid += 1
        name = "%s_%d" % (name, T.uid)
        self.t = stack.enter_context(alloc(name, shape, dtype))
        self.b = Buf(name)
        self.d = sched.get_dsem() if dma else None

    def __getitem__(self, idx):
        return self.t[idx]


class K:
    def __init__(self, cfg):
        self.cfg = cfg
        self.nc = bass.Bass("TRN2", target_bir_lowering=False)
        self.stack = contextlib.ExitStack()
        self.s = Sched(self.nc, self.stack)
        self.dram = {}
        self.mir = {}

    def ext(self, name, shape, kind="ExternalInput"):
        self.dram[name] = self.nc.dram_tensor(name, list(shape), F32, kind=kind).ap()
        return self.dram[name]

    def scratch(self, name, shape, dtype=F32):
        if name in self.cfg.get("dbg", ()):
            ap = self.nc.dram_tensor(name, list(shape), dtype, kind="ExternalOutput").ap()
        elif name in self.cfg.get("ext_in", ()):
            ap = self.nc.dram_tensor(name, list(shape), dtype, kind="ExternalInput").ap()
        else:
            ap = self.nc.dram_tensor(name, list(shape), dtype).ap()
        self.dram[name] = ap
        return ap

    def consts(self):
        s, st = self.s, self.stack
        self.identb = T(s, st, "identb", [128, 128], BF16)
        self.identf = T(s, st, "identf", [128, 128], F32)
        self.ones_row = T(s, st, "ones_row", [1, 128], F32)
        s.op("pool", lambda e: e.memset(self.identf[:], 0.0), writes=[self.identf.b])
        s.op("pool", lambda e: e.affine_select(out=self.identf[:], in_=self.identf[:], pattern=[[-1, 128]],
                                               compare_op=ALU.not_equal, fill=1.0, base=0, channel_multiplier=1),
             reads=[self.identf.b], accs=[self.identf.b])
        s.op("dve", lambda e: e.tensor_copy(self.identb[:], self.identf[:]), reads=[self.identf.b], writes=[self.identb.b])
        s.op("pool", lambda e: e.memset(self.ones_row[:], 1.0), writes=[self.ones_row.b])
        self.eps = T(s, st, "eps", [128, 1], F32)
        s.op("pool", lambda e: e.memset(self.eps[:], 1e-6), writes=[self.eps.b])
        s.flush()

    def scramble(self):
        s = self.s
        with contextlib.ExitStack() as ph:
            big = T(s, ph, "scr", [128, 40000], F32)
            ps = [T(s, ph, "scrp%d" % i, [128, 512], F32, psum=True) for i in range(8)]
            s.op("pool", lambda e: e.memset(big[:, 0:20000], 12345.0), writes=[big.b])
            s.op("dve", lambda e: e.memset(big[:, 20000:40000], -54321.0), accs=[big.b])
            for p in ps:
                s.op("dve", lambda e, p=p: e.memset(p[:], 777.0), writes=[p.b])
            s.flush()

    class WStream:
        ncast = 0

        def __init__(self, k, ph, nbuf=3, kc=4, n=512):
            self.k = k
            s = k.s
            self.raw = [T(s, ph, "wraw%d" % i, [128, kc, n], F32, dma=True) for i in range(nbuf)]
            self.bf = [T(s, ph, "wbf%d" % i, [128, kc, n], BF16, dma=True) for i in range(nbuf)]
            self.mb = Buf("wmirror")
            self.pm = None
            self.use_mirror = False
            self.i = 0
            self.pending = []
            self.loaded = 0
            self.nbuf = nbuf

        def plan(self, aps, maps=None, use_mirror=False):
            self.pending = list(aps)
            self.pm = list(maps) if maps is not None else None
            um = bool(use_mirror and maps is not None)
            if um and self.mb.writes:
                for d in list(self.mb.writes):
                    self.k.s._wait("sp", d, 0)
                self.mb.writes = {}
            self.use_mirror = um
            self.issued = 0
            self.taken = 0
            for _ in range(min(self.nbuf - 1, len(self.pending))):
                self._issue()

        def _issue(self):
            ap = self.pending[self.issued]
            j = (self.i + self.issued) % self.nbuf
            raw = self.raw[j]
            kk, nn = ap.shape
            kc = kk // 128
            if self.use_mirror:
                bf = self.bf[j]
                self.k.s.dma(bf[:, 0:kc, 0:nn], self.pm[self.issued].rearrange("(c p) n -> p c n", p=128),
                             writes=[bf.b], dsem=bf.d)
                self.issued += 1
                return
            self.k.s.dma(raw[:, 0:kc, 0:nn], ap.rearrange("(c p) n -> p c n", p=128), writes=[raw.b], dsem=raw.d)
            self.issued += 1

        def next(self):
            s = self.k.s
            if self.issued < len(self.pending):
                self._issue()
            ap = self.pending[self.taken]
            j = (self.i + self.taken) % self.nbuf
            raw, bf = self.raw[j], self.bf[j]
            kk, nn = ap.shape
            kc = kk // 128
            if not self.use_mirror:
                K.WStream.ncast += 1
                if K.WStream.ncast % 2:
                    s.op("dve", lambda e: e.tensor_copy(bf[:, 0:kc, 0:nn], raw[:, 0:kc, 0:nn]), reads=[raw.b], writes=[bf.b])
                else:
                    s.op("act", lambda e: e.copy(bf[:, 0:kc, 0:nn], raw[:, 0:kc, 0:nn]), reads=[raw.b], writes=[bf.b])
                if self.pm is not None:
                    s.dma(self.pm[self.taken].rearrange("(c p) n -> p c n", p=128), bf[:, 0:kc, 0:nn], reads=[bf.b],
                          accs=[self.mb], dsem=bf.d)
            self.taken += 1
            if self.taken == len(self.pending):
                self.i = (self.i + self.taken) % self.nbuf
            return bf, kc, nn


    def norm_block(self, xin, tok0, NT, BIG, aT, hT, xn, ss, rstd, ps, A, B, pctr):
        s, D = self.s, self.cfg["D"]
        KC = D // 128
        if True:
            if True:
                for t in range(NT):
                    s.dma(BIG[:, t, :], xin[tok0 + t * 128: tok0 + (t + 1) * 128, :], accs=[BIG.b] if t else (),
                          writes=() if t else [BIG.b], dsem=BIG.d)
                for t in range(NT):
                    s.op("dve", lambda e, t=t: e.tensor_tensor(out=xn, in0=BIG[:, t, :], in1=BIG[:, t, :], op=ALU.mult),
                         reads=[BIG.b], writes=[aT.b])
                    s.op("dve", lambda e, t=t: e.reduce_sum(out=ss[:, t:t + 1], in_=xn, axis=AX.X),
                         reads=[aT.b], writes=[ss.b] if t == 0 else (), accs=[ss.b] if t else ())
                    self.rsqrt_mean(rstd[:, t:t + 1], ss[:, t:t + 1], rstd.b, ss.b, D)
                    s.op("dve", lambda e, t=t: e.tensor_scalar(out=xn, in0=BIG[:, t, :], scalar1=rstd[:, t:t + 1], scalar2=None,
                                                          op0=ALU.mult), reads=[BIG.b, rstd.b], writes=[aT.b])
                    for g in range(0, KC, 8):
                        p = ps[pctr % 8]
                        pctr += 1
                        pb = p.t[:].bitcast(BF16)
                        nch = min(8, KC - g)
                        for c in range(nch):
                            s.op("pe", lambda e, c=c, g=g, pb=pb: e.transpose(pb[:, c * 128:(c + 1) * 128], xn[:, (g + c) * 128:(g + c + 1) * 128], self.identb[:]),
                                 reads=[aT.b, self.identb.b], writes=[p.b] if c == 0 else (), accs=[p.b] if c else ())
                        for c in range(nch):
                            s.op("act", lambda e, c=c, g=g, pb=pb, t=t: e.activation(out=hT[:, g + c, t * 128:(t + 1) * 128], in_=pb[:, c * 128:(c + 1) * 128],
                                                                                 func=AF.Identity, scale=A[:, g + c:g + c + 1], bias=B[:, g + c:g + c + 1]),
                                 reads=[p.b, A.b, B.b], writes=[hT.b] if (t == 0 and g == 0 and c == 0) else (),
                                 accs=() if (t == 0 and g == 0 and c == 0) else [hT.b])
        return pctr

    def ffn_phase(self, l, which, xin, xout, A, B, Gsrc):
        cfg, s = self.cfg, self.s
        D, DFF, S = cfg["D"], cfg["DFF"], cfg["S"]
        KC, FC = D // 128, DFF // 128
        TB = min(512, S)
        NT = TB // 128
        w_in = self.dram["ffn%d_in" % which][l]
        w_out = self.dram["ffn%d_out" % which][l]
        m_in = self.mir["ffn%d_in" % which][l]
        m_out = self.mir["ffn%d_out" % which][l]
        with contextlib.ExitStack() as ph:
            hT = T(s, ph, "hT", [128, KC, TB], BF16, dma=True)
            aT = T(s, ph, "aT", [128, FC, TB], BF16)
            BIG = T(s, ph, "BIG", [128, NT, D], F32, dma=True)
            G = T(s, ph, "G", [128, D], F32)
            grow = T(s, ph, "grow", [1, D], F32, dma=True)
            ss = T(s, ph, "ss", [128, NT], F32)
            rstd = T(s, ph, "rstd", [128, NT], F32)
            ssy = T(s, ph, "ssy", [128, NT, D // 512], F32)
            junk = T(s, ph, "junk", [128, 512], F32)
            su = [T(s, ph, "su%d" % i, [128, TB], F32) for i in range(2)]
            ps = [T(s, ph, "ps%d" % i, [128, 512], F32, psum=True) for i in range(8)]
            ws = K.WStream(self, ph)
            xn_v = aT.t[:, 0:KC // NT if False else FC, :]
            xn = aT.t[:].rearrange("p c t -> p (c t)")[:, 0:D]
            xh = hT.t[:].rearrange("p c t -> p (c t)").bitcast(F32)[:, 0:D]
            s.dma(grow[:], Gsrc, writes=[grow.b], dsem=grow.d)
            for n in range(D // 512):
                p = ps[n % 8]
                s.op("pe", lambda e, p=p, n=n: e.matmul(p[:], self.ones_row[:], grow[:, n * 512:(n + 1) * 512], start=True, stop=True),
                     reads=[grow.b, self.ones_row.b], writes=[p.b])
                s.op("act", lambda e, p=p, n=n: e.copy(G[:, n * 512:(n + 1) * 512], p[:]), reads=[p.b], accs=[G.b])
            if "dbg_G" in cfg.get("dbg", ()):
                dd = self.scratch("dbg_G", (128, D))
                s.dma(dd, G[:], reads=[G.b], dsem=BIG.d)
            pctr = 0
            for tb in range(S // TB):
                tok0 = tb * TB
                pctr = self.norm_block(xin, tok0, NT, BIG, aT, hT, xn, ss, rstd, ps, A, B, pctr)
                for fg in range(DFF // 512):
                    aps, mps = [], []
                    for half in range(2):
                        for ks in range(D // 512):
                            aps.append(w_in[ks * 512:(ks + 1) * 512, half * DFF + fg * 512: half * DFF + (fg + 1) * 512])
                            mps.append(m_in[ks * 512:(ks + 1) * 512, half * DFF + fg * 512: half * DFF + (fg + 1) * 512])
                    ws.plan(aps, mps, tb > 0)
                    for half in range(2):
                        for ks in range(D // 512):
                            bf, kc, nn = ws.next()
                            for k4 in range(kc):
                                kk = ks * 4 + k4
                                for c in range(4):
                                    p = ps[half * 4 + c]
                                    first = kk == 0
                                    s.op("pe", lambda e, p=p, bf=bf, k4=k4, c=c, kk=kk: e.matmul(p[:, 0:TB], bf[:, k4, c * 128:(c + 1) * 128], hT[:, kk, :],
                                                                                              start=(kk == 0), stop=(kk == KC - 1)),
                                         reads=[bf.b, hT.b], writes=[p.b] if first else (), accs=() if first else [p.b])
                    for c in range(4):
                        sb = su[c % 2]
                        s.op("act", lambda e, c=c, sb=sb: e.activation(out=sb[:], in_=ps[c][:, 0:TB], func=AF.Silu), reads=[ps[c].b], writes=[sb.b])
                        first = (fg == 0 and c == 0)
                        s.op("dve", lambda e, c=c, sb=sb, fg=fg: e.tensor_tensor(out=aT[:, fg * 4 + c, :], in0=sb[:], in1=ps[4 + c][:, 0:TB], op=ALU.mult),
                             reads=[sb.b, ps[4 + c].b], writes=[aT.b] if first else (), accs=() if first else [aT.b])
                for ng in range(D // 512):
                    ws.plan([w_out[ks * 512:(ks + 1) * 512, ng * 512:(ng + 1) * 512] for ks in range(DFF // 512)],
                            [m_out[ks * 512:(ks + 1) * 512, ng * 512:(ng + 1) * 512] for ks in range(DFF // 512)], tb > 0)
                    pb0 = (ng % 2) * 4
                    for ks in range(DFF // 512):
                        bf, kc, nn = ws.next()
                        for k4 in range(kc):
                            kk = ks * 4 + k4
                            for t in range(NT):
                                p = ps[pb0 + t]
                                first = kk == 0
                                s.op("pe", lambda e, p=p, bf=bf, k4=k4, t=t, kk=kk: e.matmul(p[:], aT[:, kk, t * 128:(t + 1) * 128], bf[:, k4, :],
                                                                                          start=(kk == 0), stop=(kk == FC - 1)),
                                     reads=[bf.b, aT.b], writes=[p.b] if first else (), accs=() if first else [p.b])
                    for t in range(NT):
                        p = ps[pb0 + t]
                        first = (ng == 0 and t == 0)
                        s.op("act", lambda e, p=p, t=t, ng=ng: e.copy(BIG[:, t, ng * 512:(ng + 1) * 512], p[:]), reads=[p.b],
                             writes=[BIG.b] if first else (), accs=() if first else [BIG.b])
                        s.op("dve", lambda e, t=t, ng=ng: e.tensor_tensor(out=junk[:], in0=BIG[:, t, ng * 512:(ng + 1) * 512],
                                                                       in1=BIG[:, t, ng * 512:(ng + 1) * 512], op=ALU.mult),
                             reads=[BIG.b], writes=[junk.b])
                        s.op("dve", lambda e, t=t, ng=ng: e.reduce_sum(out=ssy[:, t, ng:ng + 1], in_=junk[:], axis=AX.X),
                             reads=[junk.b], accs=[ssy.b])
                if "dbg_y" in cfg.get("dbg", ()) and tb == 0:
                    dd = self.scratch("dbg_y", (128, NT * D))
                    s.dma(dd, BIG[:].rearrange("p t d -> p (t d)"), reads=[BIG.b], dsem=BIG.d)
                    dd2 = self.scratch("dbg_a", (128, FC * TB), BF16)
                    s.dma(dd2, aT[:].rearrange("p c t -> p (c t)"), reads=[aT.b], dsem=BIG.d)
                for t in range(NT):
                    s.op("dve", lambda e, t=t: e.reduce_sum(out=ss[:, t:t + 1], in_=ssy[:, t, :], axis=AX.X), reads=[ssy.b], accs=[ss.b])
                    self.rsqrt_mean(rstd[:, t:t + 1], ss[:, t:t + 1], rstd.b, ss.b, D)
                    s.dma(xh, xin[tok0 + t * 128: tok0 + (t + 1) * 128, :], writes=[hT.b], dsem=hT.d)
                    s.op("dve", lambda e, t=t: e.scalar_tensor_tensor(out=BIG[:, t, :], in0=BIG[:, t, :], scalar=rstd[:, t:t + 1], in1=G[:],
                                                                 op0=ALU.mult, op1=ALU.mult), reads=[BIG.b, rstd.b, G.b], accs=[BIG.b])
                    s.op("dve", lambda e, t=t: e.tensor_tensor(out=BIG[:, t, :], in0=BIG[:, t, :], in1=xh, op=ALU.add),
                         reads=[BIG.b, hT.b], accs=[BIG.b])
                    s.dma(xout[tok0 + t * 128: tok0 + (t + 1) * 128, :], BIG[:, t, :], reads=[BIG.b], dsem=BIG.d)
            s.flush()


    def merge_phase(self, l, xin, xout, Gsrc):
        cfg, s = self.cfg, self.s
        D, S = cfg["D"], cfg["S"]
        KC = D // 128
        TB = min(512, S)
        NT = TB // 128
        w_out = self.dram["w_out"][l]
        w_br = self.dram["w_branch"][l]
        m_out = self.mir["w_out"][l]
        m_br = self.mir["w_branch"][l]
        gT = self.mx["gT"]
        yT = self.mx["yT"]
        with contextlib.ExitStack() as ph:
            mT = T(s, ph, "mT", [128, KC, TB], BF16)
            BIG = T(s, ph, "BIG", [128, NT, D], F32, dma=True)
            G = T(s, ph, "G", [128, D], F32)
            grow = T(s, ph, "grow", [1, D], F32, dma=True)
            ss = T(s, ph, "ss", [128, NT], F32)
            rstd = T(s, ph, "rstd", [128, NT], F32)
            ssy = T(s, ph, "ssy", [128, NT, D // 512], F32)
            junk = T(s, ph, "junk", [128, 512], F32)
            ybuf = T(s, ph, "ybuf", [128, 3, 8, TB], BF16, dma=True)
            gts = [T(s, ph, "gts%d" % i, [128, TB], F32, dma=True) for i in range(4)]
            macc = T(s, ph, "macc", [128, 4, TB], F32)
            tmp = T(s, ph, "tmp", [128, TB], F32)
            ps = [T(s, ph, "ps%d" % i, [128, 512], F32, psum=True) for i in range(8)]
            ws = K.WStream(self, ph, nbuf=2)
            xh = ybuf.t[:].rearrange("p a c t -> p (a c t)").bitcast(F32)[:, 0:D]
            s.dma(grow[:], Gsrc, writes=[grow.b], dsem=grow.d)
            for n in range(D // 512):
                p = ps[n % 8]
                s.op("pe", lambda e, p=p, n=n: e.matmul(p[:], self.ones_row[:], grow[:, n * 512:(n + 1) * 512], start=True, stop=True),
                     reads=[grow.b, self.ones_row.b], writes=[p.b])
                s.op("act", lambda e, p=p, n=n: e.copy(G[:, n * 512:(n + 1) * 512], p[:]), reads=[p.b], accs=[G.b])
            gi = 0
            grp = 0
            for tb in range(S // TB):
                tok0 = tb * TB
                for br in range(3):
                    s.dma(ybuf[:, br, :, :], yT[br].rearrange("(c p) s -> p c s", p=128)[:, :, tok0:tok0 + TB],
                          writes=[ybuf.b] if br == 0 else (), accs=[ybuf.b] if br else (), dsem=ybuf.d)
                for dg in range(D // 512):
                    for br in range(3):
                        ws.plan([w_br[br, ks * 512:(ks + 1) * 512, dg * 512:(dg + 1) * 512] for ks in range(2)],
                                [m_br[br, ks * 512:(ks + 1) * 512, dg * 512:(dg + 1) * 512] for ks in range(2)], tb > 0)
                        pb0 = (grp % 2) * 4
                        grp += 1
                        for ks in range(2):
                            bf, kc, nn = ws.next()
                            for k4 in range(kc):
                                kk = ks * 4 + k4
                                for c in range(4):
                                    p = ps[pb0 + c]
                                    first = kk == 0
                                    s.op("pe", lambda e, p=p, bf=bf, k4=k4, c=c, kk=kk, br=br: e.matmul(
                                        p[:, 0:TB], bf[:, k4, c * 128:(c + 1) * 128], ybuf[:, br, kk, :], start=(kk == 0), stop=(kk == 7)),
                                        reads=[bf.b, ybuf.b], writes=[p.b] if first else (), accs=() if first else [p.b])
                        for c in range(4):
                            p = ps[pb0 + c]
                            dch = dg * 4 + c
                            gt = gts[gi % 4]
                            gi += 1
                            s.dma(gt[:], gT[br * D + dch * 128: br * D + (dch + 1) * 128, tok0:tok0 + TB], writes=[gt.b], dsem=gt.d)
                            if br == 0:
                                s.op("dve", lambda e, p=p, c=c, gt=gt: e.tensor_tensor(out=macc[:, c, :], in0=p[:, 0:TB], in1=gt[:], op=ALU.mult),
                                     reads=[p.b, gt.b], writes=[macc.b] if c == 0 else (), accs=[macc.b] if c else ())
                            else:
                                s.op("dve", lambda e, p=p, gt=gt: e.tensor_tensor(out=tmp[:], in0=p[:, 0:TB], in1=gt[:], op=ALU.mult),
                                     reads=[p.b, gt.b], writes=[tmp.b])
                                s.op("dve", lambda e, c=c: e.tensor_tensor(out=macc[:, c, :], in0=macc[:, c, :], in1=tmp[:], op=ALU.add),
                                     reads=[macc.b, tmp.b], accs=[macc.b])
                            if br == 2:
                                first = (dg == 0 and c == 0)
                                s.op("act", lambda e, c=c, dch=dch: e.copy(mT[:, dch, :], macc[:, c, :]), reads=[macc.b],
                                     writes=[mT.b] if first else (), accs=() if first else [mT.b])
                for ng in range(D // 512):
                    ws.plan([w_out[ks * 512:(ks + 1) * 512, ng * 512:(ng + 1) * 512] for ks in range(D // 512)],
                            [m_out[ks * 512:(ks + 1) * 512, ng * 512:(ng + 1) * 512] for ks in range(D // 512)], tb > 0)
                    pb0 = (ng % 2) * 4
                    for ks in range(D // 512):
                        bf, kc, nn = ws.next()
                        for k4 in range(kc):
                            kk = ks * 4 + k4
                            for t in range(NT):
                                p = ps[pb0 + t]
                                first = kk == 0
                                s.op("pe", lambda e, p=p, bf=bf, k4=k4, t=t, kk=kk: e.matmul(p[:], mT[:, kk, t * 128:(t + 1) * 128], bf[:, k4, :],
                                                                                          start=(kk == 0), stop=(kk == KC - 1)),
                                     reads=[bf.b, mT.b], writes=[p.b] if first else (), accs=() if first else [p.b])
                    for t in range(NT):
                        p = ps[pb0 + t]
                        first = (ng == 0 and t == 0)
                        s.op("act", lambda e, p=p, t=t, ng=ng: e.copy(BIG[:, t, ng * 512:(ng + 1) * 512], p[:]), reads=[p.b],
                             writes=[BIG.b] if first else (), accs=() if first else [BIG.b])
                        s.op("dve", lambda e, t=t, ng=ng: e.tensor_tensor(out=junk[:], in0=BIG[:, t, ng * 512:(ng + 1) * 512],
                                                                       in1=BIG[:, t, ng * 512:(ng + 1) * 512], op=ALU.mult),
                             reads=[BIG.b], writes=[junk.b])
                        s.op("dve", lambda e, t=t, ng=ng: e.reduce_sum(out=ssy[:, t, ng:ng + 1], in_=junk[:], axis=AX.X),
                             reads=[junk.b], accs=[ssy.b])
                for t in range(NT):
                    s.op("dve", lambda e, t=t: e.reduce_sum(out=ss[:, t:t + 1], in_=ssy[:, t, :], axis=AX.X), reads=[ssy.b], accs=[ss.b])
                    self.rsqrt_mean(rstd[:, t:t + 1], ss[:, t:t + 1], rstd.b, ss.b, D)
                    s.dma(xh, xin[tok0 + t * 128: tok0 + (t + 1) * 128, :], writes=[ybuf.b], dsem=ybuf.d)
                    s.op("dve", lambda e, t=t: e.scalar_tensor_tensor(out=BIG[:, t, :], in0=BIG[:, t, :], scalar=rstd[:, t:t + 1], in1=G[:],
                                                                 op0=ALU.mult, op1=ALU.mult), reads=[BIG.b, rstd.b, G.b], accs=[BIG.b])
                    s.op("dve", lambda e, t=t: e.tensor_tensor(out=BIG[:, t, :], in0=BIG[:, t, :], in1=xh, op=ALU.add),
                         reads=[BIG.b, ybuf.b], accs=[BIG.b])
                    s.dma(xout[tok0 + t * 128: tok0 + (t + 1) * 128, :], BIG[:, t, :], reads=[BIG.b], dsem=BIG.d)
            s.flush()

    def rsqrt_mean(self, out, ssq, outb, ssb, n, npart=128):
        s = self.s
        s.op("act", lambda e: e.activation(out=out, in_=ssq, func=AF.Sqrt, scale=1.0 / n, bias=self.eps[0:npart, 0:1]), reads=[ssb, self.eps.b], accs=[outb])
        s.op("dve", lambda e: e.reciprocal(out, out), reads=[outb], accs=[outb])

    def colload(self, ph, dst_ap, src_vec, C, ps, name):
        s = self.s
        tmp = T(s, ph, "cl_" + name, [C, 128], F32, dma=True)
        s.dma(tmp[:], src_vec.rearrange("(c p) -> c p", p=128), writes=[tmp.b], dsem=tmp.d)
        s.op("pe", lambda e: e.transpose(ps[:, 0:C], tmp[:], self.identf[0:C, 0:C]), reads=[tmp.b, self.identf.b], writes=[ps.b])
        return ps

    def mod_setup(self):
        s, st, cfg = self.s, self.stack, self.cfg
        KC = cfg["D"] // 128
        self.AB = [T(s, st, "AB%d" % i, [128, KC], F32) for i in range(6)]
        self.condT = T(s, st, "condT", [128, 2], F32)

    def cond_phase(self, cvec):
        s, cfg = self.s, self.cfg
        D = cfg["D"]
        KC = D // 128
        wcd = self.dram["w_c_down"]
        with contextlib.ExitStack() as ph:
            ps = [T(s, ph, "ps%d" % i, [128, 512], F32, psum=True) for i in range(2)]
            cT = T(s, ph, "cT", [128, KC], F32)
            w = T(s, ph, "wcd", [128, KC, 256], F32, dma=True)
            self.colload(ph, None, cvec, KC, ps[0], "c")
            s.op("dve", lambda e: e.tensor_copy(cT[:], ps[0][:, 0:KC]), reads=[ps[0].b], writes=[cT.b])
            s.dma(w[:], wcd.rearrange("(c p) r -> p c r", p=128), writes=[w.b], dsem=w.d)
            for rc in range(2):
                for c in range(KC):
                    s.op("pe", lambda e, rc=rc, c=c: e.matmul(ps[1][:, rc:rc + 1], w[:, c, rc * 128:(rc + 1) * 128], cT[:, c:c + 1],
                                                         start=(c == 0), stop=(c == KC - 1)),
                         reads=[w.b, cT.b], writes=[ps[1].b] if (c == 0 and rc == 0) else (), accs=() if (c == 0 and rc == 0) else [ps[1].b])
            s.op("act", lambda e: e.activation(out=self.condT[:], in_=ps[1][:, 0:2], func=AF.Silu), reads=[ps[1].b], writes=[self.condT.b])
            s.flush()

    def mod_phase(self, l, grow_dram):
        s, cfg = self.s, self.cfg
        D = cfg["D"]
        KC = D // 128
        wcu = self.dram["w_c_up"][l]
        gains = self.dram["norm_gains"][l]
        PW = min(1024, D)
        with contextlib.ExitStack() as ph:
            ps = [T(s, ph, "ps%d" % i, [128, 512], F32, psum=True) for i in range(4)]
            piece = [T(s, ph, "wcu%d" % i, [128, 2, PW], F32, dma=True) for i in range(2)]
            gcol = T(s, ph, "gcol", [128, KC], F32)
            grow = T(s, ph, "grow", [1, D], F32, dma=True)
            gain_row = T(s, ph, "gain_row", [1, D], F32, dma=True)
            pi = 0
            for sub in range(3):
                self.colload(ph, None, gains[2 * sub], KC, ps[0], "g%d" % sub)
                s.op("dve", lambda e: e.tensor_copy(gcol[:], ps[0][:, 0:KC]), reads=[ps[0].b], writes=[gcol.b])
                for which in range(3):
                    m = 3 * sub + which
                    if which == 2:
                        s.dma(gain_row[:], gains[2 * sub + 1].rearrange("(o d) -> o d", o=1), writes=[gain_row.b], dsem=gain_row.d)
                    for pc in range(D // PW):
                        pt = piece[pi % 2]
                        pi += 1
                        s.dma(pt[:], wcu[:, m * D + pc * PW: m * D + (pc + 1) * PW].rearrange("(c p) n -> p c n", p=128),
                              writes=[pt.b], dsem=pt.d)
                        if which < 2:
                            pcol = ps[1 + which]
                            for cc in range(PW // 128):
                                c = pc * (PW // 128) + cc
                                for rc in range(2):
                                    first = (c == 0 and rc == 0)
                                    s.op("pe", lambda e, pt=pt, rc=rc, cc=cc, c=c, pcol=pcol: e.matmul(
                                        pcol[:, c:c + 1], pt[:, rc, cc * 128:(cc + 1) * 128], self.condT[:, rc:rc + 1], start=(rc == 0), stop=(rc == 1)),
                                        reads=[pt.b, self.condT.b], writes=[pcol.b] if first else (), accs=() if first else [pcol.b])
                        else:
                            for n in range(PW // 512):
                                for rc in range(2):
                                    s.op("pe", lambda e, pt=pt, rc=rc, n=n: e.matmul(ps[3][0:1, :], self.condT[:, rc:rc + 1], pt[:, rc, n * 512:(n + 1) * 512],
                                                                                 start=(rc == 0), stop=(rc == 1)),
                                         reads=[pt.b, self.condT.b], writes=[ps[3].b] if rc == 0 else (), accs=[ps[3].b] if rc else ())
                                col0 = pc * PW + n * 512
                                res = 0.5 if sub != 1 else 1.0
                                s.op("dve", lambda e, col0=col0, res=res: e.scalar_tensor_tensor(
                                    out=grow[:, col0:col0 + 512], in0=ps[3][0:1, :], scalar=res, in1=gain_row[:, col0:col0 + 512],
                                    op0=ALU.mult, op1=ALU.mult), reads=[ps[3].b, gain_row.b], accs=[grow.b])
                    if which == 0:
                        s.op("dve", lambda e, sub=sub: e.tensor_copy(self.AB[2 * sub + 1][:], ps[1][:, 0:KC]), reads=[ps[1].b], writes=[self.AB[2 * sub + 1].b])
                    elif which == 1:
                        s.op("dve", lambda e, sub=sub: e.scalar_tensor_tensor(out=self.AB[2 * sub][:], in0=ps[2][:, 0:KC], scalar=1.0, in1=gcol[:],
                                                                         op0=ALU.add, op1=ALU.mult),
                             reads=[ps[2].b, gcol.b], writes=[self.AB[2 * sub].b])
                    else:
                        s.dma(grow_dram[sub], grow[:], reads=[grow.b], dsem=grow.d)
            s.flush()

    def mixer_scratch(self):
        cfg = self.cfg
        S, D = cfg["S"], cfg["D"]
        sc = self.scratch
        self.mx = dict(
            aqT=sc("aqT", (1024, S)), afT=sc("afT", (1024, S)), af=sc("af", (S, 1024)), ai=sc("ai", (S, 1024)),
            ag=sc("ag", (S, 1024)), bqT=sc("bqT", (1024, S)), blat=sc("blat", (S, 512)), biqT=sc("biqT", (1024, S)),
            bikT=sc("bikT", (64, S)), biw=sc("biw", (S, 16)), cqT=sc("cqT", (1024, S)), ckT=sc("ckT", (128, S)),
            cv=sc("cv", (S, 128)), gT=sc("gT", (3 * D, S)),
            yT=sc("yT", (3, 1024, S), BF16),
        )

    def proj_phase(self, l, xin, A, B):
        cfg, s = self.cfg, self.s
        D, S = cfg["D"], cfg["S"]
        KC = D // 128
        TB = min(512, S)
        NT = TB // 128
        w_in = self.dram["w_in"][l]
        m_in = self.mir["w_in"][l]
        mx = self.mx
        jobs = []
        col = 0
        for name, ncols, modes in (("aq", 1024, ("fm",)), ("af", 1024, ("fm", "tm")), ("ai", 1024, ("tm",)), ("ag", 1024, ("tm",)),
                                   ("bq", 1024, ("fm",)), ("blat", 512, ("tm",)), ("biq", 1024, ("fm",)), ("bik", 64, ("fm",)),
                                   ("biw", 16, ("tm",)), ("cq", 1024, ("fm",)), ("ck", 128, ("fm",)), ("cv", 128, ("tm",)),
                                   ("g", 3 * D, ("fm",))):
            for m in modes:
                dst = mx[name + "T"] if m == "fm" else mx[name]
                jobs.append((col, ncols, m, dst, AF.Sigmoid if name == "g" else AF.Identity))
            col += ncols
        with contextlib.ExitStack() as ph:
            hT = T(s, ph, "hT", [128, KC, TB], BF16)
            xnT = T(s, ph, "xnT", [128, D], BF16)
            BIG = T(s, ph, "BIG", [128, NT, D], F32, dma=True)
            ss = T(s, ph, "ss", [128, NT], F32)
            rstd = T(s, ph, "rstd", [128, NT], F32)
            stage = [T(s, ph, "stg%d" % i, [128, 512], F32, dma=True) for i in range(4)]
            ps = [T(s, ph, "ps%d" % i, [128, 512], F32, psum=True) for i in range(8)]
            ws = K.WStream(self, ph)
            pctr = 0
            sctr = 0
            gctr = 0
            for tb in range(S // TB):
                tok0 = tb * TB
                pctr = self.norm_block(xin, tok0, NT, BIG, xnT, hT, xnT[:], ss, rstd, ps, A, B, pctr)
                if "dbg_AB" in cfg.get("dbg", ()) and tb == 0:
                    dd = self.scratch("dbg_AB", (128, 2 * KC + NT))
                    s.dma(dd[:, 0:KC], A[:], reads=[A.b], dsem=BIG.d)
                    s.dma(dd[:, KC:2 * KC], B[:], reads=[B.b], dsem=BIG.d)
                    s.dma(dd[:, 2 * KC:], rstd[:], reads=[rstd.b], dsem=BIG.d)
                if "dbg_hT" in cfg.get("dbg", ()) and tb == 0:
                    dd = self.scratch("dbg_hT", (128, KC * TB), BF16)
                    s.dma(dd, hT[:].rearrange("p c t -> p (c t)"), reads=[hT.b], dsem=BIG.d)
                for (col0, ncols, mode, dst, func) in jobs:
                    for c0 in range(0, ncols, 512):
                        w = min(512, ncols - c0)
                        ws.plan([w_in[ks * 512:(ks + 1) * 512, col0 + c0: col0 + c0 + w] for ks in range(D // 512)],
                                [m_in[ks * 512:(ks + 1) * 512, col0 + c0: col0 + c0 + w] for ks in range(D // 512)], tb > 0)
                        pb0 = (gctr % 2) * 4
                        gctr += 1
                        nchunk = (w + 127) // 128
                        for ks in range(D // 512):
                            bf, kc, nn = ws.next()
                            if "dbg_w" in cfg.get("dbg", ()) and gctr == 1 and tb == 0:
                                dd = self.scratch("dbg_w", (128, 4 * 512), BF16)
                                s.dma(dd, bf[:].rearrange("p c t -> p (c t)"), reads=[bf.b], dsem=BIG.d)
                            for k4 in range(kc):
                                kk = ks * 4 + k4
                                first = kk == 0
                                if mode == "fm":
                                    for c in range(nchunk):
                                        mc = min(128, w - c * 128)
                                        p = ps[pb0 + c]
                                        s.op("pe", lambda e, p=p, bf=bf, k4=k4, c=c, kk=kk, mc=mc: e.matmul(
                                            p[0:mc, 0:TB], bf[:, k4, c * 128:c * 128 + mc], hT[:, kk, :], start=(kk == 0), stop=(kk == KC - 1)),
                                            reads=[bf.b, hT.b], writes=[p.b] if first else (), accs=() if first else [p.b])
                                else:
                                    for t in range(NT):
                                        p = ps[pb0 + t]
                                        s.op("pe", lambda e, p=p, bf=bf, k4=k4, t=t, kk=kk, w=w: e.matmul(
                                            p[:, 0:w], hT[:, kk, t * 128:(t + 1) * 128], bf[:, k4, 0:w], start=(kk == 0), stop=(kk == KC - 1)),
                                            reads=[bf.b, hT.b], writes=[p.b] if first else (), accs=() if first else [p.b])
                        if mode == "fm":
                            for c in range(nchunk):
                                mc = min(128, w - c * 128)
                                p = ps[pb0 + c]
                                sg = stage[sctr % 4]
                                sctr += 1
                                s.op("act", lambda e, p=p, sg=sg, mc=mc, func=func: e.activation(out=sg[0:mc, 0:TB], in_=p[0:mc, 0:TB], func=func),
                                     reads=[p.b], writes=[sg.b])
                                s.dma(dst[c0 + c * 128: c0 + c * 128 + mc, tok0:tok0 + TB], sg[0:mc, 0:TB], reads=[sg.b], dsem=sg.d)
                        else:
                            for t in range(NT):
                                p = ps[pb0 + t]
                                sg = stage[sctr % 4]
                                sctr += 1
                                s.op("act", lambda e, p=p, sg=sg, w=w: e.copy(sg[:, 0:w], p[:, 0:w]), reads=[p.b], writes=[sg.b])
                                s.dma(dst[tok0 + t * 128: tok0 + (t + 1) * 128, c0:c0 + w], sg[:, 0:w], reads=[sg.b], dsem=sg.d)
            s.flush()

    PITCH = 512
    YSZ = 128 * 512 + 512

    def bcast_rows(self, dst_ap, row_ap, n, ps, npart, reads, dstb, first=True):
        s = self.s
        for c0 in range(0, n, 512):
            w = min(512, n - c0)
            s.op("pe", lambda e, c0=c0, w=w: e.matmul(ps[0:npart, 0:w], self.ones_row[0:1, 0:npart], row_ap[0:1, c0:c0 + w], start=True, stop=True),
                 reads=list(reads) + [self.ones_row.b], writes=[ps.b])
            s.op("act", lambda e, c0=c0, w=w: e.copy(dst_ap[0:npart, c0:c0 + w], ps[0:npart, 0:w]), reads=[ps.b],
                 writes=[dstb] if (first and c0 == 0) else (), accs=() if (first and c0 == 0) else [dstb])

    def setup_phase(self):
        s, st, cfg = self.s, self.stack, self.cfg
        DEPTH = cfg["DEPTH"]
        self.U = T(s, st, "U", [64, 64], F32)
        self.Mgt = T(s, st, "Mgt", [64, 64], F32)
        s.op("pool", lambda e: e.memset(self.U[:], 1.0), writes=[self.U.b])
        s.op("pool", lambda e: e.affine_select(out=self.U[:], in_=self.U[:], pattern=[[1, 64]], compare_op=ALU.is_ge, fill=0.0,
                                               base=0, channel_multiplier=-1), reads=[self.U.b], accs=[self.U.b])
        s.op("pool", lambda e: e.memset(self.Mgt[:], 1.0), writes=[self.Mgt.b])
        s.op("pool", lambda e: e.affine_select(out=self.Mgt[:], in_=self.Mgt[:], pattern=[[-1, 64]], compare_op=ALU.is_gt, fill=0.0,
                                               base=0, channel_multiplier=1), reads=[self.Mgt.b], accs=[self.Mgt.b])
        self.cmask = T(s, st, "cmask", [128, 128], F32)
        s.op("pool", lambda e: e.memset(self.cmask[:], 0.0), writes=[self.cmask.b])
        s.op("pool", lambda e: e.affine_select(out=self.cmask[:], in_=self.cmask[:], pattern=[[-1, 128]], compare_op=ALU.is_ge, fill=-1e30,
                                               base=0, channel_multiplier=1), reads=[self.cmask.b], accs=[self.cmask.b])
        lb_d = self.scratch("lb_d", (DEPTH, 1024))
        Z_d = self.scratch("Z_d", (2, 24, 384))
        self.Yc_d = self.nc.dram_tensor("Yc_d", [16 * K.YSZ], F32)
        self.Yb_d = self.nc.dram_tensor("Yb_d", [8 * K.YSZ], F32)
        with contextlib.ExitStack() as ph:
            lg = T(s, ph, "lg", [1, DEPTH, 1024], F32, dma=True)
            ex = T(s, ph, "ex", [1, DEPTH, 1024], F32)
            lbrow = T(s, ph, "lbrow", [1, DEPTH, 1024], F32, dma=True)
            mx = T(s, ph, "mx", [1, 1024], F32)
            sm = T(s, ph, "sm", [1, 1024], F32)
            cum = T(s, ph, "cum", [1, 1024], F32)
            s.dma(lg[:], self.dram["lb_logits"].rearrange("(o l) d -> o l d", o=1), writes=[lg.b], dsem=lg.d)
            s.op("dve", lambda e: e.tensor_copy(mx[:], lg[:, 0, :]), reads=[lg.b], writes=[mx.b])
            for l in range(1, DEPTH):
                s.op("dve", lambda e, l=l: e.tensor_tensor(out=mx[:], in0=mx[:], in1=lg[:, l, :], op=ALU.max), reads=[mx.b, lg.b], accs=[mx.b])
            for l in range(DEPTH):
                s.op("dve", lambda e, l=l: e.tensor_tensor(out=ex[:, l, :], in0=lg[:, l, :], in1=mx[:], op=ALU.subtract), reads=[lg.b, mx.b],
                     writes=[ex.b] if l == 0 else (), accs=[ex.b] if l else ())
            s.op("act", lambda e: e.activation(out=ex[:], in_=ex[:], func=AF.Exp), reads=[ex.b], accs=[ex.b])
            s.op("dve", lambda e: e.tensor_copy(sm[:], ex[:, 0, :]), reads=[ex.b], writes=[sm.b])
            for l in range(1, DEPTH):
                s.op("dve", lambda e, l=l: e.tensor_tensor(out=sm[:], in0=sm[:], in1=ex[:, l, :], op=ALU.add), reads=[sm.b, ex.b], accs=[sm.b])
            s.op("dve", lambda e: e.reciprocal(sm[:], sm[:]), reads=[sm.b], accs=[sm.b])
            s.op("dve", lambda e: e.memset(lbrow[:, 0, :], 0.0), writes=[lbrow.b])
            s.op("dve", lambda e: e.memset(cum[:], 0.0), writes=[cum.b])
            for l in range(1, DEPTH):
                s.op("dve", lambda e, l=l: e.tensor_tensor(out=ex[:, l, :], in0=ex[:, l, :], in1=sm[:], op=ALU.mult), reads=[ex.b, sm.b], accs=[ex.b])
                s.op("dve", lambda e, l=l: e.tensor_tensor(out=cum[:], in0=cum[:], in1=ex[:, l, :], op=ALU.add), reads=[cum.b, ex.b], accs=[cum.b])
                s.op("dve", lambda e, l=l: e.tensor_copy(lbrow[:, l, :], cum[:]), reads=[cum.b], accs=[lbrow.b])
            s.dma(lb_d.rearrange("(o l) d -> o l d", o=1), lbrow[:], reads=[lbrow.b], dsem=lbrow.d)
            relT = T(s, ph, "relT", [33, 24], F32, dma=True)
            oh = T(s, ph, "oh", [33, 2 * 383], F32, dma=True)
            Zsb = T(s, ph, "Zsb", [24, 2, 384], F32, dma=True)
            psz = [T(s, ph, "psz%d" % i, [128, 512], F32, psum=True) for i in range(2)]
            s.op("pool", lambda e: e.memset(relT[:], -1e30), writes=[relT.b])
            s.dma(relT[0:32, :], self.dram["rel_table"], reads=[relT.b], accs=[relT.b], dsem=relT.d)
            s.dma(oh[:], self.dram["t5_onehot"], writes=[oh.b], dsem=oh.d)
            s.op("pool", lambda e: e.memset(Zsb[:], 0.0), writes=[Zsb.b])
            for j in range(2):
                s.op("pe", lambda e, j=j: e.matmul(psz[j][0:24, 0:383], relT[:], oh[:, j * 383:(j + 1) * 383], start=True, stop=True),
                     reads=[relT.b, oh.b], writes=[psz[j].b])
                s.op("act", lambda e, j=j: e.copy(Zsb[:, j, 0:383], psz[j][0:24, 0:383]), reads=[psz[j].b], accs=[Zsb.b])
            s.dma(Z_d.rearrange("j h i -> h j i"), Zsb[:], reads=[Zsb.b], dsem=Zsb.d)
            s.flush()
        with contextlib.ExitStack() as ph:
            dummy = T(s, ph, "dummy", [1, 8], F32, dma=True)
            for j, (Y, nh, h0) in enumerate(((self.Yc_d, 16, 8), (self.Yb_d, 8, 0))):
                for h in range(nh):
                    src = bass.AP(tensor=Z_d.tensor, offset=(j * 24 + h0 + h) * 384, ap=[[0, 128], [1, 383]])
                    dst = bass.AP(tensor=Y, offset=h * K.YSZ, ap=[[K.PITCH + 1, 128], [1, 383]])
                    s.dma(dst, src, accs=[dummy.b], dsem=dummy.d)
            s.flush()

    def bias_view(self, Y, h):
        return bass.AP(tensor=Y, offset=h * K.YSZ + 127, ap=[[K.PITCH, 128], [1, 256]])

    def swa_phase(self, l):
        cfg, s = self.cfg, self.s
        S = cfg["S"]
        NB = S // 128
        mx_ = self.mx
        with contextlib.ExitStack() as ph:
            ps = [T(s, ph, "ps%d" % i, [128, 512], F32, psum=True) for i in range(8)]
            stg = T(s, ph, "stg", [128, S], F32, dma=True)
            qb = T(s, ph, "qb", [128, S], BF16)
            kdup = [T(s, ph, "kdup%d" % g, [128, S], BF16) for g in range(2)]
            vb = T(s, ph, "vb", [128, NB, 128], BF16)
            vst = T(s, ph, "vst", [128, NB, 128], F32, dma=True)
            biasC = T(s, ph, "biasC", [128, 16, 256], F32, dma=True)
            sinkB = T(s, ph, "sinkB", [128, 16], F32)
            srow = T(s, ph, "srow", [1, 16], F32, dma=True)
            lg = T(s, ph, "lg", [128, 256], F32)
            pe_ = T(s, ph, "pexp", [128, 256], F32)
            pn = T(s, ph, "pn", [128, 256], BF16)
            pT = T(s, ph, "pT", [128, 2, 128], BF16)
            sm = T(s, ph, "sm", [128, 4], F32)
            yc = T(s, ph, "yc", [128, NB, 1024], BF16)
            ycT = T(s, ph, "ycT", [128, 8, S], BF16, dma=True)
            for h in range(16):
                s.dma(biasC[:, h, :], self.bias_view(self.Yc_d, h), writes=[biasC.b] if h == 0 else (), accs=[biasC.b] if h else (), dsem=biasC.d)
            s.dma(srow[:], self.dram["sinks"][l].rearrange("(o h) -> o h", o=1), writes=[srow.b], dsem=srow.d)
            self.bcast_rows(sinkB, srow, 16, ps[0], 128, [srow.b], sinkB.b)
            for g in range(2):
                s.dma(stg[0:64, :], mx_["ckT"][g * 64:(g + 1) * 64, :], writes=[stg.b], dsem=stg.d)
                s.dma(stg[64:128, :], mx_["ckT"][g * 64:(g + 1) * 64, :], accs=[stg.b], dsem=stg.d)
                s.op("dve", lambda e, g=g: e.tensor_copy(kdup[g][:], stg[:]), reads=[stg.b], writes=[kdup[g].b])
            s.dma(vst[:], mx_["cv"].rearrange("(n s) d -> s n d", s=128), writes=[vst.b], dsem=vst.d)
            s.op("dve", lambda e: e.tensor_copy(vb[:], vst[:]), reads=[vst.b], writes=[vb.b])
            pc = 0
            for j in range(8):
                g = j // 4
                s.dma(stg[:], mx_["cqT"][j * 128:(j + 1) * 128, :], writes=[stg.b], dsem=stg.d)
                s.op("dve", lambda e: e.tensor_copy(qb[:], stg[:]), reads=[stg.b], writes=[qb.b])
                for hh in range(2):
                    h = 2 * j + hh
                    hb = hh * 64
                    for nb in range(NB):
                        k0 = max(nb - 1, 0) * 128
                        kw = 256 if nb > 0 else 128
                        b0 = 0 if nb > 0 else 128
                        pL = ps[pc % 8]; pc += 1
                        s.op("pe", lambda e, pL=pL, hb=hb, nb=nb, k0=k0, kw=kw, g=g: e.matmul(
                            pL[:, 0:kw], qb[hb:hb + 64, nb * 128:(nb + 1) * 128], kdup[g][hb:hb + 64, k0:k0 + kw], start=True, stop=True),
                            reads=[qb.b, kdup[g].b], writes=[pL.b])
                        s.op("dve", lambda e, pL=pL, kw=kw, b0=b0, h=h: e.scalar_tensor_tensor(
                            out=lg[:, 0:kw], in0=pL[:, 0:kw], scalar=0.125, in1=biasC[:, h, b0:b0 + kw], op0=ALU.mult, op1=ALU.add),
                            reads=[pL.b, biasC.b], writes=[lg.b])
                        s.op("dve", lambda e, kw=kw: e.reduce_max(out=sm[:, 0:1], in_=lg[:, 0:kw], axis=AX.X), reads=[lg.b], writes=[sm.b])
                        s.op("dve", lambda e, h=h: e.tensor_tensor(out=sm[:, 0:1], in0=sm[:, 0:1], in1=sinkB[:, h:h + 1], op=ALU.max),
                             reads=[sm.b, sinkB.b], accs=[sm.b])
                        s.op("dve", lambda e: e.tensor_scalar(out=sm[:, 1:2], in0=sm[:, 0:1], scalar1=-1.0, scalar2=None, op0=ALU.mult),
                             reads=[sm.b], accs=[sm.b])
                        s.op("act", lambda e, kw=kw: e.activation(out=pe_[:, 0:kw], in_=lg[:, 0:kw], func=AF.Exp, bias=sm[:, 1:2]),
                             reads=[lg.b, sm.b], writes=[pe_.b])
                        s.op("act", lambda e, h=h: e.activation(out=sm[:, 2:3], in_=sinkB[:, h:h + 1], func=AF.Exp, bias=sm[:, 1:2]),
                             reads=[sinkB.b, sm.b], accs=[sm.b])
                        s.op("dve", lambda e, kw=kw: e.reduce_sum(out=sm[:, 3:4], in_=pe_[:, 0:kw], axis=AX.X), reads=[pe_.b], accs=[sm.b])
                        s.op("dve", lambda e: e.tensor_tensor(out=sm[:, 3:4], in0=sm[:, 3:4], in1=sm[:, 2:3], op=ALU.add), reads=[sm.b], accs=[sm.b])
                        s.op("dve", lambda e: e.reciprocal(sm[:, 3:4], sm[:, 3:4]), reads=[sm.b], accs=[sm.b])
                        s.op("dve", lambda e, kw=kw: e.tensor_scalar(out=pn[:, 0:kw], in0=pe_[:, 0:kw], scalar1=sm[:, 3:4], scalar2=None, op0=ALU.mult),
                             reads=[pe_.b, sm.b], writes=[pn.b])
                        nkb = kw // 128
                        pt = ps[pc % 8]; pc += 1
                        ptb = pt.t[:].bitcast(BF16)
                        for kb in range(nkb):
                            s.op("pe", lambda e, kb=kb, ptb=ptb: e.transpose(ptb[:, kb * 128:(kb + 1) * 128], pn[:, kb * 128:(kb + 1) * 128], self.identb[:]),
                                 reads=[pn.b, self.identb.b], writes=[pt.b] if kb == 0 else (), accs=[pt.b] if kb else ())
                        s.op("act", lambda e, ptb=ptb, kw=kw: e.copy(pT[:].rearrange("p a b -> p (a b)")[:, 0:kw], ptb[:, 0:kw]), reads=[pt.b], writes=[pT.b])
                        pO = ps[pc % 8]; pc += 1
                        for kb in range(nkb):
                            blk = k0 // 128 + kb
                            s.op("pe", lambda e, kb=kb, blk=blk, pO=pO, g=g, nkb=nkb: e.matmul(
                                pO[:, 0:64], pT[:, kb, :], vb[:, blk, g * 64:(g + 1) * 64], start=(kb == 0), stop=(kb == nkb - 1)),
                                reads=[pT.b, vb.b], writes=[pO.b] if kb == 0 else (), accs=[pO.b] if kb else ())
                        first = (j == 0 and hh == 0 and nb == 0)
                        s.op("act", lambda e, pO=pO, nb=nb, h=h: e.copy(yc[:, nb, h * 64:(h + 1) * 64], pO[:, 0:64]), reads=[pO.b],
                             writes=[yc.b] if first else (), accs=() if first else [yc.b])
            for nb in range(NB):
                pt = ps[pc % 8]; pc += 1
                ptb = pt.t[:].bitcast(BF16)
                for c in range(8):
                    s.op("pe", lambda e, c=c, nb=nb, ptb=ptb: e.transpose(ptb[:, c * 128:(c + 1) * 128], yc[:, nb, c * 128:(c + 1) * 128], self.identb[:]),
                         reads=[yc.b, self.identb.b], writes=[pt.b] if c == 0 else (), accs=[pt.b] if c else ())
                s.op("act", lambda e, nb=nb, ptb=ptb: e.copy(ycT[:, :, nb * 128:(nb + 1) * 128], ptb.rearrange("p (c t) -> p c t", c=8)), reads=[pt.b],
                     writes=[ycT.b] if nb == 0 else (), accs=[ycT.b] if nb else ())
            s.dma(self.mx["yT"][2].rearrange("(c p) s -> p c s", p=128), ycT[:], reads=[ycT.b], dsem=ycT.d)
            s.flush()

    def dsa_phase(self, l):
        cfg, s = self.cfg, self.s
        S, TOPK = cfg["S"], cfg["TOPK"]
        NB = S // 128
        mx_ = self.mx
        SCALE = 128 ** -0.5
        with contextlib.ExitStack() as ph:
            ps = [T(s, ph, "ps%d" % i, [128, 512], F32, psum=True) for i in range(8)]
            stg = T(s, ph, "stg", [128, S], F32, dma=True)
            lat = T(s, ph, "lat", [128, 512], F32, dma=True)
            latn = T(s, ph, "latn", [128, 512], BF16)
            latT = T(s, ph, "latT", [128, 4, S], BF16)
            sq = T(s, ph, "sq", [128, 512], F32)
            sm = T(s, ph, "sm", [128, 8], F32)
            kvg = T(s, ph, "kvg", [128, 512], F32)
            grow_ = T(s, ph, "kvgrow", [1, 512], F32, dma=True)
            wkv = T(s, ph, "wkv", [128, 4, 256], F32, dma=True)
            wkvb = T(s, ph, "wkvb", [128, 4, 256], BF16)
            kT = T(s, ph, "kT", [128, S], BF16)
            vb = T(s, ph, "vb", [128, NB, 128], BF16)
            s.dma(grow_[:], self.dram["kv_norm"][l].rearrange("(o d) -> o d", o=1), writes=[grow_.b], dsem=grow_.d)
            self.bcast_rows(kvg, grow_, 512, ps[0], 128, [grow_.b], kvg.b)
            s.dma(wkv[:], self.dram["w_kv_up"][l].rearrange("(c p) n -> p c n", p=128), writes=[wkv.b], dsem=wkv.d)
            s.op("dve", lambda e: e.tensor_copy(wkvb[:], wkv[:]), reads=[wkv.b], writes=[wkvb.b])
            pc = 0
            for nb in range(NB):
                s.dma(lat[:], mx_["blat"][nb * 128:(nb + 1) * 128, :], writes=[lat.b], dsem=lat.d)
                s.op("dve", lambda e, nb=nb: e.tensor_tensor(out=sq[:], in0=lat[:], in1=lat[:], op=ALU.mult), reads=[lat.b], writes=[sq.b])
                s.op("dve", lambda e: e.reduce_sum(out=sm[:, 0:1], in_=sq[:], axis=AX.X), reads=[sq.b], writes=[sm.b])
                self.rsqrt_mean(sm[:, 1:2], sm[:, 0:1], sm.b, sm.b, 512)
                s.op("dve", lambda e, nb=nb: e.scalar_tensor_tensor(out=latn[:], in0=lat[:], scalar=sm[:, 1:2], in1=kvg[:], op0=ALU.mult, op1=ALU.mult),
                     reads=[lat.b, sm.b, kvg.b], writes=[latn.b])
                pt = ps[pc % 8]; pc += 1
                ptb = pt.t[:].bitcast(BF16)
                for c in range(4):
                    s.op("pe", lambda e, c=c, ptb=ptb: e.transpose(ptb[:, c * 128:(c + 1) * 128], latn[:, c * 128:(c + 1) * 128], self.identb[:]),
                         reads=[latn.b, self.identb.b], writes=[pt.b] if c == 0 else (), accs=[pt.b] if c else ())
                s.op("act", lambda e, nb=nb, ptb=ptb: e.copy(latT[:, :, nb * 128:(nb + 1) * 128], ptb[:, 0:512].rearrange("p (c t) -> p c t", c=4)),
                     reads=[pt.b], writes=[latT.b] if nb == 0 else (), accs=[latT.b] if nb else ())
            for t0 in range(0, S, 512):
                p = ps[pc % 8]; pc += 1
                for c in range(4):
                    s.op("pe", lambda e, c=c, p=p, t0=t0: e.matmul(p[:], wkvb[:, c, 0:128], latT[:, c, t0:t0 + 512], start=(c == 0), stop=(c == 3)),
                         reads=[wkvb.b, latT.b], writes=[p.b] if c == 0 else (), accs=[p.b] if c else ())
                s.op("act", lambda e, p=p, t0=t0: e.copy(kT[:, t0:t0 + 512], p[:]), reads=[p.b], writes=[kT.b] if t0 == 0 else (), accs=[kT.b] if t0 else ())
            for nb in range(NB):
                p = ps[pc % 8]; pc += 1
                for c in range(4):
                    s.op("pe", lambda e, c=c, p=p, nb=nb: e.matmul(p[:, 0:128], latT[:, c, nb * 128:(nb + 1) * 128], wkvb[:, c, 128:256], start=(c == 0), stop=(c == 3)),
                         reads=[wkvb.b, latT.b], writes=[p.b] if c == 0 else (), accs=[p.b] if c else ())
                s.op("act", lambda e, p=p, nb=nb: e.copy(vb[:, nb, :], p[:, 0:128]), reads=[p.b], writes=[vb.b] if nb == 0 else (), accs=[vb.b] if nb else ())
            ikd = T(s, ph, "ikd", [128, S], BF16)
            iqb = T(s, ph, "iqb", [128, 8, S], BF16)
            qb = T(s, ph, "qb", [128, 8, S], BF16)
            iw = T(s, ph, "iw", [128, NB, 16], F32, dma=True)
            biasB = T(s, ph, "biasB", [128, 8, 256], F32, dma=True)
            cB = T(s, ph, "cB", [128, 24], F32)
            crow = T(s, ph, "crow", [1, 24], F32, dma=True)
            s.dma(stg[0:64, :], mx_["bikT"], writes=[stg.b], dsem=stg.d)
            s.dma(stg[64:128, :], mx_["bikT"], accs=[stg.b], dsem=stg.d)
            s.op("dve", lambda e: e.tensor_copy(ikd[:], stg[:]), reads=[stg.b], writes=[ikd.b])
            for c in range(8):
                s.dma(stg[:], mx_["biqT"][c * 128:(c + 1) * 128, :], writes=[stg.b], dsem=stg.d)
                s.op("dve", lambda e, c=c: e.tensor_copy(iqb[:, c, :], stg[:]), reads=[stg.b], writes=[iqb.b] if c == 0 else (), accs=[iqb.b] if c else ())
            for c in range(8):
                s.dma(stg[:], mx_["bqT"][c * 128:(c + 1) * 128, :], writes=[stg.b], dsem=stg.d)
                s.op("dve", lambda e, c=c: e.tensor_copy(qb[:, c, :], stg[:]), reads=[stg.b], writes=[qb.b] if c == 0 else (), accs=[qb.b] if c else ())
            s.dma(iw[:], mx_["biw"].rearrange("(n t) h -> t n h", t=128), writes=[iw.b], dsem=iw.d)
            for h in range(8):
                s.dma(biasB[:, h, :], self.bias_view(self.Yb_d, h), writes=[biasB.b] if h == 0 else (), accs=[biasB.b] if h else (), dsem=biasB.d)
            s.dma(crow[:], self.dram["rel_table"][31:32, :], writes=[crow.b], dsem=crow.d)
            self.bcast_rows(cB, crow, 24, ps[0], 128, [crow.b], cB.b)
            acc = T(s, ph, "acc", [128, S], F32)
            work = T(s, ph, "work", [128, S], F32)
            rl = T(s, ph, "rl", [128, S], F32)
            mb = work
            m8 = T(s, ph, "m8", [128, 8], F32)
            lg = T(s, ph, "lg", [128, S], F32)
            pn = T(s, ph, "pn", [128, S], BF16)
            pT = T(s, ph, "pT", [128, NB, 128], BF16)
            ybT = T(s, ph, "ybT", [128, 8, S], BF16, dma=True)
            for qt in range(NB):
                kw = (qt + 1) * 128
                q0 = qt * 128
                use_topk = kw > TOPK
                if use_topk:
                    for ih in range(16):
                        c, hb = ih // 2, (ih % 2) * 64
                        pb = (ih % 2) * 4
                        for n0 in range(0, kw, 512):
                            w = min(512, kw - n0)
                            p = ps[pb + n0 // 512]
                            s.op("pe", lambda e, p=p, c=c, hb=hb, n0=n0, w=w, q0=q0: e.matmul(
                                p[:, 0:w], iqb[hb:hb + 64, c, q0:q0 + 128], ikd[hb:hb + 64, n0:n0 + w], start=True, stop=True),
                                reads=[iqb.b, ikd.b], writes=[p.b])
                            s.op("act", lambda e, p=p, n0=n0, w=w: e.activation(out=rl[:, n0:n0 + w], in_=p[:, 0:w], func=AF.Relu),
                                 reads=[p.b], writes=[rl.b] if n0 == 0 else (), accs=[rl.b] if n0 else ())
                        if ih == 0:
                            s.op("dve", lambda e, kw=kw, qt=qt: e.tensor_scalar(out=acc[:, 0:kw], in0=rl[:, 0:kw], scalar1=iw[:, qt, 0:1], scalar2=None, op0=ALU.mult),
                                 reads=[rl.b, iw.b], writes=[acc.b])
                        else:
                            s.op("dve", lambda e, kw=kw, qt=qt, ih=ih: e.scalar_tensor_tensor(
                                out=acc[:, 0:kw], in0=rl[:, 0:kw], scalar=iw[:, qt, ih:ih + 1], in1=acc[:, 0:kw], op0=ALU.mult, op1=ALU.add),
                                reads=[rl.b, iw.b, acc.b], accs=[acc.b])
                    s.op("dve", lambda e, q0=q0: e.tensor_tensor(out=acc[:, q0:q0 + 128], in0=acc[:, q0:q0 + 128], in1=self.cmask[:], op=ALU.add),
                         reads=[acc.b, self.cmask.b], accs=[acc.b])
                    s.op("dve", lambda e, kw=kw: e.tensor_copy(work[:, 0:kw], acc[:, 0:kw]), reads=[acc.b], writes=[work.b])
                    for r in range(TOPK // 8):
                        s.op("dve", lambda e, kw=kw: e.max(out=m8[:], in_=work[:, 0:kw]), reads=[work.b], writes=[m8.b])
                        if r < TOPK // 8 - 1:
                            s.op("dve", lambda e, kw=kw: e.match_replace(out=work[:, 0:kw], in_to_replace=m8[:], in_values=work[:, 0:kw], imm_value=-3e38),
                                 reads=[work.b, m8.b], accs=[work.b])
                    s.op("dve", lambda e, kw=kw: e.tensor_scalar(out=mb[:, 0:kw], in0=acc[:, 0:kw], scalar1=m8[:, 7:8], scalar2=-1e30,
                                                            op0=ALU.is_lt, op1=ALU.mult), reads=[acc.b, m8.b], writes=[mb.b])
                for h in range(8):
                    pb = (h % 2) * 4
                    for n0 in range(0, kw, 512):
                        w = min(512, kw - n0)
                        p = ps[pb + n0 // 512]
                        s.op("pe", lambda e, p=p, h=h, n0=n0, w=w, q0=q0: e.matmul(p[:, 0:w], qb[:, h, q0:q0 + 128], kT[:, n0:n0 + w], start=True, stop=True),
                             reads=[qb.b, kT.b], writes=[p.b])
                    w0 = max(kw - 256, 0)
                    first = True
                    for n0 in range(0, w0, 512):
                        w = min(512, w0 - n0)
                        p = ps[pb + n0 // 512]
                        s.op("dve", lambda e, p=p, n0=n0, w=w, h=h: e.tensor_scalar(out=lg[:, n0:n0 + w], in0=p[:, 0:w], scalar1=SCALE, scalar2=cB[:, h:h + 1],
                                                                               op0=ALU.mult, op1=ALU.add), reads=[p.b, cB.b],
                             writes=[lg.b] if first else (), accs=() if first else [lg.b])
                        first = False
                    ww = kw - w0
                    b0 = 256 - ww
                    for n0 in range(w0, kw, 128):
                        p = ps[pb + n0 // 512]
                        o = n0 % 512
                        bo = b0 + (n0 - w0)
                        s.op("dve", lambda e, p=p, n0=n0, o=o, bo=bo, h=h: e.scalar_tensor_tensor(
                            out=lg[:, n0:n0 + 128], in0=p[:, o:o + 128], scalar=SCALE, in1=biasB[:, h, bo:bo + 128], op0=ALU.mult, op1=ALU.add),
                            reads=[p.b, biasB.b], writes=[lg.b] if first else (), accs=() if first else [lg.b])
                        first = False
                    if use_topk:
                        s.op("dve", lambda e, kw=kw: e.tensor_tensor(out=lg[:, 0:kw], in0=lg[:, 0:kw], in1=mb[:, 0:kw], op=ALU.add), reads=[lg.b, mb.b], accs=[lg.b])
                    s.op("dve", lambda e, kw=kw: e.reduce_max(out=sm[:, 2:3], in_=lg[:, 0:kw], axis=AX.X), reads=[lg.b], writes=[sm.b])
                    s.op("dve", lambda e: e.tensor_scalar(out=sm[:, 3:4], in0=sm[:, 2:3], scalar1=-1.0, scalar2=None, op0=ALU.mult), reads=[sm.b], accs=[sm.b])
                    s.op("act", lambda e, kw=kw: e.activation(out=lg[:, 0:kw], in_=lg[:, 0:kw], func=AF.Exp, bias=sm[:, 3:4]), reads=[lg.b, sm.b], accs=[lg.b])
                    s.op("dve", lambda e, kw=kw: e.reduce_sum(out=sm[:, 4:5], in_=lg[:, 0:kw], axis=AX.X), reads=[lg.b], accs=[sm.b])
                    s.op("dve", lambda e: e.reciprocal(sm[:, 4:5], sm[:, 4:5]), reads=[sm.b], accs=[sm.b])
                    s.op("dve", lambda e, kw=kw: e.tensor_scalar(out=pn[:, 0:kw], in0=lg[:, 0:kw], scalar1=sm[:, 4:5], scalar2=None, op0=ALU.mult),
                         reads=[lg.b, sm.b], writes=[pn.b])
                    nkb = kw // 128
                    for k8 in range(0, nkb, 8):
                        pt = ps[pc % 8]; pc += 1
                        ptb = pt.t[:].bitcast(BF16)
                        nn = min(8, nkb - k8)
                        for kb in range(nn):
                            s.op("pe", lambda e, kb=kb, k8=k8, ptb=ptb: e.transpose(ptb[:, kb * 128:(kb + 1) * 128], pn[:, (k8 + kb) * 128:(k8 + kb + 1) * 128], self.identb[:]),
                                 reads=[pn.b, self.identb.b], writes=[pt.b] if kb == 0 else (), accs=[pt.b] if kb else ())
                        s.op("act", lambda e, ptb=ptb, k8=k8, nn=nn: e.copy(pT[:, k8:k8 + nn, :], ptb[:, 0:nn * 128].rearrange("p (a b) -> p a b", b=128)),
                             reads=[pt.b], writes=[pT.b] if k8 == 0 else (), accs=[pT.b] if k8 else ())
                    pO = ps[pc % 8]; pc += 1
                    for kb in range(nkb):
                        s.op("pe", lambda e, kb=kb, pO=pO, nkb=nkb: e.matmul(pO[:, 0:128], vb[:, kb, :], pT[:, kb, :], start=(kb == 0), stop=(kb == nkb - 1)),
                             reads=[pT.b, vb.b], writes=[pO.b] if kb == 0 else (), accs=[pO.b] if kb else ())
                    first = (qt == 0 and h == 0)
                    s.op("act", lambda e, pO=pO, h=h, q0=q0: e.copy(ybT[:, h, q0:q0 + 128], pO[:, 0:128]), reads=[pO.b],
                         writes=[ybT.b] if first else (), accs=() if first else [ybT.b])
            s.dma(self.mx["yT"][1].rearrange("(c p) s -> p c s", p=128), ybT[:], reads=[ybT.b], dsem=ybT.d)
            s.flush()

    def hgrn_phase(self, l):
        cfg, s = self.cfg, self.s
        S = cfg["S"]
        CS = 32
        SEG = cfg.get("HSEG", min(512, S))
        NCHT = S // CS
        NCH = SEG // CS
        mx_ = self.mx
        lb_d = self.dram["lb_d"]
        with contextlib.ExitStack() as ph:
            ps = [T(s, ph, "ps%d" % i, [128, 512], F32, psum=True) for i in range(8)]
            lbc = T(s, ph, "lbc", [128, 8], F32)
            omc = T(s, ph, "omc", [128, 8], F32)
            lbB = T(s, ph, "lbB", [CS, 1024], F32)
            omB = T(s, ph, "omB", [CS, 1024], F32)
            hgB = T(s, ph, "hgB", [CS, 128], F32)
            row = T(s, ph, "row", [1, 1024], F32, dma=True)
            row2 = T(s, ph, "row2", [1, 128], F32, dma=True)
            self.colload(ph, None, lb_d[l], 8, ps[0], "lb")
            s.op("dve", lambda e: e.tensor_copy(lbc[:], ps[0][:, 0:8]), reads=[ps[0].b], writes=[lbc.b])
            s.op("dve", lambda e: e.tensor_scalar(out=omc[:], in0=lbc[:], scalar1=-1.0, scalar2=1.0, op0=ALU.mult, op1=ALU.add), reads=[lbc.b], writes=[omc.b])
            s.dma(row[:], lb_d[l:l + 1, :], writes=[row.b], dsem=row.d)
            self.bcast_rows(lbB, row, 1024, ps[1], CS, [row.b], lbB.b)
            s.op("dve", lambda e: e.tensor_scalar(out=omB[:], in0=lbB[:], scalar1=-1.0, scalar2=1.0, op0=ALU.mult, op1=ALU.add), reads=[lbB.b], writes=[omB.b])
            s.dma(row2[:], self.dram["hgrn_norm"][l].rearrange("(o d) -> o d", o=1), writes=[row2.b], dsem=row2.d)
            self.bcast_rows(hgB, row2, 128, ps[1], CS, [row2.b], hgB.b)
            qT = T(s, ph, "qT", [128, NCH, CS], F32, dma=True)
            kTf = T(s, ph, "kTf", [128, NCH, CS], F32, dma=True)
            bT = T(s, ph, "bT", [128, NCH, CS], F32)
            d1 = T(s, ph, "d1", [128, NCH, CS], F32)
            bmid = T(s, ph, "bmid", [128, NCH], F32)
            bend = T(s, ph, "bend", [128, NCH], F32)
            qtil = T(s, ph, "qtil", [128, NCH, CS], BF16)
            ktil = T(s, ph, "ktil", [128, NCH, CS], BF16)
            qb = T(s, ph, "qbb", [128, NCH, CS], BF16)
            f_tm = T(s, ph, "f_tm", [CS, NCH, 128], F32, dma=True)
            lf_tm = T(s, ph, "lf_tm", [CS, NCH, 128], F32)
            k_tm = T(s, ph, "k_tm", [CS, NCH, 128], F32)
            i_tm = T(s, ph, "i_tm", [CS, NCH, 128], F32, dma=True)
            g_tm = T(s, ph, "g_tm", [CS, NCH, 128], F32, dma=True)
            kend = T(s, ph, "kend", [CS, NCH, 128], BF16)
            vb = T(s, ph, "vb", [CS, NCH, 128], BF16)
            o_sb = T(s, ph, "o_sb", [CS, NCH, 128], F32)
            ssq = T(s, ph, "ssq", [CS, NCH], F32)
            att = T(s, ph, "att", [CS, CS], BF16)
            state = T(s, ph, "state", [128, 128], F32)
            stb = T(s, ph, "stb", [128, 128], BF16)
            yaT = T(s, ph, "yaT", [128, SEG], BF16, dma=True)
            pc = 2
            for h, sg in [(h_, g_) for h_ in range(8) for g_ in range(S // SEG)]:
                hs = slice(h * 128, (h + 1) * 128)
                ts = slice(sg * SEG, (sg + 1) * SEG)
                cs_ = slice(sg * NCH, (sg + 1) * NCH)
                bc = lambda t, hs=hs: t[:, hs].unsqueeze(1).to_broadcast([CS, NCH, 128])
                s.dma(qT[:].rearrange("p n t -> p (n t)"), mx_["aqT"][hs, ts], writes=[qT.b], dsem=qT.d)
                s.dma(kTf[:].rearrange("p n t -> p (n t)"), mx_["afT"][hs, ts], writes=[kTf.b], dsem=kTf.d)
                s.dma(f_tm[:], mx_["af"].rearrange("(n t) d -> t n d", t=CS)[:, cs_, hs], writes=[f_tm.b], dsem=f_tm.d)
                s.dma(i_tm[:], mx_["ai"].rearrange("(n t) d -> t n d", t=CS)[:, cs_, hs], writes=[i_tm.b], dsem=i_tm.d)
                s.dma(g_tm[:], mx_["ag"].rearrange("(n t) d -> t n d", t=CS)[:, cs_, hs], writes=[g_tm.b], dsem=g_tm.d)
                s.op("act", lambda e: e.activation(out=kTf[:], in_=kTf[:], func=AF.Sigmoid, scale=-1.0), reads=[kTf.b], accs=[kTf.b])
                s.op("dve", lambda e, h=h: e.tensor_scalar(out=kTf[:], in0=kTf[:], scalar1=omc[:, h:h + 1], scalar2=None, op0=ALU.mult), reads=[kTf.b, omc.b], accs=[kTf.b])
                s.op("act", lambda e: e.activation(out=k_tm[:], in_=f_tm[:], func=AF.Sigmoid, scale=-1.0), reads=[f_tm.b], writes=[k_tm.b])
                s.op("dve", lambda e, bc=bc: e.tensor_tensor(out=k_tm[:], in0=k_tm[:], in1=bc(omB), op=ALU.mult), reads=[k_tm.b, omB.b], accs=[k_tm.b])
                s.op("act", lambda e: e.activation(out=lf_tm[:], in_=f_tm[:], func=AF.Sigmoid), reads=[f_tm.b], writes=[lf_tm.b])
                s.op("dve", lambda e, bc=bc: e.tensor_tensor(out=lf_tm[:], in0=lf_tm[:], in1=bc(omB), op=ALU.mult), reads=[lf_tm.b, omB.b], accs=[lf_tm.b])
                s.op("dve", lambda e, bc=bc: e.tensor_tensor(out=lf_tm[:], in0=lf_tm[:], in1=bc(lbB), op=ALU.add), reads=[lf_tm.b, lbB.b], accs=[lf_tm.b])
                s.op("dve", lambda e: e.tensor_scalar(out=lf_tm[:], in0=lf_tm[:], scalar1=1e-20, scalar2=None, op0=ALU.max), reads=[lf_tm.b], accs=[lf_tm.b])
                s.op("act", lambda e: e.activation(out=lf_tm[:], in_=lf_tm[:], func=AF.Ln), reads=[lf_tm.b], accs=[lf_tm.b])
                s.op("act", lambda e: e.activation(out=vb[:], in_=i_tm[:], func=AF.Silu), reads=[i_tm.b], writes=[vb.b])
                s.op("act", lambda e: e.activation(out=g_tm[:], in_=g_tm[:], func=AF.Silu), reads=[g_tm.b], accs=[g_tm.b])
                NPB = 512 // CS
                for n8 in range(0, NCH, NPB):
                    p = ps[pc % 8]; pc += 1
                    for n in range(n8, min(n8 + NPB, NCH)):
                        s.op("pe", lambda e, p=p, n=n, n8=n8: e.matmul(p[:, (n - n8) * CS:(n - n8 + 1) * CS], lf_tm[:, n, :], self.U[0:CS, 0:CS], start=True, stop=True),
                             reads=[lf_tm.b, self.U.b], writes=[p.b] if n == n8 else (), accs=[p.b] if n != n8 else ())
                    nn = min(NPB, NCH - n8)
                    s.op("act", lambda e, p=p, n8=n8, nn=nn: e.copy(bT[:, n8:n8 + nn, :], p[:, 0:nn * CS].rearrange("p (a b) -> p a b", b=CS)),
                         reads=[p.b], writes=[bT.b] if n8 == 0 else (), accs=[bT.b] if n8 else ())
                for n4 in range(0, NCH, 4):
                    p = ps[pc % 8]; pc += 1
                    for n in range(n4, min(n4 + 4, NCH)):
                        s.op("pe", lambda e, p=p, n=n, n4=n4: e.matmul(p[0:CS, (n - n4) * 128:(n - n4 + 1) * 128], self.Mgt[0:CS, 0:CS], lf_tm[:, n, :], start=True, stop=True),
                             reads=[lf_tm.b, self.Mgt.b], writes=[p.b] if n == n4 else (), accs=[p.b] if n != n4 else ())
                    nn = min(4, NCH - n4)
                    s.op("act", lambda e, p=p, n4=n4, nn=nn: e.activation(out=o_sb[:, n4:n4 + nn, :], in_=p[0:CS, 0:nn * 128].rearrange("p (a b) -> p a b", b=128), func=AF.Exp),
                         reads=[p.b], writes=[o_sb.b] if n4 == 0 else (), accs=[o_sb.b] if n4 else ())
                s.op("dve", lambda e: e.tensor_tensor(out=kend[:], in0=k_tm[:], in1=o_sb[:], op=ALU.mult), reads=[k_tm.b, o_sb.b], writes=[kend.b])
                s.op("dve", lambda e: e.tensor_copy(bmid[:], bT[:, :, CS // 2 - 1]), reads=[bT.b], writes=[bmid.b])
                s.op("dve", lambda e: e.tensor_tensor(out=d1[:], in0=bT[:], in1=bmid[:].unsqueeze(2).to_broadcast([128, NCH, CS]), op=ALU.subtract),
                     reads=[bT.b, bmid.b], writes=[d1.b])
                s.op("act", lambda e: e.activation(out=bT[:], in_=bT[:], func=AF.Exp), reads=[bT.b], accs=[bT.b])
                s.op("dve", lambda e: e.tensor_copy(bend[:], bT[:, :, CS - 1]), reads=[bT.b], writes=[bend.b])
                s.op("dve", lambda e: e.tensor_tensor(out=qb[:], in0=qT[:], in1=bT[:], op=ALU.mult), reads=[qT.b, bT.b], writes=[qb.b])
                s.op("act", lambda e: e.activation(out=bT[:], in_=d1[:], func=AF.Exp), reads=[d1.b], writes=[bT.b])
                s.op("dve", lambda e: e.tensor_tensor(out=qtil[:], in0=qT[:], in1=bT[:], op=ALU.mult), reads=[qT.b, bT.b], writes=[qtil.b])
                s.op("act", lambda e: e.activation(out=bT[:], in_=d1[:], func=AF.Exp, scale=-1.0), reads=[d1.b], writes=[bT.b])
                s.op("dve", lambda e: e.tensor_tensor(out=ktil[:], in0=kTf[:], in1=bT[:], op=ALU.mult), reads=[kTf.b, bT.b], writes=[ktil.b])
                for n in range(NCH):
                    gn = sg * NCH + n
                    pA = ps[pc % 8]; pc += 1
                    s.op("pe", lambda e, pA=pA, n=n: e.matmul(pA[0:CS, 0:CS], ktil[:, n, :], qtil[:, n, :], start=True, stop=True),
                         reads=[ktil.b, qtil.b], writes=[pA.b])
                    s.op("dve", lambda e, pA=pA: e.tensor_tensor(out=att[:], in0=pA[0:CS, 0:CS], in1=self.U[0:CS, 0:CS], op=ALU.mult), reads=[pA.b, self.U.b], writes=[att.b])
                    pO = ps[pc % 8]; pc += 1
                    s.op("pe", lambda e, pO=pO, n=n, gn=gn: e.matmul(pO[0:CS, 0:128], att[:], vb[:, n, :], start=True, stop=(gn == 0)),
                         reads=[att.b, vb.b], writes=[pO.b])
                    if gn > 0:
                        s.op("pe", lambda e, pO=pO, n=n: e.matmul(pO[0:CS, 0:128], qb[:, n, :], stb[:], start=False, stop=True),
                             reads=[qb.b, stb.b], accs=[pO.b])
                    s.op("act", lambda e, pO=pO, n=n: e.copy(o_sb[:, n, :], pO[0:CS, 0:128]), reads=[pO.b], writes=[o_sb.b] if n == 0 else (), accs=[o_sb.b] if n else ())
                    if gn < NCHT - 1:
                        pS = ps[pc % 8]; pc += 1
                        s.op("pe", lambda e, pS=pS, n=n: e.matmul(pS[:, 0:128], kend[:, n, :], vb[:, n, :], start=True, stop=True),
                             reads=[kend.b, vb.b], writes=[pS.b])
                        if gn == 0:
                            s.op("dve", lambda e, pS=pS: e.tensor_copy(state[:], pS[:, 0:128]), reads=[pS.b], writes=[state.b])
                        else:
                            s.op("dve", lambda e, pS=pS, n=n: e.scalar_tensor_tensor(out=state[:], in0=state[:], scalar=bend[:, n:n + 1], in1=pS[:, 0:128],
                                                                                 op0=ALU.mult, op1=ALU.add), reads=[state.b, bend.b, pS.b], accs=[state.b])
                        s.op("act", lambda e: e.copy(stb[:], state[:]), reads=[state.b], writes=[stb.b])
                s.op("dve", lambda e: e.tensor_tensor(out=lf_tm[:], in0=o_sb[:], in1=o_sb[:], op=ALU.mult), reads=[o_sb.b], writes=[lf_tm.b])
                s.op("dve", lambda e: e.tensor_reduce(out=ssq[:], in_=lf_tm[:], axis=AX.X, op=ALU.add), reads=[lf_tm.b], writes=[ssq.b])
                self.rsqrt_mean(ssq[:], ssq[:], ssq.b, ssq.b, 128, npart=CS)
                s.op("dve", lambda e: e.tensor_tensor(out=o_sb[:], in0=o_sb[:], in1=ssq[:].unsqueeze(2).to_broadcast([CS, NCH, 128]), op=ALU.mult),
                     reads=[o_sb.b, ssq.b], accs=[o_sb.b])
                s.op("dve", lambda e: e.tensor_tensor(out=o_sb[:], in0=o_sb[:], in1=hgB[:].unsqueeze(1).to_broadcast([CS, NCH, 128]), op=ALU.mult),
                     reads=[o_sb.b, hgB.b], accs=[o_sb.b])
                s.op("dve", lambda e: e.tensor_tensor(out=o_sb[:], in0=o_sb[:], in1=g_tm[:], op=ALU.mult), reads=[o_sb.b, g_tm.b], accs=[o_sb.b])
                for n8 in range(0, NCH, NPB):
                    p = ps[pc % 8]; pc += 1
                    nn = min(NPB, NCH - n8)
                    for n in range(n8, n8 + nn):
                        s.op("pe", lambda e, p=p, n=n, n8=n8: e.transpose(p[:, (n - n8) * CS:(n - n8 + 1) * CS], o_sb[:, n, :], self.identf[0:CS, 0:CS]),
                             reads=[o_sb.b, self.identf.b], writes=[p.b] if n == n8 else (), accs=[p.b] if n != n8 else ())
                    s.op("act", lambda e, p=p, n8=n8, nn=nn: e.copy(yaT[:, n8 * CS:(n8 + nn) * CS], p[:, 0:nn * CS]), reads=[p.b],
                         writes=[yaT.b] if n8 == 0 else (), accs=[yaT.b] if n8 else ())
                s.dma(self.mx["yT"][0][hs, ts], yaT[:], reads=[yaT.b], dsem=yaT.d)
            s.flush()


def build_program(cfg):
    k = K(cfg)
    D, S, DEPTH, DFF, NBE = cfg["D"], cfg["S"], cfg["DEPTH"], cfg["DFF"], cfg["NBE"]
    DIN = 8016 + 3 * D
    for name, shape in (("x", (NBE, S, D)), ("c", (NBE, D)), ("w_c_down", (D, 256)), ("w_c_up", (DEPTH, 256, 9 * D)),
                        ("norm_gains", (DEPTH, 6, D)), ("w_in", (DEPTH, D, DIN)), ("lb_logits", (DEPTH, 1024)),
                        ("hgrn_norm", (DEPTH, 128)), ("kv_norm", (DEPTH, 512)), ("w_kv_up", (DEPTH, 512, 256)),
                        ("rel_table", (32, 24)), ("sinks", (DEPTH, 16)), ("w_branch", (DEPTH, 3, 1024, D)),
                        ("w_out", (DEPTH, D, D)), ("ffn1_in", (DEPTH, D, 2 * DFF)), ("ffn1_out", (DEPTH, DFF, D)),
                        ("ffn2_in", (DEPTH, D, 2 * DFF)), ("ffn2_out", (DEPTH, DFF, D)), ("t5_onehot", (33, 766))):
        k.ext(name, shape)
    out = k.ext("out", (NBE, S, D), kind="ExternalOutput")
    for name in ("w_in", "w_branch", "w_out", "ffn1_in", "ffn1_out", "ffn2_in", "ffn2_out"):
        k.mir[name] = [k.nc.dram_tensor("%s_bf16_%d" % (name, l_), list(k.dram[name].shape[1:]), BF16).ap() for l_ in range(DEPTH)]
    xres = k.scratch("xres", (S, D))
    growd = k.scratch("grow_d", (3, 1, D))
    k.mixer_scratch()
    k.consts()
    k.mod_setup()
    k.setup_phase()
    for b in range(NBE):
        k.cond_phase(k.dram["c"][b])
        for l in range(DEPTH):
            k.mod_phase(l, growd)
            k.ffn_phase(l, 1, k.dram["x"][b] if l == 0 else xres, xres, k.AB[0], k.AB[1], growd[0])
            k.proj_phase(l, xres, k.AB[2], k.AB[3])
            k.hgrn_phase(l)
            k.dsa_phase(l)
            k.swa_phase(l)
            k.merge_phase(l, xres, xres, growd[1])
            k.ffn_phase(l, 2, xres, out[b] if l == DEPTH - 1 else xres, k.AB[4], k.AB[5], growd[2])
    return k


def t5_bucket_np(d):
    d = np.maximum(d, 0)
    df = np.maximum(d, 1).astype(np.float32)
    large = 16 + (np.log(df / np.float32(16)) / np.float32(math.log(128 / 16)) * np.float32(16)).astype(np.int32)
    large = np.minimum(large, 31)
    return np.where(d < 16, d, large)
def t5_onehot():
    oh = np.zeros((33, 2, 383), np.float32)
    for i in range(383):
        dist = 255 - i
        b = int(t5_bucket_np(np.array([dist]))[0])
        if 0 <= dist < 128:
            oh[b, 0, i] = 1
        else:
            oh[32, 0, i] = 1
        if dist >= 0:
            oh[b, 1, i] = 1
        else:
            oh[32, 1, i] = 1
    return oh.reshape(33, 766)


N_CORES = 8


def run_cores(inputs, n_cores, batch_ids):
    cfg = dict(FULL)
    cfg["NBE"] = len(batch_ids[0])
    Sched.SERIAL = False
    prog = build_program(cfg)
    oh = t5_onehot()
    shared = {kk: np.ascontiguousarray(v, dtype=np.float32) for kk, v in inputs.items() if kk not in ("x", "c")}
    in_maps = []
    for ids in batch_ids:
        m = dict(shared)
        m["x"] = np.ascontiguousarray(inputs["x"][ids], dtype=np.float32)
        m["c"] = np.ascontiguousarray(inputs["c"][ids], dtype=np.float32)
        m["t5_onehot"] = oh
        in_maps.append(m)
    res = run_bass_kernel_spmd(prog.nc, in_maps, core_ids=list(range(n_cores)))
    return [np.asarray(r["out"]) for r in res.results]


def kernel(**inputs):
    B = inputs["x"].shape[0]
    per = B // N_CORES
    batch_ids = [list(range(c * per, (c + 1) * per)) for c in range(N_CORES)]
    outs = run_cores(inputs, N_CORES, batch_ids)
    return np.concatenate(outs, axis=0).astype(np.float32)
```
